# Optimizing a Trainium2 kernel written in Bass

```python
import math
import jax
import jax.numpy as jnp
from jax import lax
import numpy as np

D_MODEL = 2048
BATCH = 16
SEQ = 2048
DEPTH = 2

CTX_LEN = 256
GRID_W = 64

MIX_WIDTH = D_MODEL
GROUP_WIDTH = MIX_WIDTH // 4
NORM_EPS = 1e-6
MASK_VALUE = -1e30
LOG_FLOOR = 1e-30

RW_HEAD = 64
RW_HEADS = GROUP_WIDTH // RW_HEAD
RW_DECAY_RANK = 64
RW_ICLR_RANK = 64
RW_GATE_RANK = 128
RW_GN_EPS = 64e-5
RW_COLS = 3 * GROUP_WIDTH + 2 * RW_DECAY_RANK + 2 * RW_ICLR_RANK + RW_GATE_RANK

S5_GROUP = 16
S5_GROUPS = GROUP_WIDTH // S5_GROUP
S5_STATE = 64
S5_DT_MIN = 1e-3
S5_DT_MAX = 1e-1
S5_COLS = GROUP_WIDTH

HG_HEADS = 4
HG_KEY = GROUP_WIDTH // HG_HEADS
HG_VAL = GROUP_WIDTH // HG_HEADS
HG_CHUNK = 16
HG_COLS = 5 * GROUP_WIDTH

AT_HEAD = 64
AT_Q_HEADS = GROUP_WIDTH // AT_HEAD
AT_KV_HEADS = 2
AT_WINDOW = 128
AT_BLOCK = AT_WINDOW
ROPE_BASE = 10000.0
AT_COLS = (AT_Q_HEADS + 2 * AT_KV_HEADS) * AT_HEAD

IN_COLS = RW_COLS + S5_COLS + HG_COLS + AT_COLS

N_EXPERTS = 32
TOP_K = 4
D_EXPERT = D_MODEL
SWIGLU_LIMIT = 7.0
SWIGLU_ALPHA = 1.702
MOE_BLOCK = 512

kernel_name = 'hybrid_flow_backbone_block'


def rmsnorm(x, g, eps=NORM_EPS):
    xf = x.astype(jnp.float32)
    y = xf * lax.rsqrt(jnp.mean(xf * xf, axis=-1, keepdims=True) + eps)
    return (y * g.astype(jnp.float32)).astype(x.dtype)


def split_cols(z, widths):
    cuts, s = [], 0
    for w in widths[:-1]:
        s += w
        cuts.append(s)
    return jnp.split(z, cuts, axis=-1)


def to_heads(t, n_heads):
    return t.reshape(t.shape[:-1] + (n_heads, t.shape[-1] // n_heads))


def centred_shift(z, mu_prev, mu_next):
    z_prev = jnp.pad(z[:, :-1], ((0, 0), (1, 0), (0, 0)))
    z_next = jnp.pad(z[:, 1:], ((0, 0), (0, 1), (0, 0)))
    return z + (z_prev - z) * mu_prev + (z_next - z) * mu_next


def rwkv_prepare(z, mu, w0, w2, a0, a2, k_k, k_a):
    z = centred_shift(z, mu[0], mu[1])
    r, k, v, dw_f, dw_b, da_f, da_b, g_low = split_cols(
        z, [GROUP_WIDTH] * 3 + [RW_DECAY_RANK] * 2 + [RW_ICLR_RANK] * 2 + [RW_GATE_RANK])
    kk = to_heads((k * k_k).astype(jnp.float32), RW_HEADS)
    kk = kk / jnp.maximum(jnp.linalg.norm(kk, axis=-1, keepdims=True), 1e-12)
    dirs = []
    for d, (dw, da) in enumerate(((dw_f, da_f), (dw_b, da_b))):
        w_log = -jax.nn.softplus(-(w0[d] + jnp.tanh(dw) @ w2[d])) - 0.5
        decay = jnp.exp(-jnp.exp(w_log.astype(jnp.float32)))
        a = jax.nn.sigmoid(a0[d] + da @ a2[d])
        k_eff = k * (1.0 + (a - 1.0) * k_a)
        a_h = to_heads(a.astype(jnp.float32), RW_HEADS)
        dirs.append((to_heads(decay, RW_HEADS), to_heads(k_eff, RW_HEADS), -kk, kk * a_h))
    return to_heads(r, RW_HEADS), to_heads(k, RW_HEADS), to_heads(v, RW_HEADS), g_low, dirs


def rwkv7_scan(r, decay, k, v, a_vec, b_vec, s0, reverse):
    xs = tuple(jnp.swapaxes(t.astype(jnp.float32), 0, 1) for t in (r, decay, k, v, a_vec, b_vec))

    def step(S, inp):
        r_t, w_t, k_t, v_t, a_t, b_t = inp
        sa = jnp.einsum('bhvk,bhk->bhv', S, a_t)
        S = S * w_t[:, :, None, :] + sa[..., None] * b_t[:, :, None, :] + v_t[..., None] * k_t[:, :, None, :]
        return S, jnp.einsum('bhvk,bhk->bhv', S, r_t)

    s_final, ys = lax.scan(step, s0, xs, reverse=reverse)
    return jnp.swapaxes(ys, 0, 1), s_final


def rwkv_readout(y, r, k, v, g_low, r_k, ln_w, ln_b, g2):
    mean = jnp.mean(y, axis=-1, keepdims=True)
    var = jnp.var(y, axis=-1, keepdims=True)
    y = ((y - mean) * lax.rsqrt(var + RW_GN_EPS)).reshape(y.shape[:-2] + (GROUP_WIDTH,)) * ln_w + ln_b
    bonus = (jnp.sum(r * k * r_k, axis=-1, keepdims=True) * v).reshape(y.shape)
    g = jax.nn.sigmoid(g_low) @ g2
    return (y + bonus) * g


def rwkv_mixer(zc, zl, mu, w0, w2, a0, a2, g2, k_k, k_a, r_k, ln_w, ln_b, ctx_out):
    rc, kc, vc, gc, dirs_c = rwkv_prepare(zc, mu, w0, w2, a0, a2, k_k, k_a)
    rl, kl, vl, gl, dirs_l = rwkv_prepare(zl, mu, w0, w2, a0, a2, k_k, k_a)
    s_zero = jnp.zeros((zl.shape[0], RW_HEADS, RW_HEAD, RW_HEAD), jnp.float32)
    yc, yl = 0.0, 0.0
    for d, reverse in enumerate((False, True)):
        wc, kec, ac, bc = dirs_c[d]
        wl, kel, al, bl = dirs_l[d]
        yc_d, s_ctx = rwkv7_scan(rc, wc, kec, vc, ac, bc, s_zero, reverse)
        yl_d, _ = rwkv7_scan(rl, wl, kel, vl, al, bl, s_ctx, reverse)
        yc, yl = yc + yc_d, yl + yl_d
    out_l = rwkv_readout(yl, rl, kl, vl, gl, r_k, ln_w, ln_b, g2)
    out_c = rwkv_readout(yc, rc, kc, vc, gc, r_k, ln_w, ln_b, g2) if ctx_out else None
    return out_c, out_l


def s5_discretise(lam_re, lam_im, log_step, b_re, b_im):
    f32 = jnp.float32
    lam_re, lam_im, b_re, b_im = (t.astype(f32) for t in (lam_re, lam_im, b_re, b_im))
    step = jnp.exp(log_step.astype(f32))[:, None]
    zr, zi = lam_re * step, lam_im * step
    e = jnp.exp(zr)
    num_r, num_i = e * jnp.cos(zi) - 1.0, e * jnp.sin(zi)
    den = lam_re * lam_re + lam_im * lam_im
    coef_r = (num_r * lam_re + num_i * lam_im) / den
    coef_i = (num_i * lam_re - num_r * lam_im) / den
    bb_r = coef_r[..., None] * b_re - coef_i[..., None] * b_im
    bb_i = coef_r[..., None] * b_im + coef_i[..., None] * b_re
    return zr, zi, bb_r, bb_i


def complex_affine_combine(e1, e2):
    a1r, a1i, b1r, b1i = e1
    a2r, a2i, b2r, b2i = e2
    return (a2r * a1r - a2i * a1i, a2r * a1i + a2i * a1r,
            a2r * b1r - a2i * b1i + b2r, a2r * b1i + a2i * b1r + b2i)


def s5_scan(u, zr, zi, bb_r, bb_i, h0, reverse):
    L = u.shape[1]
    ut = jnp.swapaxes(u, 0, 1).astype(jnp.float32)
    bu_r = jnp.einsum('lbgc,gpc->lbgp', ut, bb_r)
    bu_i = jnp.einsum('lbgc,gpc->lbgp', ut, bb_i)
    e = jnp.exp(zr)
    a_r = jnp.broadcast_to((e * jnp.cos(zi))[None, None], (L, 1) + zr.shape)
    a_i = jnp.broadcast_to((e * jnp.sin(zi))[None, None], (L, 1) + zr.shape)
    _, _, h_r, h_i = lax.associative_scan(complex_affine_combine, (a_r, a_i, bu_r, bu_i), reverse=reverse)
    if h0 is not None:
        pos = jnp.arange(L, dtype=jnp.float32)
        n = (L - pos) if reverse else (pos + 1.0)
        n = n[:, None, None, None]
        mag = jnp.exp(n * zr)
        p_r, p_i = mag * jnp.cos(n * zi), mag * jnp.sin(n * zi)
        h0r, h0i = h0[0][None], h0[1][None]
        h_r = h_r + p_r * h0r - p_i * h0i
        h_i = h_i + p_r * h0i + p_i * h0r
    final = (h_r[0], h_i[0]) if reverse else (h_r[-1], h_i[-1])
    return h_r, h_i, final


def s5_readout(h_r, h_i, c_re, c_im):
    return jnp.einsum('lbgp,gcp->blgc', h_r, c_re) - jnp.einsum('lbgp,gcp->blgc', h_i, c_im)


def s5_mixer(uc, ul, lam_re, lam_im, log_step, b_re, b_im, c_re, c_im, d_skip, glu_w, glu_b, ctx_out):
    grp = lambda u: u.reshape(u.shape[:-1] + (S5_GROUPS, S5_GROUP))
    ucg, ulg = grp(uc), grp(ul)
    yc, yl = 0.0, 0.0
    for d, reverse in enumerate((False, True)):
        zr, zi, bb_r, bb_i = s5_discretise(lam_re[d], lam_im[d], log_step[d], b_re[d], b_im[d])
        hc_r, hc_i, final_c = s5_scan(ucg, zr, zi, bb_r, bb_i, None, reverse)
        hl_r, hl_i, _ = s5_scan(ulg, zr, zi, bb_r, bb_i, final_c, reverse)
        yl = yl + s5_readout(hl_r, hl_i, c_re[d], c_im[d])
        if ctx_out:
            yc = yc + s5_readout(hc_r, hc_i, c_re[d], c_im[d])

    def finish(y, u):
        y = jax.nn.gelu(y.reshape(u.shape) + d_skip * u)
        return y * jax.nn.sigmoid(y @ glu_w + glu_b)

    return (finish(yc, uc) if ctx_out else None), finish(yl, ul)


def hgrn_chunk(q, k, v, log_f, s0):
    B_, L, H, _ = q.shape
    n = L // HG_CHUNK
    rs = lambda t: t.reshape(B_, n, HG_CHUNK, H, t.shape[-1])
    q, k, v, log_f = rs(q), rs(k), rs(v), rs(log_f)
    b = jnp.cumsum(log_f, axis=2)
    b_last = b[:, :, -1:]
    causal = jnp.tril(jnp.ones((HG_CHUNK, HG_CHUNK), bool))
    diff = b[:, :, :, None] - b[:, :, None, :]
    rel = jnp.exp(jnp.where(causal[None, None, :, :, None, None], diff, MASK_VALUE))
    att = jnp.einsum('bnthd,bnshd,bntshd->bnhts', q, k, rel)
    o_intra = jnp.einsum('bnhts,bnshv->bnthv', att, v)
    q_in = q * jnp.exp(b)
    k_end = k * jnp.exp(b_last - b)
    decay = jnp.exp(b_last[:, :, 0])

    def step(S, inp):
        qi, ke, vv, dc = inp
        o = jnp.einsum('bthd,bhdv->bthv', qi, S)
        S = S * dc[..., None] + jnp.einsum('bshd,bshv->bhdv', ke, vv)
        return S, o

    xs = tuple(jnp.swapaxes(t, 0, 1) for t in (q_in, k_end, v, decay))
    s_final, o_inter = lax.scan(step, s0, xs)
    o = o_intra + jnp.swapaxes(o_inter, 0, 1)
    return o.reshape(B_, L, H, v.shape[-1]), s_final


def hgrn_mixer(zc, zl, lb, f_bias, norm_g, ctx_out):
    f32 = jnp.float32
    lb_h = lb.reshape(HG_HEADS, HG_KEY).astype(f32)
    log_lb = jnp.log(jnp.maximum(lb_h, LOG_FLOOR))
    log_one_minus_lb = jnp.log1p(-lb_h)

    def prep(z):
        q, f_f, f_b, i, g = split_cols(z, [GROUP_WIDTH] * 5)
        gates = []
        for d, f in enumerate((f_f, f_b)):
            zf = to_heads(f + f_bias[d], HG_HEADS).astype(f32)
            log_f = jnp.logaddexp(log_one_minus_lb + jax.nn.log_sigmoid(zf), log_lb)
            key = (1.0 - lb_h) * jax.nn.sigmoid(-zf)
            gates.append((key, log_f))
        return to_heads(q, HG_HEADS).astype(f32), to_heads(i, HG_HEADS).astype(f32), g, gates

    flip = lambda t: jnp.flip(t, axis=1)
    qc, ic, gc, gates_c = prep(zc)
    ql, il, gl, gates_l = prep(zl)
    s_zero = jnp.zeros((zl.shape[0], HG_HEADS, HG_KEY, HG_VAL), f32)
    oc_f, s_cf = hgrn_chunk(qc, gates_c[0][0], ic, gates_c[0][1], s_zero)
    ol_f, _ = hgrn_chunk(ql, gates_l[0][0], il, gates_l[0][1], s_cf)
    oc_b, s_cb = hgrn_chunk(flip(qc), flip(gates_c[1][0]), flip(ic), flip(gates_c[1][1]), s_zero)
    ol_b, _ = hgrn_chunk(flip(ql), flip(gates_l[1][0]), flip(il), flip(gates_l[1][1]), s_cb)

    def readout(o, g):
        o = rmsnorm(o, norm_g) * jax.nn.sigmoid(to_heads(g, HG_HEADS))
        return o.reshape(o.shape[:-2] + (GROUP_WIDTH,))

    out_l = readout(ol_f + flip(ol_b), gl)
    out_c = readout(oc_f + flip(oc_b), gc) if ctx_out else None
    return out_c, out_l


def apply_rope_2d(t, cos_r, sin_r, cos_c, sin_c):
    def rot(u, cos, sin):
        u1, u2 = jnp.split(u, 2, axis=-1)
        cos, sin = cos[:, None, :], sin[:, None, :]
        return jnp.concatenate([u1 * cos - u2 * sin, u2 * cos + u1 * sin], axis=-1)
    t_row, t_col = jnp.split(t, 2, axis=-1)
    return jnp.concatenate([rot(t_row, cos_r, sin_r), rot(t_col, cos_c, sin_c)], axis=-1)


def window_attention(q, k, v, kc, vc, sink):
    B_, L = q.shape[:2]
    nb = L // AT_BLOCK
    grp = AT_Q_HEADS // AT_KV_HEADS
    scale = AT_HEAD ** -0.5
    qb = q.reshape(B_, nb, AT_BLOCK, AT_KV_HEADS, grp, AT_HEAD)

    def band(t):
        tp = jnp.pad(t, ((0, 0), (AT_BLOCK, AT_BLOCK), (0, 0), (0, 0)))
        tp = tp.reshape(B_, nb + 2, AT_BLOCK, AT_KV_HEADS, AT_HEAD)
        return jnp.concatenate([tp[:, :-2], tp[:, 1:-1], tp[:, 2:]], axis=2)

    kb, vb = band(k), band(v)
    q_pos = jnp.arange(L).reshape(nb, AT_BLOCK)
    k_pos = (jnp.arange(nb)[:, None] - 1) * AT_BLOCK + jnp.arange(3 * AT_BLOCK)[None, :]
    valid = ((k_pos[:, None, :] >= 0) & (k_pos[:, None, :] < L)
             & (jnp.abs(q_pos[:, :, None] - k_pos[:, None, :]) <= AT_WINDOW))
    s_lat = jnp.einsum('bnqhgd,bnkhd->bnhgqk', qb, kb).astype(jnp.float32) * scale
    s_lat = jnp.where(valid[None, :, None, None], s_lat, MASK_VALUE)
    s_ctx = jnp.einsum('bnqhgd,bchd->bnhgqc', qb, kc).astype(jnp.float32) * scale
    s_sink = jnp.broadcast_to(sink.reshape(AT_KV_HEADS, grp, 1, 1).astype(jnp.float32),
                              s_lat.shape[:-1] + (1,))
    p = jax.nn.softmax(jnp.concatenate([s_lat, s_ctx, s_sink], axis=-1), axis=-1)
    n_lat, n_ctx = 3 * AT_BLOCK, kc.shape[1]
    o = (jnp.einsum('bnhgqk,bnkhd->bnqhgd', p[..., :n_lat], vb)
         + jnp.einsum('bnhgqc,bchd->bnqhgd', p[..., n_lat:n_lat + n_ctx], vc))
    return o.reshape(B_, L, AT_Q_HEADS * AT_HEAD)


def context_attention(qc, kc, vc, sink):
    B_, Lc = qc.shape[:2]
    grp = AT_Q_HEADS // AT_KV_HEADS
    q = qc.reshape(B_, Lc, AT_KV_HEADS, grp, AT_HEAD)
    s = jnp.einsum('bqhgd,bkhd->bhgqk', q, kc).astype(jnp.float32) * AT_HEAD ** -0.5
    s_sink = jnp.broadcast_to(sink.reshape(AT_KV_HEADS, grp, 1, 1).astype(jnp.float32), s.shape[:-1] + (1,))
    p = jax.nn.softmax(jnp.concatenate([s, s_sink], axis=-1), axis=-1)
    o = jnp.einsum('bhgqk,bkhd->bqhgd', p[..., :Lc], vc)
    return o.reshape(B_, Lc, AT_Q_HEADS * AT_HEAD)


def attn_mixer(zc, zl, q_norm, k_norm, sink, rope, ctx_out):
    def qkv(z):
        q, k, v = split_cols(z, [AT_Q_HEADS * AT_HEAD, AT_KV_HEADS * AT_HEAD, AT_KV_HEADS * AT_HEAD])
        return (rmsnorm(to_heads(q, AT_Q_HEADS), q_norm), rmsnorm(to_heads(k, AT_KV_HEADS), k_norm),
                to_heads(v, AT_KV_HEADS))
    qc, kc, vc = qkv(zc)
    ql, kl, vl = qkv(zl)
    ql, kl = apply_rope_2d(ql, *rope), apply_rope_2d(kl, *rope)
    out_l = window_attention(ql, kl, vl, kc, vc, sink)
    out_c = context_attention(qc, kc, vc, sink) if ctx_out else None
    return out_c, out_l


def moe_ffn(h, router_w, router_b, w_gu, b_gu, w_down, b_down):
    n_tok, d = h.shape
    n_assign = n_tok * TOP_K
    logits = (h @ router_w + router_b).astype(jnp.float32)
    top_val, top_idx = lax.top_k(logits, TOP_K)
    gates = jax.nn.softmax(top_val, axis=-1).reshape(-1)
    flat_e = top_idx.reshape(-1)
    order = jnp.argsort(flat_e)
    sorted_e = flat_e[order]
    token = order // TOP_K
    counts = jnp.bincount(flat_e, length=N_EXPERTS)
    padded = (counts + MOE_BLOCK - 1) // MOE_BLOCK * MOE_BLOCK
    start = jnp.cumsum(counts) - counts
    pad_end = jnp.cumsum(padded)
    pad_start = pad_end - padded
    dest = pad_start[sorted_e] + jnp.arange(n_assign) - start[sorted_e]
    n_rows = -(-n_assign // MOE_BLOCK) * MOE_BLOCK + N_EXPERTS * MOE_BLOCK
    n_blocks = n_rows // MOE_BLOCK
    rows = jnp.zeros((n_rows, d), h.dtype).at[dest].set(h[token])
    block_expert = jnp.minimum(
        jnp.searchsorted(pad_end, jnp.arange(n_blocks) * MOE_BLOCK, side='right'), N_EXPERTS - 1)

    def expert_block(args):
        xb, e = args
        gu = xb @ w_gu[e] + b_gu[e]
        gate, up = gu[:, 0::2], gu[:, 1::2]
        gate = jnp.minimum(gate, SWIGLU_LIMIT)
        up = jnp.clip(up, -SWIGLU_LIMIT, SWIGLU_LIMIT)
        return ((up + 1.0) * gate * jax.nn.sigmoid(SWIGLU_ALPHA * gate)) @ w_down[e] + b_down[e]

    out = lax.map(expert_block, (rows.reshape(n_blocks, MOE_BLOCK, d), block_expert)).reshape(n_rows, -1)
    y = out[dest] * gates[order][:, None]
    return jnp.zeros((n_tok, y.shape[-1]), y.dtype).at[token].add(y)


def trunk_layer(x, xc, c, c_ctx, p, lb, rope, ctx_out):
    mod = (jax.nn.silu(c) @ p['w_mod'] + p['b_mod'])[:, None, :]
    mod_c = (jax.nn.silu(c_ctx) @ p['w_mod'] + p['b_mod'])[None, None, :]
    sh1, sc1, g1, sh2, sc2, g2 = jnp.split(mod, 6, axis=-1)
    csh1, csc1, cg1, csh2, csc2, cg2 = jnp.split(mod_c, 6, axis=-1)

    h = rmsnorm(x, p['norm1']) * (1.0 + sc1) + sh1
    hc = rmsnorm(xc, p['norm1']) * (1.0 + csc1) + csh1
    z = h @ p['w_in']
    zc = hc @ p['w_in']
    widths = [RW_COLS, S5_COLS, HG_COLS, AT_COLS]
    z_rw, z_s5, z_hg, z_at = split_cols(z, widths)
    zc_rw, zc_s5, zc_hg, zc_at = split_cols(zc, widths)

    outs = (rwkv_mixer(zc_rw, z_rw, *p['rw'], ctx_out),
            s5_mixer(zc_s5, z_s5, *p['s5'], ctx_out),
            hgrn_mixer(zc_hg, z_hg, lb, *p['hg'], ctx_out),
            attn_mixer(zc_at, z_at, *p['at'], rope, ctx_out))
    y = jnp.concatenate([o[1] for o in outs], axis=-1) @ p['w_out']
    x = x + g1 * y
    h2 = rmsnorm(x, p['norm2']) * (1.0 + sc2) + sh2
    B_, L, D = x.shape
    if ctx_out:
        yc = jnp.concatenate([o[0] for o in outs], axis=-1) @ p['w_out']
        xc = xc + cg1 * yc
        h2c = rmsnorm(xc, p['norm2']) * (1.0 + csc2) + csh2
        tokens = jnp.concatenate([h2.reshape(-1, D), h2c.reshape(-1, D)], axis=0)
        f = moe_ffn(tokens, *p['moe'])
        x = x + g2 * f[:B_ * L].reshape(x.shape)
        xc = xc + cg2 * f[B_ * L:].reshape(xc.shape)
    else:
        x = x + g2 * moe_ffn(h2.reshape(-1, D), *p['moe']).reshape(x.shape)
    return x, xc


def setup_inputs(seed: int = 0) -> dict:
    key = jax.random.key(seed)
    keys = iter(jax.random.split(key, 64))
    f32 = jnp.float32
    D, GW = D_MODEL, GROUP_WIDTH

    def nrm(shape, scale=1.0):
        return scale * jax.random.normal(next(keys), shape, f32)

    def uni(shape, lo, hi):
        return jax.random.uniform(next(keys), shape, f32, lo, hi)

    def gain(shape):
        return 1.0 + nrm(shape, 0.02)

    s5_shape = (DEPTH, 2, S5_GROUPS, S5_STATE)
    return {
        'x': nrm((BATCH, SEQ, D)),
        'c': nrm((BATCH, D)),
        'ctx': nrm((BATCH, CTX_LEN, D)),
        'c_ctx': nrm((D,)),
        'norm1_g': gain((DEPTH, D)),
        'norm2_g': gain((DEPTH, D)),
        'w_mod': nrm((DEPTH, D, 6 * D), 0.5 * D ** -0.5),
        'b_mod': nrm((DEPTH, 6 * D), 0.01),
        'w_in': nrm((DEPTH, D, IN_COLS), D ** -0.5),
        'w_out': nrm((DEPTH, MIX_WIDTH, D), MIX_WIDTH ** -0.5),
        'rw_mu': uni((DEPTH, 2, RW_COLS), 0.0, 0.5),
        'rw_w0': uni((DEPTH, 2, GW), -6.0, 0.0),
        'rw_w2': nrm((DEPTH, 2, RW_DECAY_RANK, GW), 0.1 * RW_DECAY_RANK ** -0.5),
        'rw_a0': nrm((DEPTH, 2, GW), 0.1),
        'rw_a2': nrm((DEPTH, 2, RW_ICLR_RANK, GW), 0.1 * RW_ICLR_RANK ** -0.5),
        'rw_g2': nrm((DEPTH, RW_GATE_RANK, GW), RW_GATE_RANK ** -0.5),
        'rw_k_k': 0.85 + nrm((DEPTH, GW), 0.02),
        'rw_k_a': gain((DEPTH, GW)),
        'rw_r_k': nrm((DEPTH, RW_HEADS, RW_HEAD), 0.1),
        'rw_ln_w': gain((DEPTH, GW)),
        'rw_ln_b': nrm((DEPTH, GW), 0.01),
        's5_lam_re': -0.5 + nrm(s5_shape, 0.01),
        's5_lam_im': jnp.pi * jnp.arange(S5_STATE, dtype=f32) + nrm(s5_shape, 0.01),
        's5_log_step': uni((DEPTH, 2, S5_GROUPS), math.log(S5_DT_MIN), math.log(S5_DT_MAX)),
        's5_b_re': nrm(s5_shape + (S5_GROUP,), (2 * S5_GROUP) ** -0.5),
        's5_b_im': nrm(s5_shape + (S5_GROUP,), (2 * S5_GROUP) ** -0.5),
        's5_c_re': nrm((DEPTH, 2, S5_GROUPS, S5_GROUP, S5_STATE), 0.5),
        's5_c_im': nrm((DEPTH, 2, S5_GROUPS, S5_GROUP, S5_STATE), 0.5),
        's5_d': nrm((DEPTH, GW), 0.5),
        's5_glu_w': nrm((DEPTH, GW, GW), GW ** -0.5),
        's5_glu_b': nrm((DEPTH, GW), 0.01),
        'hg_f_bias': 1.0 + nrm((DEPTH, 2, GW), 0.1),
        'hg_lb_logits': nrm((DEPTH, GW), 0.1),
        'hg_norm_g': gain((DEPTH, HG_VAL)),
        'at_q_norm': gain((DEPTH, AT_HEAD)),
        'at_k_norm': gain((DEPTH, AT_HEAD)),
        'at_sink': nrm((DEPTH, AT_Q_HEADS), 0.5),
        'moe_router_w': nrm((DEPTH, D, N_EXPERTS), D ** -0.5),
        'moe_router_b': nrm((DEPTH, N_EXPERTS), 0.01),
        'moe_w_gu': nrm((DEPTH, N_EXPERTS, D, 2 * D_EXPERT), D ** -0.5),
        'moe_b_gu': nrm((DEPTH, N_EXPERTS, 2 * D_EXPERT), 0.01),
        'moe_w_down': nrm((DEPTH, N_EXPERTS, D_EXPERT, D), D_EXPERT ** -0.5),
        'moe_b_down': nrm((DEPTH, N_EXPERTS, D), 0.01),
    }


def reference(x, c, ctx, c_ctx, norm1_g, norm2_g, w_mod, b_mod, w_in, w_out,
              rw_mu, rw_w0, rw_w2, rw_a0, rw_a2, rw_g2, rw_k_k, rw_k_a, rw_r_k, rw_ln_w, rw_ln_b,
              s5_lam_re, s5_lam_im, s5_log_step, s5_b_re, s5_b_im, s5_c_re, s5_c_im, s5_d,
              s5_glu_w, s5_glu_b, hg_f_bias, hg_lb_logits, hg_norm_g,
              at_q_norm, at_k_norm, at_sink,
              moe_router_w, moe_router_b, moe_w_gu, moe_b_gu, moe_w_down, moe_b_down):
    f32 = jnp.float32
    L = x.shape[1]
    rows = L // GRID_W
    row = jnp.repeat(jnp.arange(rows), GRID_W).astype(f32)
    col = (jnp.arange(rows * GRID_W) % GRID_W).astype(f32)
    n_freq = AT_HEAD // 4
    inv_freq = ROPE_BASE ** (-jnp.arange(n_freq, dtype=f32) / n_freq)
    ang_r, ang_c = row[:, None] * inv_freq, col[:, None] * inv_freq
    rope = (jnp.cos(ang_r), jnp.sin(ang_r), jnp.cos(ang_c), jnp.sin(ang_c))

    lb_p = jax.nn.softmax(hg_lb_logits.astype(f32), axis=0)
    lower_bounds = jnp.cumsum(lb_p, axis=0) - lb_p[0]

    xc = ctx
    for l in range(DEPTH):
        p = dict(
            norm1=norm1_g[l], norm2=norm2_g[l], w_mod=w_mod[l], b_mod=b_mod[l],
            w_in=w_in[l], w_out=w_out[l],
            rw=(rw_mu[l], rw_w0[l], rw_w2[l], rw_a0[l], rw_a2[l], rw_g2[l], rw_k_k[l], rw_k_a[l],
                rw_r_k[l], rw_ln_w[l], rw_ln_b[l]),
            s5=(s5_lam_re[l], s5_lam_im[l], s5_log_step[l], s5_b_re[l], s5_b_im[l], s5_c_re[l],
                s5_c_im[l], s5_d[l], s5_glu_w[l], s5_glu_b[l]),
            hg=(hg_f_bias[l], hg_norm_g[l]),
            at=(at_q_norm[l], at_k_norm[l], at_sink[l]),
            moe=(moe_router_w[l], moe_router_b[l], moe_w_gu[l], moe_b_gu[l], moe_w_down[l], moe_b_down[l]),
        )
        x, xc = trunk_layer(x, xc, c, c_ctx, p, lower_bounds[l], rope, l < DEPTH - 1)
    return x
```

```python
import math
from contextlib import ExitStack

import numpy as np
import concourse.bass as bass
import concourse.mybir as mybir
from concourse.bass_utils import run_bass_kernel_spmd

F32 = mybir.dt.float32
BF16 = mybir.dt.bfloat16
AF = mybir.ActivationFunctionType
ALU = mybir.AluOpType
AX = mybir.AxisListType


class Cfg:
    NB = 2
    LC = 256
    LL = 2048
    D = 2048
    DEPTH = 2
    NE = 32
    NCORES = 8
    GRID_W = 64
    debug = ()
    phases = ("attn", "s5", "hg", "rw", "moe")

    @property
    def T(self):
        return self.LC + self.LL


GW = 512
RW_COLS = 1920
S5_COLS = 512
HG_COLS = 2560
AT_COLS = 768
IN_COLS = RW_COLS + S5_COLS + HG_COLS + AT_COLS
C_RW = 0
C_S5 = RW_COLS
C_HG = C_S5 + S5_COLS
C_AT = C_HG + HG_COLS
NORM_EPS = 1e-6


class Dom:
    def __init__(self, name, sem, mult):
        self.name, self.sem, self.mult, self.count = name, sem, mult, 0


class Buf:
    __slots__ = ("w", "r", "name")

    def __init__(self, name=""):
        self.w = None
        self.r = {}
        self.name = name


class K:
    def __init__(self, nc, stack, n_lanes=40):
        self.nc = nc
        self.eng = {"pe": nc.tensor, "act": nc.scalar, "dve": nc.vector,
                    "pool": nc.gpsimd, "sp": nc.sync}
        self.dom = {n: Dom(n, stack.enter_context(nc.semaphore("s_" + n)), 1)
                    for n in self.eng}
        self.lanes = [Dom("l%d" % i, stack.enter_context(nc.semaphore("l%d" % i)), 16)
                      for i in range(n_lanes)]
        self.waited = {n: {} for n in self.eng}
        self.rr = 0
        self.qrr = 0
        self.ninst = 0

    def _wait(self, e, d, c):
        if c <= 0:
            return
        if self.waited[e].get(d.name, 0) >= c:
            return
        self.eng[e].wait_ge(d.sem, c * d.mult)
        self.waited[e][d.name] = c

    def _deps(self, e, r, w, extra=()):
        need = {}

        def add(dc):
            d, c = dc
            if need.get(d, 0) < c:
                need[d] = c
        for b in r:
            if b.w is not None:
                add(b.w)
        for b in w:
            if b.w is not None:
                add(b.w)
            for d, c in b.r.items():
                add((d, c))
        for dc in extra:
            add(dc)
        for d, c in need.items():
            if e == "pe" and d.name == "pe":
                continue
            self._wait(e, d, c)

    def op(self, e, fn, r=(), w=()):
        self._deps(e, r, w)
        ins = fn(self.eng[e])
        d = self.dom[e]
        d.count += 1
        ins.then_inc(d.sem, 1)
        for b in r:
            b.r[d] = d.count
        for b in w:
            b.w = (d, d.count)
            b.r = {}
        self.ninst += 1
        return ins

    def dma(self, q, out, in_, r=(), w=(), **kw):
        if q is None:
            q = ("sp", "act")[self.qrr % 2]
            self.qrr += 1
        lane = self.lanes[self.rr]
        self.rr = (self.rr + 1) % len(self.lanes)
        self._deps(q, r, w, extra=[(lane, lane.count)])
        ins = self.eng[q].dma_start(out=out, in_=in_, **kw)
        lane.count += 1
        ins.then_inc(lane.sem, 16)
        for b in r:
            b.r[lane] = lane.count
        for b in w:
            b.w = (lane, lane.count)
            b.r = {}
        self.ninst += 1
        return ins

    def barrier(self, engines=("pe", "act", "dve", "pool", "sp")):
        doms = list(self.dom.values()) + self.lanes
        for e in engines:
            for d in doms:
                if d.name == e:
                    continue
                self._wait(e, d, d.count)

    def finish(self):
        self.barrier(engines=("sp", "pool", "act", "dve", "pe"))


def _chunks(total, size):
    out = []
    s = 0
    while s < total:
        out.append((s, min(size, total - s)))
        s += size
    return out


class Prog:
    def __init__(self, cfg):
        self.cfg = cfg
        self.nc = bass.Bass("TRN2", target_bir_lowering=False)
        self.stack = ExitStack()
        self.k = None
        self.din = {}
        self.dbg = {}

    def inp(self, name, shape):
        t = self.nc.dram_tensor(name, list(shape), F32, kind="ExternalInput").ap()
        self.din[name] = t
        return t

    def scratch(self, name, shape, dt=F32):
        return self.nc.dram_tensor(name, list(shape), dt).ap()

    def out(self, name, shape, dt=F32):
        return self.nc.dram_tensor(name, list(shape), dt, kind="ExternalOutput").ap()

    def sb(self, st, name, shape, dt=F32):
        self._uid = getattr(self, "_uid", 0) + 1
        return st.enter_context(self.nc.sbuf_tensor("%s_%d" % (name, self._uid), list(shape), dt))

    def build(self):
        cfg, nc = self.cfg, self.nc
        NB, T, D, DEPTH, NE = cfg.NB, cfg.T, cfg.D, cfg.DEPTH, cfg.NE
        R = NB + 1
        st = self.stack
        st.enter_context(nc.allow_non_contiguous_dma(reason="layout"))
        st.enter_context(nc.allow_low_precision(reason="bf16 matmul operands"))
        self.k = k = K(nc, st)
        self.xin = self.inp("xin", [NB, T, D])
        self.cvec = self.inp("cvec", [R, D])
        self.norm1 = self.inp("norm1_g", [DEPTH, D])
        self.norm2 = self.inp("norm2_g", [DEPTH, D])
        self.w_mod = self.inp("w_mod", [DEPTH, D, 6 * D])
        self.b_mod = self.inp("b_mod", [DEPTH, 6 * D])
        self.w_in = self.inp("w_in", [DEPTH, D, IN_COLS])
        self.ident_d = self.inp("ident", [128, 128])
        self.inp_at_q = self.inp("at_q_norm", [DEPTH, 64])
        self.inp_at_k = self.inp("at_k_norm", [DEPTH, 64])
        self.inp_at_sink = self.inp("at_sink", [DEPTH, 8])
        self.inp_s5_lre = self.inp("s5_lam_re", [DEPTH, 2, 32, 64])
        self.inp_s5_lim = self.inp("s5_lam_im", [DEPTH, 2, 32, 64])
        self.inp_s5_ls = self.inp("s5_log_step", [DEPTH, 2, 32])
        self.inp_s5_bre = self.inp("s5_b_re", [DEPTH, 2, 32, 64, 16])
        self.inp_s5_bim = self.inp("s5_b_im", [DEPTH, 2, 32, 64, 16])
        self.inp_s5_cre = self.inp("s5_c_re", [DEPTH, 2, 32, 16, 64])
        self.inp_s5_cim = self.inp("s5_c_im", [DEPTH, 2, 32, 16, 64])
        self.inp_s5_d = self.inp("s5_d", [DEPTH, 512])
        self.inp_s5_glu_w = self.inp("s5_glu_w", [DEPTH, 512, 512])
        self.inp_s5_glu_b = self.inp("s5_glu_b", [DEPTH, 512])
        self.inp_hg_fb = self.inp("hg_f_bias", [DEPTH, 2, 512])
        self.inp_hg_lbl = self.inp("hg_lb_logits", [DEPTH, 512])
        self.inp_hg_ng = self.inp("hg_norm_g", [DEPTH, 128])
        self.inp_rw_mu = self.inp("rw_mu", [DEPTH, 2, 1920])
        self.inp_rw_w0 = self.inp("rw_w0", [DEPTH, 2, 512])
        self.inp_rw_w2 = self.inp("rw_w2", [DEPTH, 2, 64, 512])
        self.inp_rw_a0 = self.inp("rw_a0", [DEPTH, 2, 512])
        self.inp_rw_a2 = self.inp("rw_a2", [DEPTH, 2, 64, 512])
        self.inp_rw_g2 = self.inp("rw_g2", [DEPTH, 128, 512])
        self.inp_rw_kk = self.inp("rw_k_k", [DEPTH, 512])
        self.inp_rw_ka = self.inp("rw_k_a", [DEPTH, 512])
        self.inp_rw_rk = self.inp("rw_r_k", [DEPTH, 512])
        self.inp_rw_lnw = self.inp("rw_ln_w", [DEPTH, 512])
        self.inp_rw_lnb = self.inp("rw_ln_b", [DEPTH, 512])
        self.c_ind2 = self.inp("c_ind2", [2, 128])
        self.c_antiI = self.inp("c_antiI", [128, 128])
        self.RWP = self.scratch("RWP", [5, NB * 8, 128, T])
        self.VTK = self.scratch("VTK", [NB, T, 2, 4, 64], BF16)
        self.YR = self.scratch("YR", [T, 2, NB * 8 * 64])
        self.inp_w_out = self.inp("w_out", [DEPTH, D, D])
        self.inp_rw_ = self.inp("moe_router_w", [DEPTH, D, NE])
        self.inp_rb_ = self.inp("moe_router_b", [DEPTH, NE])
        self.inp_wgu = self.inp("moe_w_gu", [DEPTH, NE, D, 2 * D])
        self.inp_bgu = self.inp("moe_b_gu", [DEPTH, NE, 2 * D])
        self.inp_wd = self.inp("moe_w_down", [DEPTH, NE, D, D])
        self.inp_bd = self.inp("moe_b_down", [DEPTH, NE, D])
        self.MODROW = self.scratch("MODROW", [DEPTH, R, 6 * D])
        self.b_MODROW = Buf("MODROW")
        self.yout = self.out("y", [NB, cfg.LL, D])
        self.b_yout = Buf("yout")
        self.c_cos = self.inp("c_cos", [64, cfg.LL])
        self.c_sin = self.inp("c_sin", [64, cfg.LL])
        self.c_rot = self.inp("c_rot", [64, 64])
        self.c_mprev = self.inp("c_mprev", [128, 128])
        self.c_mnext = self.inp("c_mnext", [128, 128])
        self.ident = self.sb(st, "ident_sb", [128, 128], F32)
        self.b_ident = Buf("ident")
        k.dma("sp", self.ident[:], self.ident_d[:, :], w=[self.b_ident])
        self.ps = [st.enter_context(nc.psum_tensor("psb%d" % i, [128, 512], F32)) for i in range(8)]
        self.b_ps = [Buf("ps%d" % i) for i in range(8)]
        self.XT = self.scratch("XT", [NB, T, D])
        self.ZT = self.scratch("ZT", [NB, IN_COLS, T])
        self.b_XT = [Buf("XT%d" % b) for b in range(NB)]
        self.b_ZT = [Buf("ZT%d" % b) for b in range(NB)]
        if "ot" in cfg.debug:
            self.OT = self.out("dbg_ot", [NB, D, T], BF16)
        else:
            self.OT = self.scratch("OT", [NB, D, T], BF16)
        self.b_OT = [Buf("OT%d" % b) for b in range(NB)]
        if "zt" in cfg.debug:
            self.dbg["zt"] = self.out("dbg_zt", [NB, IN_COLS, T])
        if "mod" in cfg.debug:
            self.dbg["mod"] = self.out("dbg_mod", [128, 96 * R])

        for l in range(DEPTH):
            self.layer(l)
            if cfg.debug and "full" not in cfg.debug:
                break
        k.finish()
        return nc

    def layer(self, l):
        cfg, nc, k = self.cfg, self.nc, self.k
        NB = cfg.NB
        ctx_out = l < cfg.DEPTH - 1 or (bool(cfg.debug) and "full" not in cfg.debug)
        with ExitStack() as lst:
            self.phase_mod(l, lst)
            for b in range(NB):
                with ExitStack() as bst:
                    src = self.xin if l == 0 else self.XT
                    self.phase_norm_T(l, b, bst, src, which=1)
                    self.phase_inproj(l, b, bst)
                    k.barrier()
            if "attn" in cfg.phases:
                for b in range(NB):
                    self.phase_attn(l, b, ctx_out)
            if "s5" in cfg.phases:
                with ExitStack() as st2:
                    self.phase_s5_prep(l, st2)
                    for b in range(NB):
                        self.phase_s5(l, b)
                    k.barrier()
            if "rw" in cfg.phases:
                with ExitStack() as st2:
                    self.phase_rw(l, st2)
                    k.barrier()
            if "hg" in cfg.phases:
                with ExitStack() as st2:
                    self.phase_hg_prep(l, st2)
                    for b in range(NB):
                        self.phase_hg(l, b)
                    k.barrier()
            k.barrier()
            if "moe" in cfg.phases:
                ctx_o = l < cfg.DEPTH - 1
                with ExitStack() as st2:
                    self.phase_outproj(l, st2, ctx_o)
                    k.barrier()
                with ExitStack() as st2:
                    self.phase_moe(l, st2, ctx_o, l == cfg.DEPTH - 1)
                    k.barrier()

    def phase_mod(self, l, lst):
        cfg, nc, k = self.cfg, self.nc, self.k
        NB, D = cfg.NB, cfg.D
        R = NB + 1
        KT = D // 128
        self.modT = self.sb(lst, "modT", [128, 6 * KT, R], F32)
        self.sc1e = self.sb(lst, "sc1e", [128, KT, R], F32)
        self.sc2e = self.sb(lst, "sc2e", [128, KT, R], F32)
        self.b_mod_sb = Buf("modT")
        b_modT = self.b_mod_sb
        with ExitStack() as ph:
            cT = self.sb(ph, "cT", [128, KT, R], F32)
            bm = self.sb(ph, "bm", [128, 6 * KT], F32)
            g1 = self.sb(ph, "g1", [128, KT], F32)
            g2 = self.sb(ph, "g2", [128, KT], F32)
            wm = [self.sb(ph, "wm%d" % i, [128, KT, 512], F32) for i in range(2)]
            b_cT, b_bm, b_g = Buf(), Buf(), Buf()
            b_wm = [Buf(), Buf()]
            for r in range(R):
                k.dma("sp", cT[:, :, r], self.cvec[r].rearrange("(k p) -> p k", p=128), w=[b_cT])
            k.dma("sp", bm[:], self.b_mod[l].rearrange("(j p) -> p j", p=128), w=[b_bm])
            k.dma("sp", g1[:], self.norm1[l].rearrange("(j p) -> p j", p=128), w=[b_g])
            k.dma("sp", g2[:], self.norm2[l].rearrange("(j p) -> p j", p=128), w=[b_g])
            k.op("act", lambda e: e.activation(out=cT[:], in_=cT[:], func=AF.Silu), r=[b_cT], w=[b_cT])
            wsrc = self.w_mod[l].rearrange("(k p) c -> p k c", p=128)
            nch = 6 * D // 512
            for ch in range(nch):
                wb, bwb = wm[ch % 2], b_wm[ch % 2]
                k.dma(None, wb[:], wsrc[:, :, ch * 512:(ch + 1) * 512], w=[bwb])
                pb, bpb = self.ps[ch % 2], self.b_ps[ch % 2]
                for j in range(4):
                    for kt in range(KT):
                        k.op("pe", lambda e, j=j, kt=kt: e.matmul(
                            pb[:, j * R:(j + 1) * R], wb[:, kt, j * 128:(j + 1) * 128], cT[:, kt, :],
                            start=(kt == 0), stop=(kt == KT - 1)), r=[bwb, b_cT], w=[bpb])
                k.op("dve", lambda e, ch=ch: e.tensor_tensor(
                    out=self.modT[:, ch * 4:(ch + 1) * 4, :],
                    in0=pb[:, 0:4 * R].rearrange("p (j r) -> p j r", r=R),
                    in1=bm[:, ch * 4:(ch + 1) * 4].unsqueeze(2).to_broadcast([128, 4, R]),
                    op=ALU.add), r=[bpb, b_bm], w=[b_modT])
            for (dst, g, off) in ((self.sc1e, g1, KT), (self.sc2e, g2, 4 * KT)):
                k.op("dve", lambda e, dst=dst, g=g, off=off: e.scalar_tensor_tensor(
                    out=dst[:], in0=self.modT[:, off:off + KT, :], scalar=1.0,
                    in1=g[:].unsqueeze(2).to_broadcast([128, KT, R]),
                    op0=ALU.add, op1=ALU.mult), r=[b_modT, b_g], w=[b_modT])
            if "mod" in cfg.debug:
                k.dma("sp", self.dbg["mod"][:, :], self.modT[:].rearrange("p j r -> p (j r)"), r=[b_modT])
            k.barrier()

    def phase_norm_T(self, l, b, bst, src, which):
        cfg = self.cfg
        NB, D, T, LC = cfg.NB, cfg.D, cfg.T, cfg.LC
        KT = D // 128
        self.hT = self.sb(bst, "hT", [128, KT, T], BF16)
        self.b_hT = Buf("hT")
        groups = [(src[b, t0:t0 + n, :], n, NB, t0, [self.b_XT[b]]) for (t0, n) in _chunks(LC, 512)] + \
                 [(src[b, LC + t0:LC + t0 + n, :], n, b, LC + t0, [self.b_XT[b]]) for (t0, n) in _chunks(T - LC, 512)]
        self.norm_groups(self.hT, self.b_hT, groups, which)

    def norm_groups(self, dst, b_dst, groups, which, ntmax=4):
        cfg, nc, k = self.cfg, self.nc, self.k
        D = cfg.D
        KT = D // 128
        sce = self.sc1e if which == 1 else self.sc2e
        sh_off = 0 if which == 1 else 3 * KT
        with ExitStack() as ph:
            xt = [self.sb(ph, "xt%d" % i, [128, ntmax, D], F32) for i in range(2)]
            junk = self.sb(ph, "junk", [128, D], BF16)
            ss = self.sb(ph, "ss", [128, 8], F32)
            b_xt = [Buf(), Buf()]
            b_junk, b_ss = Buf(), Buf()
            ev = 0
            for gi, (src_ap, ntok, r, t0, rb) in enumerate(groups):
                nt = ntok // 128
                x, bx = xt[gi % 2], b_xt[gi % 2]
                k.dma(None, x[:, 0:nt, :], src_ap.rearrange("(n p) d -> p n d", p=128), r=rb, w=[bx])
                so = (gi % 2) * 4
                for n in range(nt):
                    k.op("act", lambda e, n=n: e.activation(
                        out=junk[:], in_=x[:, n, :], func=AF.Square, accum_out=ss[:, so + n:so + n + 1]),
                        r=[bx], w=[b_junk, b_ss])
                k.op("dve", lambda e: e.tensor_scalar(
                    out=ss[:, so:so + nt], in0=ss[:, so:so + nt], scalar1=1.0 / D, scalar2=NORM_EPS,
                    op0=ALU.mult, op1=ALU.add), r=[b_ss], w=[b_ss])
                k.op("act", lambda e: e.activation(
                    out=ss[:, so:so + nt], in_=ss[:, so:so + nt], func=AF.Sqrt), r=[b_ss], w=[b_ss])
                k.op("dve", lambda e: e.reciprocal(
                    out=ss[:, so:so + nt], in_=ss[:, so:so + nt]), r=[b_ss], w=[b_ss])
                for n in range(nt):
                    k.op("pool", lambda e, n=n: e.tensor_scalar(
                        out=x[:, n, :], in0=x[:, n, :], scalar1=ss[:, so + n:so + n + 1], scalar2=None,
                        op0=ALU.mult), r=[bx, b_ss], w=[bx])
                for kt in range(KT):
                    pi = kt % 4
                    pb, bpb = self.ps[pi], self.b_ps[pi]
                    for n in range(nt):
                        k.op("pe", lambda e, n=n, kt=kt: e.transpose(
                            out=pb[:, n * 128:(n + 1) * 128], in_=x[:, n, kt * 128:(kt + 1) * 128],
                            identity=self.ident[:]), r=[bx, self.b_ident], w=[bpb])
                    if ev % 2 == 0:
                        k.op("dve", lambda e, kt=kt: e.tensor_scalar(
                            out=dst[:, kt, t0:t0 + ntok], in0=pb[:, 0:ntok],
                            scalar1=sce[:, kt, r:r + 1], scalar2=self.modT[:, sh_off + kt, r:r + 1],
                            op0=ALU.mult, op1=ALU.add), r=[bpb, self.b_mod_sb], w=[b_dst])
                    else:
                        k.op("act", lambda e, kt=kt: e.activation(
                            out=dst[:, kt, t0:t0 + ntok], in_=pb[:, 0:ntok], func=AF.Identity,
                            scale=sce[:, kt, r:r + 1], bias=self.modT[:, sh_off + kt, r:r + 1]),
                            r=[bpb, self.b_mod_sb], w=[b_dst])
                    ev += 1
            k.barrier()

    def phase_inproj(self, l, b, bst):
        cfg, nc, k = self.cfg, self.nc, self.k
        D, T = cfg.D, cfg.T
        KT = D // 128
        with ExitStack() as ph:
            wb = [self.sb(ph, "wi%d" % i, [128, KT, 512], BF16) for i in range(2)]
            zb = [self.sb(ph, "zb%d" % i, [128, T], F32) for i in range(2)]
            b_wb = [Buf(), Buf()]
            b_zb = [Buf(), Buf()]
            wsrc = self.w_in[l].rearrange("(k p) c -> p k c", p=128)
            zi = 0
            ev = 0
            for ci, (c0, cw) in enumerate(_chunks(IN_COLS, 512)):
                w, bw = wb[ci % 2], b_wb[ci % 2]
                k.dma("pool", w[:, :, 0:cw], wsrc[:, :, c0:c0 + cw], w=[bw])
                for ct in range(cw // 128):
                    z, bz = zb[zi % 2], b_zb[zi % 2]
                    zi += 1
                    for (t0, tn) in _chunks(T, 512):
                        pi = 4 + ev % 4
                        pb, bpb = self.ps[pi], self.b_ps[pi]
                        for kt in range(KT):
                            k.op("pe", lambda e, kt=kt: e.matmul(
                                pb[:, 0:tn], w[:, kt, ct * 128:(ct + 1) * 128], self.hT[:, kt, t0:t0 + tn],
                                start=(kt == 0), stop=(kt == KT - 1)), r=[bw, self.b_hT], w=[bpb])
                        if ev % 2 == 0:
                            k.op("dve", lambda e: e.tensor_copy(out=z[:, t0:t0 + tn], in_=pb[:, 0:tn]),
                                 r=[bpb], w=[bz])
                        else:
                            k.op("act", lambda e: e.activation(out=z[:, t0:t0 + tn], in_=pb[:, 0:tn],
                                                               func=AF.Copy), r=[bpb], w=[bz])
                        ev += 1
                    cc = c0 + ct * 128
                    k.dma(None, self.ZT[b, cc:cc + 128, :], z[:, :], r=[bz], w=[self.b_ZT[b]])
                    if "zt" in cfg.debug:
                        k.dma(None, self.dbg["zt"][b, cc:cc + 128, :], z[:, :], r=[bz])
            k.barrier()


    def copy(self, eng, out, in_, r=(), w=()):
        if eng == "act":
            return self.k.op("act", lambda e: e.activation(out=out, in_=in_, func=AF.Copy), r=r, w=w)
        return self.k.op(eng, lambda e: e.tensor_copy(out=out, in_=in_), r=r, w=w)

    def load_col(self, st, name, src_ap, n, bufs):
        t = self.sb(st, name, [n, 1], F32)
        self.k.dma("sp", t[:, :], src_ap.rearrange("(p o) -> p o", o=1), w=bufs)
        return t

    def phase_attn(self, l, b, ctx_out):
        cfg, nc, k = self.cfg, self.nc, self.k
        T, LC, LL = cfg.T, cfg.LC, cfg.LL
        NBLK, NCB = T // 128, LC // 128
        with ExitStack() as ph:
            b_c = Buf("attn_consts")
            qg = self.load_col(ph, "qgain", self.inp_at_q[l], 64, [b_c])
            kg = self.load_col(ph, "kgain", self.inp_at_k[l], 64, [b_c])
            esink = self.sb(ph, "esink", [128, 8], F32)
            k.dma("sp", esink[:, :], self.inp_at_sink[l].partition_broadcast(128), w=[b_c])
            k.op("act", lambda e: e.activation(out=esink[:], in_=esink[:], func=AF.Exp), r=[b_c], w=[b_c])
            cosT = self.sb(ph, "cosT", [64, LL], F32)
            sinT = self.sb(ph, "sinT", [64, LL], F32)
            k.dma("sp", cosT[:, :], self.c_cos[:, :], w=[b_c])
            k.dma("act", sinT[:, :], self.c_sin[:, :], w=[b_c])
            rotT = self.sb(ph, "rotT", [64, 64], F32)
            k.dma("sp", rotT[:, :], self.c_rot[:, :], w=[b_c])
            ones64 = self.sb(ph, "ones64", [64, 64], F32)
            k.op("pool", lambda e: e.memset(ones64[:], 1.0), w=[b_c])
            onesb = self.sb(ph, "onesb", [128, 128], BF16)
            k.op("pool", lambda e: e.memset(onesb[:], 1.0), w=[b_c])
            mprev = self.sb(ph, "mprev", [128, 128], BF16)
            mnext = self.sb(ph, "mnext", [128, 128], BF16)
            k.dma("pool", mprev[:, :], self.c_mprev[:, :], w=[b_c])
            k.dma("pool", mnext[:, :], self.c_mnext[:, :], w=[b_c])
            kf = [self.sb(ph, "kf%d" % j, [64, T], BF16) for j in range(2)]
            Qg = [self.sb(ph, "Qg%d" % j, [64, NBLK, 4, 128], BF16) for j in range(2)]
            Vt = self.sb(ph, "Vt", [128, NBLK, 2, 128], BF16)
            Ob = self.sb(ph, "Ob", [128, 4, T], BF16)
            b_kf, b_Qg, b_Vt, b_Ob = Buf(), Buf(), Buf(), Buf()
            if not ctx_out:
                k.op("pool", lambda e: e.memset(Ob[:, :, 0:LC], 0.0), w=[b_Ob])
            with ExitStack() as p1:
                raw = [self.sb(p1, "raw%d" % i, [64, T], F32) for i in range(2)]
                sq = self.sb(p1, "sq", [64, T], F32)
                rs = self.sb(p1, "rs", [64, T], F32)
                t1 = self.sb(p1, "t1", [64, 512], F32)
                t2 = self.sb(p1, "t2", [64, 512], F32)
                b_raw = [Buf(), Buf()]
                b_sq, b_rs, b_t1, b_t2 = Buf(), Buf(), Buf(), Buf()
                for hi in range(10):
                    isq = hi < 8
                    row0 = C_AT + 64 * hi
                    x, bx = raw[hi % 2], b_raw[hi % 2]
                    k.dma(None, x[:, :], self.ZT[b, row0:row0 + 64, :], r=[self.b_ZT[b]], w=[bx])
                    k.op("act", lambda e: e.activation(out=sq[:], in_=x[:], func=AF.Square), r=[bx], w=[b_sq])
                    for ci, (t0, tn) in enumerate(_chunks(T, 512)):
                        pb, bpb = self.ps[ci % 2], self.b_ps[ci % 2]
                        k.op("pe", lambda e: e.matmul(pb[0:64, 0:tn], ones64[:], sq[:, t0:t0 + tn],
                                                      start=True, stop=True), r=[b_sq, b_c], w=[bpb])
                        k.op("dve", lambda e: e.tensor_scalar(
                            out=rs[:, t0:t0 + tn], in0=pb[0:64, 0:tn], scalar1=1.0 / 64, scalar2=NORM_EPS,
                            op0=ALU.mult, op1=ALU.add), r=[bpb], w=[b_rs])
                    k.op("act", lambda e: e.activation(out=rs[:], in_=rs[:], func=AF.Sqrt), r=[b_rs], w=[b_rs])
                    k.op("dve", lambda e: e.reciprocal(out=rs[:], in_=rs[:]), r=[b_rs], w=[b_rs])
                    gn = qg if isq else kg
                    k.op("dve", lambda e: e.scalar_tensor_tensor(
                        out=x[:], in0=x[:], scalar=gn[:, 0:1], in1=rs[:], op0=ALU.mult, op1=ALU.mult),
                        r=[bx, b_rs, b_c], w=[bx])
                    if isq:
                        kvh, g = hi // 4, hi % 4
                        def dst(t0, tn, kvh=kvh, g=g):
                            return Qg[kvh][:, t0 // 128:(t0 + tn) // 128, g, :]
                        bdst = b_Qg
                    else:
                        j = hi - 8
                        def dst(t0, tn, j=j):
                            return kf[j][:, t0:t0 + tn].rearrange("p (n q) -> p n q", q=128)
                        bdst = b_kf
                    k.op("act", lambda e: e.activation(
                        out=dst(0, LC), in_=x[:, 0:LC].rearrange("p (n q) -> p n q", q=128), func=AF.Copy),
                        r=[bx], w=[bdst])
                    for ci, (t0, tn) in enumerate(_chunks(LL, 512)):
                        pb, bpb = self.ps[2 + ci % 2], self.b_ps[2 + ci % 2]
                        k.op("pe", lambda e: e.matmul(pb[0:64, 0:tn], rotT[:], x[:, LC + t0:LC + t0 + tn],
                                                      start=True, stop=True), r=[bx, b_c], w=[bpb])
                        k.op("dve", lambda e: e.tensor_tensor(out=t1[:, 0:tn], in0=pb[0:64, 0:tn],
                                                              in1=sinT[:, t0:t0 + tn], op=ALU.mult),
                             r=[bpb, b_c], w=[b_t1])
                        k.op("pool", lambda e: e.tensor_tensor(out=t2[:, 0:tn], in0=x[:, LC + t0:LC + t0 + tn],
                                                               in1=cosT[:, t0:t0 + tn], op=ALU.mult),
                             r=[bx, b_c], w=[b_t2])
                        k.op("dve", lambda e: e.tensor_tensor(
                            out=dst(LC + t0, tn), in0=t1[:, 0:tn].rearrange("p (n q) -> p n q", q=128),
                            in1=t2[:, 0:tn].rearrange("p (n q) -> p n q", q=128), op=ALU.add),
                            r=[b_t1, b_t2], w=[bdst])
                vx, bvx = raw[0], b_raw[0]
                vT = self.sb(p1, "vT", [128, T], F32)
                b_vT = Buf()
                k.dma(None, vT[:, :], self.ZT[b, C_AT + 640:C_AT + 768, :], r=[self.b_ZT[b]], w=[b_vT])
                for n in range(NBLK):
                    pb, bpb = self.ps[n % 4], self.b_ps[n % 4]
                    k.op("pe", lambda e: e.transpose(out=pb[:, 0:128], in_=vT[:, n * 128:(n + 1) * 128],
                                                     identity=self.ident[:]), r=[b_vT, self.b_ident], w=[bpb])
                    self.copy("dve" if n % 2 else "act",
                              Vt[:, n, :, :].rearrange("p j (two d) -> p j two d", two=2),
                              pb[:, 0:128].rearrange("p (j d) -> p j d", j=2).unsqueeze(2).to_broadcast([128, 2, 2, 64]),
                              r=[bpb], w=[b_Vt])
                k.barrier()
            with ExitStack() as p2:
                P = [self.sb(p2, "P%d" % i, [128, 5, 512], BF16) for i in range(2)]
                dn = [self.sb(p2, "dn%d" % i, [128, 512], F32) for i in range(2)]
                b_P = [Buf(), Buf()]
                b_dn = [Buf(), Buf()]
                it = 0
                for kvh in range(2):
                    for qb in range(NBLK):
                        if qb < NCB:
                            if not ctx_out:
                                continue
                            kbs = [(i, None) for i in range(NCB)]
                        else:
                            kbs = [(i, None) for i in range(NCB)]
                            if qb - 1 >= NCB:
                                kbs.append((qb - 1, mprev))
                            kbs.append((qb, None))
                            if qb + 1 < NBLK:
                                kbs.append((qb + 1, mnext))
                        Pb, bP = P[it % 2], b_P[it % 2]
                        dnb, bdn = dn[it % 2], b_dn[it % 2]
                        it += 1
                        for i, (kb, msk) in enumerate(kbs):
                            pb, bpb = self.ps[i], self.b_ps[i]
                            k.op("pe", lambda e: e.matmul(
                                pb[:, :], kf[kvh][:, kb * 128:(kb + 1) * 128],
                                Qg[kvh][:, qb, :, :].rearrange("p g q -> p (g q)"),
                                start=True, stop=True), r=[b_kf, b_Qg], w=[bpb])
                            k.op("act", lambda e: e.activation(out=Pb[:, i, :], in_=pb[:, :], func=AF.Exp,
                                                               scale=0.125), r=[bpb], w=[bP])
                            if msk is not None:
                                k.op("dve", lambda e: e.tensor_tensor(
                                    out=Pb[:, i, :].rearrange("p (g q) -> p g q", g=4),
                                    in0=Pb[:, i, :].rearrange("p (g q) -> p g q", g=4),
                                    in1=msk[:].unsqueeze(1).to_broadcast([128, 4, 128]), op=ALU.mult),
                                    r=[bP, b_c], w=[bP])
                        pn, bpn = self.ps[5], self.b_ps[5]
                        pd, bpd = self.ps[6], self.b_ps[6]
                        nk = len(kbs)
                        for i, (kb, msk) in enumerate(kbs):
                            k.op("pe", lambda e: e.matmul(pn[:, :], Vt[:, kb, kvh, :], Pb[:, i, :],
                                                          start=(i == 0), stop=(i == nk - 1)),
                                 r=[b_Vt, bP], w=[bpn])
                        for i, (kb, msk) in enumerate(kbs):
                            k.op("pe", lambda e: e.matmul(pd[:, :], onesb[:], Pb[:, i, :],
                                                          start=(i == 0), stop=(i == nk - 1)),
                                 r=[b_c, bP], w=[bpd])
                        k.op("dve", lambda e: e.tensor_tensor(
                            out=dnb[:].rearrange("p (g q) -> p g q", g=4),
                            in0=pd[:, :].rearrange("p (g q) -> p g q", g=4),
                            in1=esink[:, kvh * 4:(kvh + 1) * 4].unsqueeze(2).to_broadcast([128, 4, 128]),
                            op=ALU.add), r=[bpd, b_c], w=[bdn])
                        k.op("dve", lambda e: e.reciprocal(out=dnb[:], in_=dnb[:]), r=[bdn], w=[bdn])
                        for half in range(2):
                            lo, hi_ = half * 64, half * 64 + 64
                            k.op("dve", lambda e: e.tensor_tensor(
                                out=Ob[lo:hi_, kvh * 2:kvh * 2 + 2, qb * 128:(qb + 1) * 128],
                                in0=pn[lo:hi_, :].rearrange("p (g2 gl q) -> p g2 gl q", g2=2, gl=2)[:, :, half, :],
                                in1=dnb[lo:hi_, :].rearrange("p (g2 gl q) -> p g2 gl q", g2=2, gl=2)[:, :, half, :],
                                op=ALU.mult), r=[bpn, bdn], w=[b_Ob])
                k.dma(None, self.OT[b, 1536:2048, :].rearrange("(j p) t -> p j t", p=128), Ob[:, :, :],
                      r=[b_Ob], w=[self.b_OT[b]])
                k.barrier()


    def segs(self):
        cfg = self.cfg
        return [(t0, n) for (t0, n) in _chunks(cfg.LC, 512)] + \
               [(cfg.LC + t0, n) for (t0, n) in _chunks(cfg.LL, 512)]

    def phase_s5_prep(self, l, lst):
        cfg, nc, k = self.cfg, self.nc, self.k
        P = {}
        self.s5p = P
        bc = Buf("s5consts")
        P["b"] = bc
        NT = 32
        for nm in ("rho", "cs1", "sn1"):
            P[nm] = self.sb(lst, "s5_" + nm, [128, NT], F32)
        P["BTr"] = self.sb(lst, "s5_BTr", [32, NT, 128], BF16)
        P["BTi"] = self.sb(lst, "s5_BTi", [32, NT, 128], BF16)
        P["CWr"] = self.sb(lst, "s5_CWr", [128, NT, 128], BF16)
        P["CWi"] = self.sb(lst, "s5_CWi", [128, NT, 128], BF16)
        P["dcol"] = self.sb(lst, "s5_dcol", [128, 4], F32)
        P["gb"] = self.sb(lst, "s5_gb", [128, 4], F32)
        P["GW"] = self.sb(lst, "s5_GW", [128, 4, 512], BF16)
        k.dma("sp", P["dcol"][:, :], self.inp_s5_d[l].rearrange("(f p) -> p f", p=128), w=[bc])
        k.dma("sp", P["gb"][:, :], self.inp_s5_glu_b[l].rearrange("(f p) -> p f", p=128), w=[bc])
        k.dma("pool", P["GW"][:, :, :], self.inp_s5_glu_w[l].rearrange("(f p) c -> p f c", p=128), w=[bc])
        with ExitStack() as ph:
            def t(name, shape, dt=F32):
                return self.sb(ph, "s5p_" + name, shape, dt)
            lre, lim, stp = t("lre", [128, NT]), t("lim", [128, NT]), t("stp", [128, NT])
            pat = "d (gp g2) p -> (g2 p) (d gp)"
            k.dma("sp", lre[:, :], self.inp_s5_lre[l].rearrange(pat, g2=2), w=[bc])
            k.dma("act", lim[:, :], self.inp_s5_lim[l].rearrange(pat, g2=2), w=[bc])
            ls = self.inp_s5_ls[l].rearrange("d (gp g2) -> g2 (d gp)", g2=2)
            for g2 in range(2):
                k.dma("sp", stp[g2 * 64:(g2 + 1) * 64, :], ls[g2].partition_broadcast(64), w=[bc])
            bre, bim = t("bre", [128, NT, 16]), t("bim", [128, NT, 16])
            patb = "d (gp g2) p c -> (g2 p) (d gp) c"
            k.dma("sp", bre[:, :, :], self.inp_s5_bre[l].rearrange(patb, g2=2), w=[bc])
            k.dma("act", bim[:, :, :], self.inp_s5_bim[l].rearrange(patb, g2=2), w=[bc])
            CNr, CNi = t("CNr", [32, NT, 128]), t("CNi", [32, NT, 128])
            k.op("pool", lambda e: e.memset(CNr[:], 0.0), w=[bc])
            k.op("pool", lambda e: e.memset(CNi[:], 0.0), w=[bc])
            patc = "d (gp g2) c p -> g2 c (d gp) p"
            for g2 in range(2):
                k.dma("sp", CNr[g2 * 16:(g2 + 1) * 16, :, g2 * 64:(g2 + 1) * 64],
                      self.inp_s5_cre[l].rearrange(patc, g2=2)[g2], w=[bc])
                k.dma("act", CNi[g2 * 16:(g2 + 1) * 16, :, g2 * 64:(g2 + 1) * 64],
                      self.inp_s5_cim[l].rearrange(patc, g2=2)[g2], w=[bc])
            dv = lambda fn: k.op("dve", fn, r=[bc], w=[bc])
            ac = lambda fn: k.op("act", fn, r=[bc], w=[bc])
            ac(lambda e: e.activation(out=stp[:], in_=stp[:], func=AF.Exp))
            zr, zi = t("zr", [128, NT]), t("zi", [128, NT])
            dv(lambda e: e.tensor_tensor(out=zr[:], in0=lre[:], in1=stp[:], op=ALU.mult))
            dv(lambda e: e.tensor_tensor(out=zi[:], in0=lim[:], in1=stp[:], op=ALU.mult))
            ac(lambda e: e.activation(out=P["rho"][:], in_=zr[:], func=AF.Exp))
            sa, sh, ca, tmp, tmp2 = t("sa", [128, NT]), t("sh", [128, NT]), t("ca", [128, NT]), t("tmp", [128, NT]), t("tmp2", [128, NT])
            ac(lambda e: e.activation(out=sa[:], in_=zi[:], func=AF.Sin, scale=1.0 / 32))
            ac(lambda e: e.activation(out=sh[:], in_=zi[:], func=AF.Sin, scale=1.0 / 64))
            dv(lambda e: e.tensor_tensor(out=tmp[:], in0=sh[:], in1=sh[:], op=ALU.mult))
            dv(lambda e: e.tensor_scalar(out=ca[:], in0=tmp[:], scalar1=-2.0, scalar2=1.0, op0=ALU.mult, op1=ALU.add))
            for _ in range(5):
                dv(lambda e: e.tensor_tensor(out=tmp[:], in0=ca[:], in1=ca[:], op=ALU.mult))
                dv(lambda e: e.tensor_tensor(out=tmp2[:], in0=sa[:], in1=sa[:], op=ALU.mult))
                dv(lambda e: e.scalar_tensor_tensor(out=sa[:], in0=sa[:], scalar=2.0, in1=ca[:], op0=ALU.mult, op1=ALU.mult))
                dv(lambda e: e.tensor_tensor(out=ca[:], in0=tmp[:], in1=tmp2[:], op=ALU.subtract))
            self.copy("dve", P["cs1"][:], ca[:], r=[bc], w=[bc])
            self.copy("dve", P["sn1"][:], sa[:], r=[bc], w=[bc])
            nr, ni, den, cr, ci = t("nr", [128, NT]), t("ni", [128, NT]), t("den", [128, NT]), t("cr", [128, NT]), t("ci", [128, NT])
            dv(lambda e: e.tensor_tensor(out=nr[:], in0=P["rho"][:], in1=ca[:], op=ALU.mult))
            dv(lambda e: e.tensor_scalar(out=nr[:], in0=nr[:], scalar1=-1.0, scalar2=None, op0=ALU.add))
            dv(lambda e: e.tensor_tensor(out=ni[:], in0=P["rho"][:], in1=sa[:], op=ALU.mult))
            dv(lambda e: e.tensor_tensor(out=den[:], in0=lre[:], in1=lre[:], op=ALU.mult))
            dv(lambda e: e.tensor_tensor(out=tmp[:], in0=lim[:], in1=lim[:], op=ALU.mult))
            dv(lambda e: e.tensor_tensor(out=den[:], in0=den[:], in1=tmp[:], op=ALU.add))
            dv(lambda e: e.reciprocal(out=den[:], in_=den[:]))
            dv(lambda e: e.tensor_tensor(out=cr[:], in0=nr[:], in1=lre[:], op=ALU.mult))
            dv(lambda e: e.tensor_tensor(out=tmp[:], in0=ni[:], in1=lim[:], op=ALU.mult))
            dv(lambda e: e.tensor_tensor(out=cr[:], in0=cr[:], in1=tmp[:], op=ALU.add))
            dv(lambda e: e.tensor_tensor(out=cr[:], in0=cr[:], in1=den[:], op=ALU.mult))
            dv(lambda e: e.tensor_tensor(out=ci[:], in0=ni[:], in1=lre[:], op=ALU.mult))
            dv(lambda e: e.tensor_tensor(out=tmp[:], in0=nr[:], in1=lim[:], op=ALU.mult))
            dv(lambda e: e.tensor_tensor(out=ci[:], in0=ci[:], in1=tmp[:], op=ALU.subtract))
            dv(lambda e: e.tensor_tensor(out=ci[:], in0=ci[:], in1=den[:], op=ALU.mult))
            BBr, BBi = t("BBr", [128, NT, 32]), t("BBi", [128, NT, 32])
            k.op("pool", lambda e: e.memset(BBr[:], 0.0), w=[bc])
            k.op("pool", lambda e: e.memset(BBi[:], 0.0), w=[bc])
            t3, t4 = t("t3", [128, NT, 16]), t("t4", [128, NT, 16])
            crb = cr[:].unsqueeze(2).to_broadcast([128, NT, 16])
            cib = ci[:].unsqueeze(2).to_broadcast([128, NT, 16])
            dv(lambda e: e.tensor_tensor(out=t3[:], in0=bre[:], in1=crb, op=ALU.mult))
            dv(lambda e: e.tensor_tensor(out=t4[:], in0=bim[:], in1=cib, op=ALU.mult))
            for g2 in range(2):
                lo, hi = g2 * 64, g2 * 64 + 64
                dv(lambda e: e.tensor_tensor(out=BBr[lo:hi, :, g2 * 16:(g2 + 1) * 16], in0=t3[lo:hi], in1=t4[lo:hi], op=ALU.subtract))
            dv(lambda e: e.tensor_tensor(out=t3[:], in0=bim[:], in1=crb, op=ALU.mult))
            dv(lambda e: e.tensor_tensor(out=t4[:], in0=bre[:], in1=cib, op=ALU.mult))
            for g2 in range(2):
                lo, hi = g2 * 64, g2 * 64 + 64
                dv(lambda e: e.tensor_tensor(out=BBi[lo:hi, :, g2 * 16:(g2 + 1) * 16], in0=t3[lo:hi], in1=t4[lo:hi], op=ALU.add))
            k.op("pool", lambda e: e.memset(P["CWr"][:], 0.0), w=[bc])
            k.op("pool", lambda e: e.memset(P["CWi"][:], 0.0), w=[bc])
            pb, bpb = self.ps[7], self.b_ps[7]
            for ti in range(NT):
                gq = (ti % 16) % 4
                k.op("pe", lambda e: e.transpose(out=pb[0:32, 0:128], in_=BBr[:, ti, :], identity=self.ident[:]),
                     r=[bc, self.b_ident], w=[bpb])
                k.op("pe", lambda e: e.transpose(out=pb[0:32, 128:256], in_=BBi[:, ti, :], identity=self.ident[:]),
                     r=[bc, self.b_ident], w=[bpb])
                k.op("pe", lambda e: e.transpose(out=pb[:, 256:288], in_=CNr[:, ti, :], identity=self.ident[0:32, 0:32]),
                     r=[bc, self.b_ident], w=[bpb])
                k.op("pe", lambda e: e.transpose(out=pb[:, 288:320], in_=CNi[:, ti, :], identity=self.ident[0:32, 0:32]),
                     r=[bc, self.b_ident], w=[bpb])
                self.copy("act", P["BTr"][:, ti, :], pb[0:32, 0:128], r=[bpb], w=[bc])
                self.copy("dve", P["BTi"][:, ti, :], pb[0:32, 128:256], r=[bpb], w=[bc])
                self.copy("act", P["CWr"][:, ti, gq * 32:(gq + 1) * 32], pb[:, 256:288], r=[bpb], w=[bc])
                k.op("dve", lambda e: e.tensor_scalar(out=P["CWi"][:, ti, gq * 32:(gq + 1) * 32], in0=pb[:, 288:320],
                                                      scalar1=-1.0, scalar2=None, op0=ALU.mult), r=[bpb], w=[bc])
            k.barrier()

    def phase_s5(self, l, b):
        cfg, nc, k = self.cfg, self.nc, self.k
        T, LC, LL = cfg.T, cfg.LC, cfg.LL
        P = self.s5p
        bc = P["b"]
        segs = self.segs()
        assert len(segs) <= 5

        def rsl(n0, ln):
            return slice(n0, (n0 - ln) if n0 - ln >= 0 else None, -1)

        with ExitStack() as ph:
            def t(name, shape, dt=F32):
                return self.sb(ph, "s5_" + name, shape, dt)
            cosN, sinN = t("cosN", [128, T]), t("sinN", [128, T])
            tA, tB = t("tA", [128, T // 2 + 2]), t("tB", [128, T // 2 + 2])
            wre, wim, qre, qim = t("wre", [128, T]), t("wim", [128, T]), t("qre", [128, T]), t("qim", [128, T])
            bur, bui = t("bur", [128, 512]), t("bui", [128, 512])
            ta, tb_, tc, td = t("ta", [128, 512]), t("tb", [128, 512]), t("tc", [128, 512]), t("td", [128, 512])
            hre, him = t("hre", [128, 512], BF16), t("him", [128, 512], BF16)
            uT = [t("uT%d" % i, [32, T], BF16) for i in range(2)]
            uF = t("uF", [128, T])
            Y0 = t("Y0", [128, T])
            YGf = t("YGf", [128, 4, T])
            YGb = t("YGb", [128, 4, T], BF16)
            b_tab, b_w, b_q, b_bu, b_t, b_h, b_uF, b_Y0, b_YG, b_Ob = (Buf() for _ in range(10))
            b_t2 = Buf()
            b_uT = [Buf(), Buf()]
            it = 0
            for ft in range(4):
                nacc = [0] * len(segs)
                for gq in range(4):
                    gp = ft * 4 + gq
                    u, bu_ = uT[it % 2], b_uT[it % 2]
                    it += 1
                    r0 = C_S5 + 32 * gp
                    k.dma("pool", u[:, :], self.ZT[b, r0:r0 + 32, :], r=[self.b_ZT[b]], w=[bu_])
                    for d in range(2):
                        ti = d * 16 + gp
                        k.op("pool", lambda e: e.memset(cosN[:, 0:1], 1.0), w=[b_tab])
                        k.op("pool", lambda e: e.memset(sinN[:, 0:1], 0.0), w=[b_tab])
                        self.copy("dve", cosN[:, 1:2], P["cs1"][:, ti:ti + 1], r=[bc], w=[b_tab])
                        self.copy("dve", sinN[:, 1:2], P["sn1"][:, ti:ti + 1], r=[bc], w=[b_tab])
                        m = 1
                        while m < T - 1:
                            ln = min(m, T - 1 - m)
                            cr_, ci_ = cosN[:, m:m + 1], sinN[:, m:m + 1]
                            src_c, src_s = cosN[:, 1:1 + ln], sinN[:, 1:1 + ln]
                            k.op("pool", lambda e: e.tensor_scalar(out=tA[:, 0:ln], in0=src_s, scalar1=ci_, scalar2=None, op0=ALU.mult),
                                 r=[b_tab], w=[b_t])
                            k.op("dve", lambda e: e.tensor_scalar(out=tB[:, 0:ln], in0=src_s, scalar1=cr_, scalar2=None, op0=ALU.mult),
                                 r=[b_tab], w=[b_t2])
                            k.op("dve", lambda e: e.scalar_tensor_tensor(out=cosN[:, m + 1:m + 1 + ln], in0=src_c, scalar=cr_, in1=tA[:, 0:ln],
                                                                         op0=ALU.mult, op1=ALU.subtract), r=[b_tab, b_t], w=[b_tab])
                            k.op("dve", lambda e: e.scalar_tensor_tensor(out=sinN[:, m + 1:m + 1 + ln], in0=src_c, scalar=ci_, in1=tB[:, 0:ln],
                                                                         op0=ALU.mult, op1=ALU.add), r=[b_tab, b_t2], w=[b_tab])
                            m += ln
                        for (t0, tn) in segs:
                            if d == 0:
                                csl = slice(t0, t0 + tn)
                            elif t0 < LC:
                                csl = rsl(LC - 1 - t0, tn)
                            else:
                                csl = rsl(T - 1 + LC - t0, tn)
                            pr, bpr = self.ps[5], self.b_ps[5]
                            pi_, bpi = self.ps[6], self.b_ps[6]
                            k.op("pe", lambda e: e.matmul(pr[:, 0:tn], P["BTr"][:, ti, :], u[:, t0:t0 + tn], start=True, stop=True),
                                 r=[bc, bu_], w=[bpr])
                            k.op("pe", lambda e: e.matmul(pi_[:, 0:tn], P["BTi"][:, ti, :], u[:, t0:t0 + tn], start=True, stop=True),
                                 r=[bc, bu_], w=[bpi])
                            self.copy("act", bur[:, 0:tn], pr[:, 0:tn], r=[bpr], w=[b_bu])
                            self.copy("act", bui[:, 0:tn], pi_[:, 0:tn], r=[bpi], w=[b_bu])
                            k.op("dve", lambda e: e.tensor_tensor(out=ta[:, 0:tn], in0=bur[:, 0:tn], in1=cosN[:, csl], op=ALU.mult), r=[b_bu, b_tab], w=[b_t])
                            k.op("pool", lambda e: e.tensor_tensor(out=tb_[:, 0:tn], in0=bui[:, 0:tn], in1=sinN[:, csl], op=ALU.mult), r=[b_bu, b_tab], w=[b_t2])
                            k.op("dve", lambda e: e.tensor_tensor(out=wre[:, t0:t0 + tn], in0=ta[:, 0:tn], in1=tb_[:, 0:tn], op=ALU.add), r=[b_t, b_t2], w=[b_w])
                            k.op("pool", lambda e: e.tensor_tensor(out=tc[:, 0:tn], in0=bui[:, 0:tn], in1=cosN[:, csl], op=ALU.mult), r=[b_bu, b_tab], w=[b_t2])
                            k.op("dve", lambda e: e.tensor_tensor(out=td[:, 0:tn], in0=bur[:, 0:tn], in1=sinN[:, csl], op=ALU.mult), r=[b_bu, b_tab], w=[b_t])
                            k.op("pool", lambda e: e.tensor_tensor(out=wim[:, t0:t0 + tn], in0=tc[:, 0:tn], in1=td[:, 0:tn], op=ALU.subtract), r=[b_t, b_t2], w=[b_w])
                        rho_b = P["rho"][:, ti:ti + 1]
                        for (wsrc, qdst) in ((wre, qre), (wim, qim)):
                            if d == 0:
                                k.op("dve", lambda e: e.tensor_tensor_scan(
                                    out=qdst[:, 0:T], data0=rho_b.to_broadcast([128, T]), data1=wsrc[:, 0:T],
                                    initial=0.0, op0=ALU.mult, op1=ALU.add), r=[b_w, bc], w=[b_q])
                            else:
                                k.op("dve", lambda e: e.tensor_tensor_scan(
                                    out=qdst[:, rsl(LC - 1, LC)], data0=rho_b.to_broadcast([128, LC]),
                                    data1=wsrc[:, rsl(LC - 1, LC)], initial=0.0, op0=ALU.mult, op1=ALU.add),
                                    r=[b_w, bc], w=[b_q])
                                k.op("dve", lambda e: e.tensor_tensor_scan(
                                    out=qdst[:, rsl(T - 1, LL)], data0=rho_b.to_broadcast([128, LL]),
                                    data1=wsrc[:, rsl(T - 1, LL)], initial=qdst[:, 0:1], op0=ALU.mult, op1=ALU.add),
                                    r=[b_w, bc, b_q], w=[b_q])
                        for si, (t0, tn) in enumerate(segs):
                            if d == 0:
                                csl = slice(t0, t0 + tn)
                            elif t0 < LC:
                                csl = rsl(LC - 1 - t0, tn)
                            else:
                                csl = rsl(T - 1 + LC - t0, tn)
                            tsl = slice(t0, t0 + tn)
                            k.op("dve", lambda e: e.tensor_tensor(out=ta[:, 0:tn], in0=qre[:, tsl], in1=cosN[:, csl], op=ALU.mult), r=[b_q, b_tab], w=[b_t])
                            k.op("pool", lambda e: e.tensor_tensor(out=tb_[:, 0:tn], in0=qim[:, tsl], in1=sinN[:, csl], op=ALU.mult), r=[b_q, b_tab], w=[b_t2])
                            k.op("dve", lambda e: e.tensor_tensor(out=hre[:, 0:tn], in0=ta[:, 0:tn], in1=tb_[:, 0:tn], op=ALU.subtract), r=[b_t, b_t2], w=[b_h])
                            k.op("pool", lambda e: e.tensor_tensor(out=tc[:, 0:tn], in0=qim[:, tsl], in1=cosN[:, csl], op=ALU.mult), r=[b_q, b_tab], w=[b_t2])
                            k.op("dve", lambda e: e.tensor_tensor(out=td[:, 0:tn], in0=qre[:, tsl], in1=sinN[:, csl], op=ALU.mult), r=[b_q, b_tab], w=[b_t])
                            k.op("pool", lambda e: e.tensor_tensor(out=him[:, 0:tn], in0=tc[:, 0:tn], in1=td[:, 0:tn], op=ALU.add), r=[b_t, b_t2], w=[b_h])
                            py, bpy = self.ps[si], self.b_ps[si]
                            k.op("pe", lambda e: e.matmul(py[:, 0:tn], P["CWr"][:, ti, :], hre[:, 0:tn],
                                                          start=(nacc[si] == 0), stop=False), r=[bc, b_h], w=[bpy])
                            nacc[si] += 1
                            k.op("pe", lambda e: e.matmul(py[:, 0:tn], P["CWi"][:, ti, :], him[:, 0:tn],
                                                          start=False, stop=(nacc[si] == 15)), r=[bc, b_h], w=[bpy])
                            nacc[si] += 1
                r0 = C_S5 + 128 * ft
                k.dma(None, uF[:, :], self.ZT[b, r0:r0 + 128, :], r=[self.b_ZT[b]], w=[b_uF])
                for si, (t0, tn) in enumerate(segs):
                    py, bpy = self.ps[si], self.b_ps[si]
                    tsl = slice(t0, t0 + tn)
                    k.op("dve", lambda e: e.scalar_tensor_tensor(out=Y0[:, tsl], in0=uF[:, tsl], scalar=P["dcol"][:, ft:ft + 1],
                                                                 in1=py[:, 0:tn], op0=ALU.mult, op1=ALU.add),
                         r=[b_uF, bc, bpy], w=[b_Y0])
                k.op("pool", lambda e: e.tensor_tensor(out=wre[:], in0=Y0[:], in1=Y0[:], op=ALU.mult), r=[b_Y0], w=[b_w])
                k.op("pool", lambda e: e.tensor_scalar(out=wre[:], in0=wre[:], scalar1=0.044715, scalar2=1.0, op0=ALU.mult, op1=ALU.add), r=[b_w], w=[b_w])
                k.op("pool", lambda e: e.tensor_tensor(out=wre[:], in0=wre[:], in1=Y0[:], op=ALU.mult), r=[b_w, b_Y0], w=[b_w])
                k.op("act", lambda e: e.activation(out=wre[:], in_=wre[:], func=AF.Sigmoid, scale=1.5957691216057308), r=[b_w], w=[b_w])
                k.op("dve", lambda e: e.tensor_tensor(out=YGf[:, ft, :], in0=Y0[:], in1=wre[:], op=ALU.mult), r=[b_w, b_Y0], w=[b_YG])
                self.copy("act", YGb[:, ft, :], YGf[:, ft, :], r=[b_YG], w=[b_YG])
            ev = 0
            b_ob2 = [Buf(), Buf()]
            for fo in range(4):
                ob, bob = (qre, qim)[fo % 2], b_ob2[fo % 2]
                for si, (t0, tn) in enumerate(segs):
                    pb, bpb = self.ps[ev % 4], self.b_ps[ev % 4]
                    ev += 1
                    for fi in range(4):
                        k.op("pe", lambda e: e.matmul(pb[:, 0:tn], P["GW"][:, fi, fo * 128:(fo + 1) * 128], YGb[:, fi, t0:t0 + tn],
                                                      start=(fi == 0), stop=(fi == 3)), r=[bc, b_YG], w=[bpb])
                    k.op("act", lambda e: e.activation(out=ta[:, 0:tn], in_=pb[:, 0:tn], func=AF.Sigmoid,
                                                       bias=P["gb"][:, fo:fo + 1]), r=[bpb, bc], w=[b_t])
                    k.op("dve", lambda e: e.tensor_tensor(out=ob[:, t0:t0 + tn], in0=YGf[:, fo, t0:t0 + tn], in1=ta[:, 0:tn], op=ALU.mult),
                         r=[b_t, b_YG], w=[bob])
                k.dma("pool", self.OT[b, 512 + fo * 128:512 + (fo + 1) * 128, :], ob[:, :], r=[bob], w=[self.b_OT[b]])
            k.barrier()


    def phase_hg_prep(self, l, lst):
        cfg, nc, k = self.cfg, self.nc, self.k
        DEPTH = cfg.DEPTH
        P = {}
        self.hgp = P
        bc = Buf("hgconsts")
        P["b"] = bc
        P["lb"] = self.sb(lst, "hg_lb", [128, 4], F32)
        P["oml"] = self.sb(lst, "hg_oml", [128, 4], F32)
        P["fb"] = self.sb(lst, "hg_fb", [128, 2, 4], F32)
        P["ng"] = self.sb(lst, "hg_ng", [128, 1], F32)
        P["Z"] = self.sb(lst, "hg_Z", [128, 255], BF16)
        P["identb"] = self.sb(lst, "hg_identb", [128, 128], BF16)
        P["ones"] = self.sb(lst, "hg_ones", [128, 128], F32)
        k.op("pool", lambda e: e.memset(P["Z"][:], 0.0), w=[bc])
        k.op("pool", lambda e: e.memset(P["Z"][:, 127:128], 1.0), r=[bc], w=[bc])
        k.op("pool", lambda e: e.memset(P["ones"][:], 1.0), w=[bc])
        self.copy("dve", P["identb"][:], self.ident[:], r=[self.b_ident], w=[bc])
        k.dma("sp", P["fb"][:, :, :], self.inp_hg_fb[l].rearrange("d (f p) -> p d f", p=128), w=[bc])
        k.dma("sp", P["ng"][:, :], self.inp_hg_ng[l].rearrange("(p o) -> p o", o=1), w=[bc])
        with ExitStack() as ph:
            lg = self.sb(ph, "hg_lg", [128, DEPTH, 4], F32)
            ssum = self.sb(ph, "hg_ssum", [128, 4], F32)
            k.dma("sp", lg[:, :, :], self.inp_hg_lbl.rearrange("l (f p) -> p l f", p=128), w=[bc])
            k.op("act", lambda e: e.activation(out=lg[:], in_=lg[:], func=AF.Exp), r=[bc], w=[bc])
            self.copy("dve", ssum[:], lg[:, 0, :], r=[bc], w=[bc])
            for j in range(1, DEPTH):
                k.op("dve", lambda e: e.tensor_tensor(out=ssum[:], in0=ssum[:], in1=lg[:, j, :], op=ALU.add), r=[bc], w=[bc])
            k.op("dve", lambda e: e.reciprocal(out=ssum[:], in_=ssum[:]), r=[bc], w=[bc])
            k.op("pool", lambda e: e.memset(P["lb"][:], 0.0), r=[bc], w=[bc])
            for j in range(1, l + 1):
                k.op("dve", lambda e: e.tensor_tensor(out=P["lb"][:], in0=P["lb"][:], in1=lg[:, j, :], op=ALU.add), r=[bc], w=[bc])
            k.op("dve", lambda e: e.tensor_tensor(out=P["lb"][:], in0=P["lb"][:], in1=ssum[:], op=ALU.mult), r=[bc], w=[bc])
            k.op("dve", lambda e: e.tensor_scalar(out=P["oml"][:], in0=P["lb"][:], scalar1=-1.0, scalar2=1.0,
                                                  op0=ALU.mult, op1=ALU.add), r=[bc], w=[bc])
            k.barrier()

    def phase_hg(self, l, b):
        cfg, nc, k = self.cfg, self.nc, self.k
        T, LC, LL = cfg.T, cfg.LC, cfg.LL
        P = self.hgp
        bc = P["b"]
        segs = self.segs()

        def rsl(n0, ln):
            return slice(n0, (n0 - ln) if n0 - ln >= 0 else None, -1)

        with ExitStack() as ph:
            def t(name, shape, dt=F32):
                return self.sb(ph, "hg_" + name, shape, dt)
            fT, kT, qT, gT, oS = t("fT", [128, T]), t("kT", [128, T]), t("qT", [128, T]), t("gT", [128, T]), t("oS", [128, T])
            vT = t("vT", [128, T], BF16)
            kv = [t("kv%d" % i, [128, T]) for i in range(2)]
            S = [t("S%d" % i, [128, T]) for i in range(2)]
            qs = [t("qs%d" % i, [128, T], BF16) for i in range(2)]
            b_f, b_q, b_v, b_g, b_o = Buf(), Buf(), Buf(), Buf(), Buf()
            b_kv, b_S, b_qs = [Buf(), Buf()], [Buf(), Buf()], [Buf(), Buf()]
            it = 0
            for h in range(4):
                r_q = C_HG + 128 * h
                k.dma(None, qT[:, :], self.ZT[b, r_q:r_q + 128, :], r=[self.b_ZT[b]], w=[b_q])
                r_v = C_HG + 1536 + 128 * h
                k.dma("pool", vT[:, :], self.ZT[b, r_v:r_v + 128, :], r=[self.b_ZT[b]], w=[b_v])
                r_g = C_HG + 2048 + 128 * h
                k.dma(None, gT[:, :], self.ZT[b, r_g:r_g + 128, :], r=[self.b_ZT[b]], w=[b_g])
                nacc = 0
                for d in range(2):
                    r_f = C_HG + 512 * (1 + d) + 128 * h
                    k.dma(None, fT[:, :], self.ZT[b, r_f:r_f + 128, :], r=[self.b_ZT[b]], w=[b_f])
                    k.op("act", lambda e: e.activation(out=fT[:], in_=fT[:], func=AF.Sigmoid, bias=P["fb"][:, d, h:h + 1]),
                         r=[b_f, bc], w=[b_f])
                    k.op("dve", lambda e: e.tensor_scalar(out=fT[:], in0=fT[:], scalar1=P["oml"][:, h:h + 1], scalar2=P["lb"][:, h:h + 1],
                                                          op0=ALU.mult, op1=ALU.add), r=[b_f, bc], w=[b_f])
                    k.op("pool", lambda e: e.tensor_scalar(out=kT[:], in0=fT[:], scalar1=-1.0, scalar2=1.0,
                                                           op0=ALU.mult, op1=ALU.add), r=[b_f], w=[b_f])
                    for j in range(128):
                        kvb, bkv = kv[it % 2], b_kv[it % 2]
                        Sb, bS = S[it % 2], b_S[it % 2]
                        qsb, bqs = qs[it % 2], b_qs[it % 2]
                        it += 1
                        for ci, (t0, tn) in enumerate(segs):
                            pb, bpb = self.ps[5 + ci % 2], self.b_ps[5 + ci % 2]
                            k.op("pe", lambda e: e.matmul(pb[:, 0:tn], P["identb"][:, j:j + 1].to_broadcast([128, 128]),
                                                          vT[:, t0:t0 + tn], start=True, stop=True), r=[bc, b_v], w=[bpb])
                            k.op("dve", lambda e: e.tensor_tensor(out=kvb[:, t0:t0 + tn], in0=pb[:, 0:tn], in1=kT[:, t0:t0 + tn], op=ALU.mult),
                                 r=[bpb, b_f], w=[bkv])
                        if d == 0:
                            k.op("dve", lambda e: e.tensor_tensor_scan(out=Sb[:, 0:T], data0=fT[:, 0:T], data1=kvb[:, 0:T], initial=0.0,
                                                                       op0=ALU.mult, op1=ALU.add), r=[b_f, bkv], w=[bS])
                        else:
                            k.op("dve", lambda e: e.tensor_tensor_scan(out=Sb[:, rsl(LC - 1, LC)], data0=fT[:, rsl(LC - 1, LC)],
                                                                       data1=kvb[:, rsl(LC - 1, LC)], initial=0.0,
                                                                       op0=ALU.mult, op1=ALU.add), r=[b_f, bkv], w=[bS])
                            k.op("dve", lambda e: e.tensor_tensor_scan(out=Sb[:, rsl(T - 1, LL)], data0=fT[:, rsl(T - 1, LL)],
                                                                       data1=kvb[:, rsl(T - 1, LL)], initial=Sb[:, 0:1],
                                                                       op0=ALU.mult, op1=ALU.add), r=[b_f, bkv, bS], w=[bS])
                        k.op("pool", lambda e: e.tensor_tensor(out=qsb[:], in0=Sb[:], in1=qT[:], op=ALU.mult), r=[bS, b_q], w=[bqs])
                        for ci, (t0, tn) in enumerate(segs):
                            py, bpy = self.ps[ci], self.b_ps[ci]
                            k.op("pe", lambda e: e.matmul(py[:, 0:tn], P["Z"][:, 127 - j:255 - j], qsb[:, t0:t0 + tn],
                                                          start=(nacc == 0), stop=(nacc == 255)), r=[bc, bqs], w=[bpy])
                        nacc += 1
                for ci, (t0, tn) in enumerate(segs):
                    py, bpy = self.ps[ci], self.b_ps[ci]
                    self.copy("act", oS[:, t0:t0 + tn], py[:, 0:tn], r=[bpy], w=[b_o])
                sq, bsq = kv[0], b_kv[0]
                rs, brs = kv[1], b_kv[1]
                k.op("act", lambda e: e.activation(out=sq[:], in_=oS[:], func=AF.Square), r=[b_o], w=[bsq])
                for ci, (t0, tn) in enumerate(segs):
                    pb, bpb = self.ps[5 + ci % 2], self.b_ps[5 + ci % 2]
                    k.op("pe", lambda e: e.matmul(pb[:, 0:tn], P["ones"][:], sq[:, t0:t0 + tn], start=True, stop=True),
                         r=[bc, bsq], w=[bpb])
                    k.op("dve", lambda e: e.tensor_scalar(out=rs[:, t0:t0 + tn], in0=pb[:, 0:tn], scalar1=1.0 / 128, scalar2=NORM_EPS,
                                                          op0=ALU.mult, op1=ALU.add), r=[bpb], w=[brs])
                k.op("act", lambda e: e.activation(out=rs[:], in_=rs[:], func=AF.Sqrt), r=[brs], w=[brs])
                k.op("dve", lambda e: e.reciprocal(out=rs[:], in_=rs[:]), r=[brs], w=[brs])
                k.op("dve", lambda e: e.scalar_tensor_tensor(out=oS[:], in0=oS[:], scalar=P["ng"][:, 0:1], in1=rs[:],
                                                             op0=ALU.mult, op1=ALU.mult), r=[b_o, brs, bc], w=[b_o])
                k.op("act", lambda e: e.activation(out=gT[:], in_=gT[:], func=AF.Sigmoid), r=[b_g], w=[b_g])
                k.op("dve", lambda e: e.tensor_tensor(out=oS[:], in0=oS[:], in1=gT[:], op=ALU.mult), r=[b_o, b_g], w=[b_o])
                k.dma("pool", self.OT[b, 1024 + 128 * h:1024 + 128 * (h + 1), :], oS[:, :], r=[b_o], w=[self.b_OT[b]])
            k.barrier()


    def phase_rw(self, l, lst):
        cfg, nc, k = self.cfg, self.nc, self.k
        NB, T, LC, LL = cfg.NB, cfg.T, cfg.LC, cfg.LL
        NBLK = T // 128
        NCOL = NB * 8
        TB, TV = 64, 8
        segs = self.segs()
        bc = Buf("rwconsts")

        def rsl(n0, ln):
            return slice(n0, (n0 - ln) if n0 - ln >= 0 else None, -1)

        def col(b, d, hp):
            return (b * 2 + d) * 4 + hp

        bones = self.sb(lst, "rw_bones", [128, 128], F32)
        bonesb = self.sb(lst, "rw_bonesb", [128, 128], BF16)
        ind2 = self.sb(lst, "rw_ind2", [2, 128], BF16)
        ZZ = self.sb(lst, "rw_ZZ", [128, 254], BF16)
        k.op("pool", lambda e: e.memset(bones[:], 0.0), w=[bc])
        k.op("pool", lambda e: e.memset(bones[0:64, 0:64], 1.0), r=[bc], w=[bc])
        k.op("pool", lambda e: e.memset(bones[64:128, 64:128], 1.0), r=[bc], w=[bc])
        self.copy("dve", bonesb[:], bones[:], r=[bc], w=[bc])
        k.dma("pool", ind2[:, :], self.c_ind2[:, :], w=[bc])
        antiI = self.sb(lst, "rw_antiI", [128, 128], F32)
        k.dma("sp", antiI[:, :], self.c_antiI[:, :], w=[bc])
        k.op("pool", lambda e: e.memset(ZZ[:], 0.0), r=[bc], w=[bc])
        k.op("pool", lambda e: e.memset(ZZ[0:64, 126:127], 1.0), r=[bc], w=[bc])
        k.op("pool", lambda e: e.memset(ZZ[64:128, 127:128], 1.0), r=[bc], w=[bc])
        pc = {}
        for nm, src in (("kk", self.inp_rw_kk[l]), ("ka", self.inp_rw_ka[l]), ("rk", self.inp_rw_rk[l]),
                        ("lnw", self.inp_rw_lnw[l]), ("lnb", self.inp_rw_lnb[l])):
            pc[nm] = self.sb(lst, "rw_" + nm, [128, 4], F32)
            k.dma("sp", pc[nm][:, :], src.rearrange("(f p) -> p f", p=128), w=[bc])
        pc["omka"] = self.sb(lst, "rw_omka", [128, 4], F32)
        k.op("dve", lambda e: e.tensor_scalar(out=pc["omka"][:], in0=pc["ka"][:], scalar1=-1.0, scalar2=1.0, op0=ALU.mult, op1=ALU.add),
             r=[bc], w=[bc])
        for nm, src in (("w0", self.inp_rw_w0[l]), ("a0", self.inp_rw_a0[l])):
            pc[nm] = self.sb(lst, "rw_" + nm, [128, 2, 4], F32)
            k.dma("sp", pc[nm][:, :, :], src.rearrange("d (f p) -> p d f", p=128), w=[bc])
        W2 = self.sb(lst, "rw_W2", [64, 2, 512], BF16)
        A2 = self.sb(lst, "rw_A2", [64, 2, 512], BF16)
        G2 = self.sb(lst, "rw_G2", [128, 512], BF16)
        k.dma("pool", W2[:, :, :], self.inp_rw_w2[l].rearrange("d r c -> r d c"), w=[bc])
        k.dma("pool", A2[:, :, :], self.inp_rw_a2[l].rearrange("d r c -> r d c"), w=[bc])
        k.dma("pool", G2[:, :], self.inp_rw_g2[l], w=[bc])
        mu = self.sb(lst, "rw_mu", [128, 15, 2], F32)
        c0 = self.sb(lst, "rw_c0", [128, 15], F32)
        for m_ in range(2):
            k.dma("sp", mu[:, :, m_], self.inp_rw_mu[l][m_].rearrange("(f p) -> p f", p=128), w=[bc])
        k.op("dve", lambda e: e.tensor_tensor(out=c0[:], in0=mu[:, :, 0], in1=mu[:, :, 1], op=ALU.add), r=[bc], w=[bc])
        k.op("dve", lambda e: e.tensor_scalar(out=c0[:], in0=c0[:], scalar1=-1.0, scalar2=1.0, op0=ALU.mult, op1=ALU.add), r=[bc], w=[bc])

        with ExitStack() as ph:
            zb = [self.sb(ph, "rw_zb%d" % i, [128, T], F32) for i in range(2)]
            zs = [self.sb(ph, "rw_zs%d" % i, [128, T], F32) for i in range(2)]
            b_zb, b_zs = [Buf(), Buf()], [Buf(), Buf()]
            it = 0
            for b in range(NB):
                for f in range(15):
                    z, bz, o, bo = zb[it % 2], b_zb[it % 2], zs[it % 2], b_zs[it % 2]
                    it += 1
                    k.dma(None, z[:, :], self.ZT[b, f * 128:(f + 1) * 128, :], r=[self.b_ZT[b]], w=[bz])
                    k.op("act", lambda e: e.activation(out=o[:], in_=z[:], func=AF.Copy, scale=c0[:, f:f + 1]), r=[bz, bc], w=[bo])
                    for (lo, hi) in ((0, LC), (LC, T)):
                        k.op("dve", lambda e: e.scalar_tensor_tensor(out=o[:, lo + 1:hi], in0=z[:, lo:hi - 1], scalar=mu[:, f, 0:1],
                                                                     in1=o[:, lo + 1:hi], op0=ALU.mult, op1=ALU.add), r=[bz, bc, bo], w=[bo])
                        k.op("dve", lambda e: e.scalar_tensor_tensor(out=o[:, lo:hi - 1], in0=z[:, lo + 1:hi], scalar=mu[:, f, 1:2],
                                                                     in1=o[:, lo:hi - 1], op0=ALU.mult, op1=ALU.add), r=[bz, bc, bo], w=[bo])
                    k.dma(None, self.ZT[b, f * 128:(f + 1) * 128, :], o[:, :], r=[bo], w=[self.b_ZT[b]])
            k.barrier()

        RWP, VTK, YR = self.RWP, self.VTK, self.YR
        b_RWP, b_VTK, b_YR = Buf(), Buf(), Buf()
        with ExitStack() as ph:
            def t(name, shape, dt=F32):
                return self.sb(ph, "rwB_" + name, shape, dt)
            rT, kT, kk, aT, tmp, bv, ke, dec = (t(n, [128, T]) for n in ("rT", "kT", "kk", "aT", "tmp", "bv", "ke", "dec"))
            vT = t("vT", [128, T])
            vtk = t("vtk", [128, NBLK, 128], BF16)
            dwT = t("dwT", [64, 2, T], BF16)
            daT = t("daT", [64, 2, T], BF16)
            b_r, b_k, b_kk, b_a, b_tmp, b_bv, b_ke, b_dec, b_v, b_vtk, b_dw = (Buf() for _ in range(11))

            rev = [t("rev%d" % i, [128, T]) for i in range(2)]
            b_rev = [Buf(), Buf()]
            rcnt = [0]

            def store(arr, cc, src, bsrc, d):
                dst = RWP[arr, cc]
                if d == 0:
                    k.dma(None, dst[:, :], src[:, :], r=[bsrc], w=[b_RWP])
                else:
                    rv, brv = rev[rcnt[0] % 2], b_rev[rcnt[0] % 2]
                    rcnt[0] += 1
                    k.op("pool", lambda e: e.tensor_copy(out=rv[:, 0:LC], in_=src[:, rsl(LC - 1, LC)]), r=[bsrc], w=[brv])
                    k.op("pool", lambda e: e.tensor_copy(out=rv[:, LC:T], in_=src[:, rsl(T - 1, LL)]), r=[bsrc, brv], w=[brv])
                    k.dma(None, dst[:, :], rv[:, :], r=[brv], w=[b_RWP])

            for b in range(NB):
                for d in range(2):
                    k.dma("pool", dwT[:, d, :], self.ZT[b, 1536 + 64 * d:1600 + 64 * d, :], r=[self.b_ZT[b]], w=[b_dw])
                    k.dma("pool", daT[:, d, :], self.ZT[b, 1664 + 64 * d:1728 + 64 * d, :], r=[self.b_ZT[b]], w=[b_dw])
                k.op("act", lambda e: e.activation(out=dwT[:], in_=dwT[:], func=AF.Tanh), r=[b_dw], w=[b_dw])
                for hp in range(4):
                    k.dma(None, rT[:, :], self.ZT[b, 128 * hp:128 * (hp + 1), :], r=[self.b_ZT[b]], w=[b_r])
                    k.dma(None, kT[:, :], self.ZT[b, 512 + 128 * hp:512 + 128 * (hp + 1), :], r=[self.b_ZT[b]], w=[b_k])
                    k.dma(None, vT[:, :], self.ZT[b, 1024 + 128 * hp:1024 + 128 * (hp + 1), :], r=[self.b_ZT[b]], w=[b_v])
                    for n in range(NBLK):
                        pb, bpb = self.ps[n % 2], self.b_ps[n % 2]
                        k.op("pe", lambda e: e.transpose(out=pb[:, 0:128], in_=vT[:, n * 128:(n + 1) * 128], identity=self.ident[:]),
                             r=[b_v, self.b_ident], w=[bpb])
                        self.copy("act", vtk[:, n, :], pb[:, 0:128], r=[bpb], w=[b_vtk])
                    for hl in range(2):
                        k.dma(None, VTK[b, :, hl, hp, :].rearrange("(n p) v -> p n v", p=128), vtk[:, :, hl * 64:(hl + 1) * 64],
                              r=[b_vtk], w=[b_VTK])
                    k.op("dve", lambda e: e.tensor_scalar(out=kk[:], in0=kT[:], scalar1=pc["kk"][:, hp:hp + 1], scalar2=None, op0=ALU.mult),
                         r=[b_k, bc], w=[b_kk])
                    k.op("act", lambda e: e.activation(out=tmp[:], in_=kk[:], func=AF.Square), r=[b_kk], w=[b_tmp])
                    for ci, (t0, tn) in enumerate(segs):
                        pb, bpb = self.ps[2 + ci % 2], self.b_ps[2 + ci % 2]
                        k.op("pe", lambda e: e.matmul(pb[:, 0:tn], bones[:], tmp[:, t0:t0 + tn], start=True, stop=True), r=[bc, b_tmp], w=[bpb])
                        k.op("act", lambda e: e.activation(out=aT[:, t0:t0 + tn], in_=pb[:, 0:tn], func=AF.Sqrt), r=[bpb], w=[b_a])
                    k.op("dve", lambda e: e.tensor_scalar(out=aT[:], in0=aT[:], scalar1=1e-12, scalar2=None, op0=ALU.max), r=[b_a], w=[b_a])
                    k.op("dve", lambda e: e.reciprocal(out=aT[:], in_=aT[:]), r=[b_a], w=[b_a])
                    k.op("dve", lambda e: e.tensor_tensor(out=kk[:], in0=kk[:], in1=aT[:], op=ALU.mult), r=[b_kk, b_a], w=[b_kk])
                    k.op("pool", lambda e: e.tensor_scalar(out=tmp[:], in0=kk[:], scalar1=-1.0, scalar2=None, op0=ALU.mult), r=[b_kk], w=[b_tmp])
                    for d in range(2):
                        cc = col(b, d, hp)
                        store(0, cc, tmp, b_tmp, d)
                        store(4, cc, rT, b_r, d)
                        for ci, (t0, tn) in enumerate(segs):
                            pb, bpb = self.ps[4 + ci % 2], self.b_ps[4 + ci % 2]
                            k.op("pe", lambda e: e.matmul(pb[:, 0:tn], W2[:, d, hp * 128:(hp + 1) * 128], dwT[:, d, t0:t0 + tn],
                                                          start=True, stop=True), r=[bc, b_dw], w=[bpb])
                            k.op("act", lambda e: e.activation(out=dec[:, t0:t0 + tn], in_=pb[:, 0:tn], func=AF.Sigmoid,
                                                               bias=pc["w0"][:, d, hp:hp + 1]), r=[bpb, bc], w=[b_dec])
                            pb2, bpb2 = self.ps[6 + ci % 2], self.b_ps[6 + ci % 2]
                            k.op("pe", lambda e: e.matmul(pb2[:, 0:tn], A2[:, d, hp * 128:(hp + 1) * 128], daT[:, d, t0:t0 + tn],
                                                          start=True, stop=True), r=[bc, b_dw], w=[bpb2])
                            k.op("act", lambda e: e.activation(out=aT[:, t0:t0 + tn], in_=pb2[:, 0:tn], func=AF.Sigmoid,
                                                               bias=pc["a0"][:, d, hp:hp + 1]), r=[bpb2, bc], w=[b_a])
                        k.op("act", lambda e: e.activation(out=dec[:], in_=dec[:], func=AF.Exp, scale=-0.6065306597126334), r=[b_dec], w=[b_dec])
                        store(2, cc, dec, b_dec, d)
                        k.op("dve", lambda e: e.tensor_tensor(out=bv[:], in0=kk[:], in1=aT[:], op=ALU.mult), r=[b_kk, b_a], w=[b_bv])
                        store(1, cc, bv, b_bv, d)
                        k.op("dve", lambda e: e.tensor_scalar(out=ke[:], in0=aT[:], scalar1=pc["ka"][:, hp:hp + 1], scalar2=pc["omka"][:, hp:hp + 1],
                                                              op0=ALU.mult, op1=ALU.add), r=[b_a, bc], w=[b_ke])
                        k.op("pool", lambda e: e.tensor_tensor(out=ke[:], in0=ke[:], in1=kT[:], op=ALU.mult), r=[b_ke, b_k], w=[b_ke])
                        store(3, cc, ke, b_ke, d)
            k.barrier()

        with ExitStack() as ph:
            def t(name, shape, dt=F32):
                return self.sb(ph, "rwC_" + name, shape, dt)
            ST = t("ST", [128, NCOL, 64])
            X = t("X", [128, NCOL, 64])
            tmpb = [t("tmpb%d" % i, [128, NCOL, 64], BF16) for i in range(2)]
            t2 = [t("t2%d" % i, [128, NCOL, 64]) for i in range(2)]
            t3 = [t("t3%d" % i, [128, NCOL, 64]) for i in range(2)]
            vb = [t("vb%d" % i, [128, NCOL, 64]) for i in range(2)]
            tq = [t("tq%d" % i, [128, NCOL, 64], BF16) for i in range(2)]
            arr = [t("arr%d" % i, [128, 5, NCOL, TB]) for i in range(2)]
            vrow = [t("vrow%d" % i, [2, TV, NCOL, 64], BF16) for i in range(2)]
            yev = [t("yev%d" % i, [128, NCOL * 64]) for i in range(2)]
            b_ST, b_X = Buf(), Buf()
            b_tmpb, b_t2, b_t3, b_vb, b_tq, b_arr, b_vrow, b_yev = ([Buf(), Buf()] for _ in range(8))
            k.op("pool", lambda e: e.memset(ST[:], 0.0), w=[b_ST])
            NH = NCOL * 64 // 512
            assert NH <= 2
            for blk in range(T // TB):
                n0 = blk * TB
                A_, bA = arr[blk % 2], b_arr[blk % 2]
                for ai in range(5):
                    k.dma(None, A_[:, ai, :, :], RWP[ai, :, :, n0:n0 + TB].rearrange("c p n -> p c n"), r=[b_RWP], w=[bA])
                for nl in range(TB):
                    n = n0 + nl
                    i2 = n % 2
                    if nl % TV == 0:
                        vi_ = (n // TV) % 2
                        vr, bvr = vrow[vi_], b_vrow[vi_]
                        for b in range(NB):
                            for d in range(2):
                                if d == 0:
                                    src = VTK[b, n:n + TV]
                                elif n < LC:
                                    src = VTK[b, rsl(LC - 1 - n, TV)]
                                else:
                                    src = VTK[b, rsl(T - 1 + LC - n, TV)]
                                c0_ = (b * 2 + d) * 4
                                k.dma(None, vr[:, :, c0_:c0_ + 4, :].rearrange("hl t c v -> hl t (c v)"),
                                      src.rearrange("t hl hp v -> hl t (hp v)"), r=[b_VTK], w=[bvr])
                    vr, bvr = vrow[(n // TV) % 2], b_vrow[(n // TV) % 2]
                    nv = nl % TV

                    def bcs(ai):
                        return A_[:, ai, :, nl:nl + 1].to_broadcast([128, NCOL, 64])
                    for hh in range(NH):
                        pv, bpv = self.ps[2 + hh], self.b_ps[2 + hh]
                        k.op("pe", lambda e: e.matmul(pv[:, :], ind2[:, :], vr[:, nv, hh * 8:(hh + 1) * 8, :],
                                                      start=True, stop=True), r=[bc, bvr], w=[bpv])
                        k.op("dve", lambda e: e.tensor_tensor(out=t3[i2][:, hh * 8:(hh + 1) * 8, :],
                                                              in0=pv[:, :].rearrange("p (c v) -> p c v", v=64),
                                                              in1=A_[:, 3, hh * 8:(hh + 1) * 8, nl:nl + 1].to_broadcast([128, 8, 64]),
                                                              op=ALU.mult), r=[bpv, bA], w=[b_t3[i2]])
                    k.op("dve", lambda e: e.tensor_tensor(out=tmpb[i2][:], in0=ST[:], in1=bcs(0), op=ALU.mult),
                         r=[b_ST, bA], w=[b_tmpb[i2]])
                    for hh in range(NH):
                        psa, bpsa = self.ps[hh], self.b_ps[hh]
                        k.op("pe", lambda e: e.matmul(psa[:, :], bonesb[:, :], tmpb[i2][:, hh * 8:(hh + 1) * 8, :],
                                                      start=True, stop=True), r=[bc, b_tmpb[i2]], w=[bpsa])
                    k.op("pool", lambda e: e.tensor_tensor(out=X[:], in0=ST[:], in1=bcs(2), op=ALU.mult), r=[b_ST, bA], w=[b_X])
                    k.op("pool", lambda e: e.tensor_tensor(out=X[:], in0=X[:], in1=t3[i2][:], op=ALU.add), r=[b_X, b_t3[i2]], w=[b_X])
                    for hh in range(NH):
                        psa, bpsa = self.ps[hh], self.b_ps[hh]
                        k.op("dve", lambda e: e.tensor_tensor(out=t2[i2][:, hh * 8:(hh + 1) * 8, :],
                                                              in0=psa[:, :].rearrange("p (c v) -> p c v", v=64),
                                                              in1=A_[:, 1, hh * 8:(hh + 1) * 8, nl:nl + 1].to_broadcast([128, 8, 64]),
                                                              op=ALU.mult), r=[bpsa, bA], w=[b_t2[i2]])
                    k.op("dve", lambda e: e.tensor_tensor(out=ST[:], in0=X[:], in1=t2[i2][:], op=ALU.add), r=[b_X, b_t2[i2]], w=[b_ST])
                    k.op("pool", lambda e: e.tensor_tensor(out=tq[i2][:], in0=ST[:], in1=bcs(4), op=ALU.mult), r=[b_ST, bA], w=[b_tq[i2]])
                    for hh in range(NH):
                        py, bpy = self.ps[4 + hh], self.b_ps[4 + hh]
                        k.op("pe", lambda e: e.matmul(py[:, :], ZZ[:, 126 - 2 * nl:254 - 2 * nl], tq[i2][:, hh * 8:(hh + 1) * 8, :],
                                                      start=(nl == 0), stop=(nl == TB - 1)), r=[bc, b_tq[i2]], w=[bpy])
                ye, bye = yev[blk % 2], b_yev[blk % 2]
                for hh in range(NH):
                    py, bpy = self.ps[4 + hh], self.b_ps[4 + hh]
                    self.copy("act", ye[:, hh * 512:(hh + 1) * 512], py[:, :], r=[bpy], w=[bye])
                k.dma(None, YR[n0:n0 + TB, :, :].rearrange("n hl c -> (n hl) c"), ye[:, :], r=[bye], w=[b_YR])
            k.barrier()

        with ExitStack() as ph:
            def t(name, shape, dt=F32):
                return self.sb(ph, "rwD_" + name, shape, dt)
            yt = [t("yt%d" % i, [128, 2, 128]) for i in range(2)]
            yT, rT, kT, vT, m1, m2 = (t(n, [128, T]) for n in ("yT", "rT", "kT", "vT", "m1", "m2"))
            gl = t("gl", [128, T], BF16)
            b_yt = [Buf(), Buf()]
            b_y, b_r, b_k, b_v, b_m1, b_m2, b_gl = (Buf() for _ in range(7))
            it = 0
            for b in range(NB):
                k.dma("pool", gl[:, :], self.ZT[b, 1792:1920, :], r=[self.b_ZT[b]], w=[b_gl])
                k.op("act", lambda e: e.activation(out=gl[:], in_=gl[:], func=AF.Sigmoid), r=[b_gl], w=[b_gl])
                for hp in range(4):
                    k.dma(None, rT[:, :], self.ZT[b, 128 * hp:128 * (hp + 1), :], r=[self.b_ZT[b]], w=[b_r])
                    k.dma(None, kT[:, :], self.ZT[b, 512 + 128 * hp:512 + 128 * (hp + 1), :], r=[self.b_ZT[b]], w=[b_k])
                    k.dma(None, vT[:, :], self.ZT[b, 1024 + 128 * hp:1024 + 128 * (hp + 1), :], r=[self.b_ZT[b]], w=[b_v])
                    for n in range(NBLK):
                        y2, by2 = yt[it % 2], b_yt[it % 2]
                        it += 1
                        p0 = n * 128
                        c_f, c_b = col(b, 0, hp), col(b, 1, hp)
                        k.dma(None, y2[:, 0, :].rearrange("p (hl v) -> p hl v", hl=2),
                              YR[p0:p0 + 128, :, c_f * 64:(c_f + 1) * 64], r=[b_YR], w=[by2])
                        nb0 = (LC - 1 - p0) if p0 < LC else (T - 1 + LC - p0)
                        k.dma(None, y2[:, 1, :].rearrange("p (hl v) -> p hl v", hl=2),
                              YR[nb0 - 127:nb0 + 1, :, c_b * 64:(c_b + 1) * 64], r=[b_YR], w=[by2])
                        pb, bpb = self.ps[n % 4], self.b_ps[n % 4]
                        k.op("pe", lambda e: e.matmul(pb[:, 0:128], y2[:, 0, :], self.ident[:], start=True, stop=False),
                             r=[by2, self.b_ident], w=[bpb])
                        k.op("pe", lambda e: e.matmul(pb[:, 0:128], y2[:, 1, :], antiI[:], start=False, stop=True),
                             r=[by2, bc], w=[bpb])
                        self.copy("act" if n % 2 else "dve", yT[:, p0:p0 + 128], pb[:, 0:128], r=[bpb], w=[b_y])
                    for ci, (t0, tn) in enumerate(segs):
                        pb, bpb = self.ps[4 + ci % 2], self.b_ps[4 + ci % 2]
                        k.op("pe", lambda e: e.matmul(pb[:, 0:tn], bones[:], yT[:, t0:t0 + tn], start=True, stop=True), r=[bc, b_y], w=[bpb])
                        k.op("dve", lambda e: e.scalar_tensor_tensor(out=m1[:, t0:t0 + tn], in0=pb[:, 0:tn], scalar=-1.0 / 64,
                                                                     in1=yT[:, t0:t0 + tn], op0=ALU.mult, op1=ALU.add),
                             r=[bpb, b_y], w=[b_m1])
                    k.op("act", lambda e: e.activation(out=m2[:], in_=m1[:], func=AF.Square), r=[b_m1], w=[b_m2])
                    for ci, (t0, tn) in enumerate(segs):
                        pb, bpb = self.ps[6 + ci % 2], self.b_ps[6 + ci % 2]
                        k.op("pe", lambda e: e.matmul(pb[:, 0:tn], bones[:], m2[:, t0:t0 + tn], start=True, stop=True), r=[bc, b_m2], w=[bpb])
                        k.op("dve", lambda e: e.tensor_scalar(out=yT[:, t0:t0 + tn], in0=pb[:, 0:tn], scalar1=1.0 / 64, scalar2=64e-5,
                                                              op0=ALU.mult, op1=ALU.add), r=[bpb, b_y], w=[b_y])
                    k.op("act", lambda e: e.activation(out=yT[:], in_=yT[:], func=AF.Sqrt), r=[b_y], w=[b_y])
                    k.op("dve", lambda e: e.reciprocal(out=yT[:], in_=yT[:]), r=[b_y], w=[b_y])
                    k.op("dve", lambda e: e.tensor_tensor(out=m1[:], in0=m1[:], in1=yT[:], op=ALU.mult), r=[b_m1, b_y], w=[b_m1])
                    k.op("dve", lambda e: e.tensor_scalar(out=m1[:], in0=m1[:], scalar1=pc["lnw"][:, hp:hp + 1], scalar2=pc["lnb"][:, hp:hp + 1],
                                                          op0=ALU.mult, op1=ALU.add), r=[b_m1, bc], w=[b_m1])
                    k.op("dve", lambda e: e.scalar_tensor_tensor(out=m2[:], in0=rT[:], scalar=pc["rk"][:, hp:hp + 1], in1=kT[:],
                                                                  op0=ALU.mult, op1=ALU.mult), r=[b_r, b_k, bc, b_m2], w=[b_m2])
                    for ci, (t0, tn) in enumerate(segs):
                        pb, bpb = self.ps[4 + ci % 2], self.b_ps[4 + ci % 2]
                        k.op("pe", lambda e: e.matmul(pb[:, 0:tn], bones[:], m2[:, t0:t0 + tn], start=True, stop=True), r=[bc, b_m2], w=[bpb])
                        k.op("dve", lambda e: e.tensor_tensor(out=yT[:, t0:t0 + tn], in0=pb[:, 0:tn], in1=vT[:, t0:t0 + tn], op=ALU.mult),
                             r=[bpb, b_v, b_y], w=[b_y])
                    k.op("pool", lambda e: e.tensor_tensor(out=m1[:], in0=m1[:], in1=yT[:], op=ALU.add), r=[b_m1, b_y], w=[b_m1])
                    for ci, (t0, tn) in enumerate(segs):
                        pb, bpb = self.ps[6 + ci % 2], self.b_ps[6 + ci % 2]
                        k.op("pe", lambda e: e.matmul(pb[:, 0:tn], G2[:, hp * 128:(hp + 1) * 128], gl[:, t0:t0 + tn], start=True, stop=True),
                             r=[bc, b_gl], w=[bpb])
                        k.op("dve", lambda e: e.tensor_tensor(out=m2[:, t0:t0 + tn], in0=pb[:, 0:tn], in1=m1[:, t0:t0 + tn], op=ALU.mult),
                             r=[bpb, b_m1, b_m2], w=[b_m2])
                    k.dma("pool", self.OT[b, 128 * hp:128 * (hp + 1), :], m2[:, :], r=[b_m2], w=[self.b_OT[b]])
            k.barrier()


    def phase_outproj(self, l, lst, ctx_out):
        cfg, nc, k = self.cfg, self.nc, self.k
        NB, T, LC, D = cfg.NB, cfg.T, cfg.LC, cfg.D
        KT = D // 128
        R = NB + 1
        bc = Buf("opconsts")
        for r in range(R):
            k.dma(None, self.MODROW[l, r].rearrange("(j p) -> p j", p=128), self.modT[:, :, r], r=[self.b_mod_sb], w=[self.b_MODROW])
        WO = self.sb(lst, "op_WO", [128, KT, D], BF16)
        k.dma("pool", WO[:, :, :], self.inp_w_out[l].rearrange("(k p) c -> p k c", p=128), w=[bc])
        with ExitStack() as ph:
            OTs = self.sb(ph, "op_OT", [128, KT, T], BF16)
            G1 = self.sb(ph, "op_G1", [128, D], F32)
            xt = [self.sb(ph, "op_x%d" % i, [128, D], F32) for i in range(2)]
            tm = [self.sb(ph, "op_t%d" % i, [128, 512], F32) for i in range(2)]
            b_OTs, b_G1 = Buf(), Buf()
            b_x, b_tm = [Buf(), Buf()], [Buf(), Buf()]
            it = 0
            ev = 0
            for b in range(NB):
                k.dma(None, OTs[:, :, :], self.OT[b].rearrange("(j p) t -> p j t", p=128), r=[self.b_OT[b]], w=[b_OTs])
                src = self.xin if l == 0 else self.XT
                cur_r = None
                for tt in range(T // 128):
                    t0 = tt * 128
                    if t0 < LC and not ctx_out:
                        continue
                    r = NB if t0 < LC else b
                    if r != cur_r:
                        k.dma("sp", G1[:, :], self.MODROW[l, r, 2 * D:3 * D].partition_broadcast(128), r=[self.b_MODROW], w=[b_G1])
                        cur_r = r
                    x, bx = xt[it % 2], b_x[it % 2]
                    it += 1
                    k.dma(None, x[:, :], src[b, t0:t0 + 128, :], r=[self.b_XT[b]], w=[bx])
                    for c in range(D // 512):
                        pb, bpb = self.ps[ev % 4], self.b_ps[ev % 4]
                        tb, btb = tm[ev % 2], b_tm[ev % 2]
                        ev += 1
                        for kt in range(KT):
                            k.op("pe", lambda e: e.matmul(pb[:, :], OTs[:, kt, t0:t0 + 128], WO[:, kt, c * 512:(c + 1) * 512],
                                                          start=(kt == 0), stop=(kt == KT - 1)), r=[b_OTs, bc], w=[bpb])
                        k.op("dve", lambda e: e.tensor_tensor(out=tb[:], in0=pb[:, :], in1=G1[:, c * 512:(c + 1) * 512], op=ALU.mult),
                             r=[bpb, b_G1], w=[btb])
                        k.op("pool", lambda e: e.tensor_tensor(out=x[:, c * 512:(c + 1) * 512], in0=x[:, c * 512:(c + 1) * 512], in1=tb[:], op=ALU.add),
                             r=[btb, bx], w=[bx])
                    k.dma(None, self.XT[b, t0:t0 + 128, :], x[:, :], r=[bx], w=[self.b_XT[b]])
            k.barrier()

    def phase_moe(self, l, lst, ctx_out, last):
        cfg, nc, k = self.cfg, self.nc, self.k
        NB, T, LC, LL, D, NE = cfg.NB, cfg.T, cfg.LC, cfg.LL, cfg.D, cfg.NE
        KT = D // 128
        HT = D // 128
        bc = Buf("moeconsts")
        lo = 0 if ctx_out else LC
        ntk = T - lo
        nt_all = ntk // 128
        n_pass = -(-nt_all // 6)
        per = -(-nt_all // n_pass)
        passes = []
        tcur = 0
        while tcur < nt_all:
            n_ = min(per, nt_all - tcur)
            passes.append((lo + tcur * 128, n_ * 128))
            tcur += n_
        TGM = per * 128
        RW = self.sb(lst, "moe_RW", [128, KT, NE], BF16)
        k.dma("pool", RW[:, :, :], self.inp_rw_[l].rearrange("(k p) e -> p k e", p=128), w=[bc])
        RB = self.sb(lst, "moe_RB", [128, NE], F32)
        k.dma("sp", RB[:, :], self.inp_rb_[l].partition_broadcast(128), w=[bc])
        BGU = self.sb(lst, "moe_BGU", [128, NE, HT, 2], F32)
        for e_ in range(NE):
            k.dma(None, BGU[:, e_, :, :], self.inp_bgu[l, e_].rearrange("(h p two) -> p h two", p=128, two=2), w=[bc])
        BD = self.sb(lst, "moe_BD", [NE, D], F32)
        k.dma("sp", BD[:, :], self.inp_bd[l], w=[bc])
        identb = self.sb(lst, "moe_identb", [128, 128], BF16)
        self.copy("dve", identb[:], self.ident[:], r=[self.b_ident], w=[bc])
        with ExitStack() as ph:
            def t(name, shape, dt=F32):
                return self.sb(ph, "moe_" + name, shape, dt)
            X = t("X", [128, KT, TGM], BF16)
            Yacc = t("Yacc", [128, KT, TGM])
            act = t("act", [128, HT, TGM], BF16)
            GT = t("GT", [NE, TGM])
            GTb = t("GTb", [NE, TGM], BF16)
            L = t("L", [128, NE])
            E = t("E", [128, NE])
            m8 = t("m8", [128, 8])
            sm = t("sm", [128, 4])
            b_X, b_Y, b_act, b_GT, b_L = Buf(), Buf(), Buf(), Buf(), Buf()
            b_wgu, b_wdn, b_g1, b_u1, b_sg = ([Buf(), Buf()] for _ in range(5))
            for b in range(NB):
                for (p0, TG) in passes:
                    chunks = _chunks(TG, 512)
                    groups = []
                    for (c0, cn) in _chunks(TG, 256):
                        a0 = p0 + c0
                        a1 = a0 + cn
                        cuts = [a0] + ([LC] if a0 < LC < a1 else []) + [a1]
                        for ci in range(len(cuts) - 1):
                            s0, s1 = cuts[ci], cuts[ci + 1]
                            groups.append((self.XT[b, s0:s1, :], s1 - s0, NB if s0 < LC else b, s0 - p0, [self.b_XT[b]]))
                    self.norm_groups(X, b_X, groups, which=2, ntmax=2)
                    for tt in range(TG // 128):
                        pb, bpb = self.ps[tt % 2], self.b_ps[tt % 2]
                        for kt in range(KT):
                            k.op("pe", lambda e: e.matmul(pb[:, 0:NE], X[:, kt, tt * 128:(tt + 1) * 128], RW[:, kt, :],
                                                          start=(kt == 0), stop=(kt == KT - 1)), r=[b_X, bc], w=[bpb])
                        k.op("dve", lambda e: e.tensor_tensor(out=L[:], in0=pb[:, 0:NE], in1=RB[:], op=ALU.add), r=[bpb, bc], w=[b_L])
                        k.op("dve", lambda e: e.max(out=m8[:], in_=L[:]), r=[b_L], w=[b_L])
                        k.op("dve", lambda e: e.tensor_scalar(out=E[:], in0=L[:], scalar1=m8[:, 3:4], scalar2=None, op0=ALU.is_ge), r=[b_L], w=[b_L])
                        k.op("dve", lambda e: e.tensor_scalar(out=sm[:, 0:1], in0=m8[:, 0:1], scalar1=-1.0, scalar2=None, op0=ALU.mult), r=[b_L], w=[b_L])
                        k.op("act", lambda e: e.activation(out=L[:], in_=L[:], func=AF.Exp, bias=sm[:, 0:1]), r=[b_L], w=[b_L])
                        k.op("dve", lambda e: e.tensor_tensor(out=E[:], in0=E[:], in1=L[:], op=ALU.mult), r=[b_L], w=[b_L])
                        k.op("dve", lambda e: e.tensor_reduce(out=sm[:, 1:2], in_=E[:], axis=AX.X, op=ALU.add), r=[b_L], w=[b_L])
                        k.op("dve", lambda e: e.reciprocal(out=sm[:, 1:2], in_=sm[:, 1:2]), r=[b_L], w=[b_L])
                        k.op("dve", lambda e: e.tensor_scalar(out=E[:], in0=E[:], scalar1=sm[:, 1:2], scalar2=None, op0=ALU.mult), r=[b_L], w=[b_L])
                        pt, bpt = self.ps[2 + tt % 2], self.b_ps[2 + tt % 2]
                        k.op("pe", lambda e: e.transpose(out=pt[0:NE, 0:128], in_=E[:, :], identity=self.ident[:]), r=[b_L, self.b_ident], w=[bpt])
                        self.copy("act", GT[:, tt * 128:(tt + 1) * 128], pt[0:NE, 0:128], r=[bpt], w=[b_GT])
                        self.copy("dve", GTb[:, tt * 128:(tt + 1) * 128], pt[0:NE, 0:128], r=[bpt], w=[b_GT])
                    pa = ExitStack()
                    wgu = [self.sb(pa, "moe_wgu%d" % i, [128, KT, 512], BF16) for i in range(2)]
                    wdn = [self.sb(pa, "moe_wdn%d" % i, [128, HT, 256], BF16) for i in range(2)]
                    g1 = [self.sb(pa, "moe_g1%d" % i, [128, 512], F32) for i in range(2)]
                    u1 = [self.sb(pa, "moe_u1%d" % i, [128, 512], F32) for i in range(2)]
                    sg = [self.sb(pa, "moe_sg%d" % i, [128, 512], F32) for i in range(2)]
                    ev = 0
                    for dt in range(KT):
                        for (c0, cn) in chunks:
                            pb, bpb = self.ps[6 + ev % 2], self.b_ps[6 + ev % 2]
                            ev += 1
                            k.op("pe", lambda e: e.matmul(pb[:, 0:cn], BD[:, dt * 128:(dt + 1) * 128], GT[:, c0:c0 + cn], start=True, stop=True),
                                 r=[bc, b_GT], w=[bpb])
                            self.copy("act", Yacc[:, dt, c0:c0 + cn], pb[:, 0:cn], r=[bpb], w=[b_Y])
                    igu = 0
                    idn = 0
                    ie = 0
                    for e_ in range(NE):
                        wsrc = self.inp_wgu[l, e_].rearrange("(k p) c -> p k c", p=128)
                        for hq in range(HT // 2):
                            w, bw = wgu[igu % 2], b_wgu[igu % 2]
                            igu += 1
                            k.dma("pool", w[:, :, :], wsrc[:, :, hq * 512:(hq + 1) * 512], w=[bw])
                            for h2 in range(2):
                                ht = hq * 2 + h2
                                for (c0, cn) in chunks:
                                    i2 = ie % 2
                                    ie += 1
                                    pg, bpg = self.ps[0 + i2], self.b_ps[0 + i2]
                                    pu, bpu = self.ps[2 + i2], self.b_ps[2 + i2]
                                    pgb, bpgb = self.ps[4 + i2], self.b_ps[4 + i2]
                                    for kt in range(KT):
                                        k.op("pe", lambda e: e.matmul(pg[:, 0:cn], w[:, kt, h2 * 256:(h2 + 1) * 256:2], X[:, kt, c0:c0 + cn],
                                                                      start=(kt == 0), stop=(kt == KT - 1)), r=[bw, b_X], w=[bpg])
                                    for kt in range(KT):
                                        k.op("pe", lambda e: e.matmul(pu[:, 0:cn], w[:, kt, h2 * 256 + 1:(h2 + 1) * 256:2], X[:, kt, c0:c0 + cn],
                                                                      start=(kt == 0), stop=(kt == KT - 1)), r=[bw, b_X], w=[bpu])
                                    k.op("pe", lambda e: e.matmul(pgb[:, 0:cn], identb[0:NE, e_:e_ + 1].to_broadcast([NE, 128]), GTb[:, c0:c0 + cn],
                                                                  start=True, stop=True), r=[bc, b_GT], w=[bpgb])
                                    k.op("dve", lambda e: e.tensor_scalar(out=g1[i2][:, 0:cn], in0=pg[:, 0:cn], scalar1=BGU[:, e_, ht, 0:1], scalar2=7.0,
                                                                          op0=ALU.add, op1=ALU.min), r=[bpg, bc], w=[b_g1[i2]])
                                    k.op("act", lambda e: e.activation(out=sg[i2][:, 0:cn], in_=g1[i2][:, 0:cn], func=AF.Sigmoid, scale=1.702),
                                         r=[b_g1[i2]], w=[b_sg[i2]])
                                    k.op("dve", lambda e: e.tensor_scalar(out=u1[i2][:, 0:cn], in0=pu[:, 0:cn], scalar1=BGU[:, e_, ht, 1:2], scalar2=7.0,
                                                                          op0=ALU.add, op1=ALU.min), r=[bpu, bc], w=[b_u1[i2]])
                                    k.op("pool", lambda e: e.tensor_scalar(out=u1[i2][:, 0:cn], in0=u1[i2][:, 0:cn], scalar1=-7.0, scalar2=1.0,
                                                                           op0=ALU.max, op1=ALU.add), r=[b_u1[i2]], w=[b_u1[i2]])
                                    k.op("pool", lambda e: e.tensor_tensor(out=g1[i2][:, 0:cn], in0=g1[i2][:, 0:cn], in1=sg[i2][:, 0:cn], op=ALU.mult),
                                         r=[b_g1[i2], b_sg[i2]], w=[b_g1[i2]])
                                    k.op("pool", lambda e: e.tensor_tensor(out=g1[i2][:, 0:cn], in0=g1[i2][:, 0:cn], in1=u1[i2][:, 0:cn], op=ALU.mult),
                                         r=[b_g1[i2], b_u1[i2]], w=[b_g1[i2]])
                                    k.op("dve", lambda e: e.tensor_tensor(out=act[:, ht, c0:c0 + cn], in0=pgb[:, 0:cn], in1=g1[i2][:, 0:cn], op=ALU.mult),
                                         r=[bpgb, b_g1[i2]], w=[b_act])
                        dsrc = self.inp_wd[l, e_].rearrange("(h p) c -> p h c", p=128)
                        for dq in range(D // 256):
                            w, bw = wdn[idn % 2], b_wdn[idn % 2]
                            idn += 1
                            k.dma("pool", w[:, :, :], dsrc[:, :, dq * 256:(dq + 1) * 256], w=[bw])
                            for d2 in range(2):
                                dt = dq * 2 + d2
                                for (c0, cn) in chunks:
                                    pb, bpb = self.ps[6 + ev % 2], self.b_ps[6 + ev % 2]
                                    ev += 1
                                    for ht in range(HT):
                                        k.op("pe", lambda e: e.matmul(pb[:, 0:cn], w[:, ht, d2 * 128:(d2 + 1) * 128], act[:, ht, c0:c0 + cn],
                                                                      start=(ht == 0), stop=(ht == HT - 1)), r=[bw, b_act], w=[bpb])
                                    k.op("dve", lambda e: e.tensor_tensor(out=Yacc[:, dt, c0:c0 + cn], in0=pb[:, 0:cn], in1=Yacc[:, dt, c0:c0 + cn], op=ALU.add),
                                         r=[bpb, b_Y], w=[b_Y])
                    k.barrier()
                    pa.close()
                    pa = ExitStack()
                    self.moe_x = [self.sb(pa, "moe_x%d" % i, [128, D], F32) for i in range(2)]
                    self.b_moe_x = [Buf(), Buf()]
                    for tt in range(TG // 128):
                        s0 = p0 + tt * 128
                        r = NB if s0 < LC else b
                        xt_, bxt = self.moe_x[tt % 2], self.b_moe_x[tt % 2]
                        k.dma(None, xt_[:, :], self.XT[b, s0:s0 + 128, :], r=[self.b_XT[b]], w=[bxt])
                        for dt in range(KT):
                            k.op("pool", lambda e: e.tensor_scalar(out=Yacc[:, dt, tt * 128:(tt + 1) * 128], in0=Yacc[:, dt, tt * 128:(tt + 1) * 128],
                                                                   scalar1=self.modT[:, 5 * KT + dt, r:r + 1], scalar2=None, op0=ALU.mult),
                                 r=[b_Y, self.b_mod_sb], w=[b_Y])
                        for c in range(KT // 4):
                            pb, bpb = self.ps[c % 4], self.b_ps[c % 4]
                            for j in range(4):
                                dt = c * 4 + j
                                k.op("pe", lambda e: e.transpose(out=pb[:, j * 128:(j + 1) * 128], in_=Yacc[:, dt, tt * 128:(tt + 1) * 128],
                                                                 identity=self.ident[:]), r=[b_Y, self.b_ident], w=[bpb])
                            k.op("dve", lambda e: e.tensor_tensor(out=xt_[:, c * 512:(c + 1) * 512], in0=pb[:, :], in1=xt_[:, c * 512:(c + 1) * 512], op=ALU.add),
                                 r=[bpb, bxt], w=[bxt])
                        if last:
                            k.dma(None, self.yout[b, s0 - LC:s0 - LC + 128, :], xt_[:, :], r=[bxt], w=[self.b_yout])
                        else:
                            k.dma(None, self.XT[b, s0:s0 + 128, :], xt_[:, :], r=[bxt], w=[self.b_XT[b]])
                    k.barrier()
                    pa.close()

def host_constants(cfg):
    c = {}
    c["ident"] = np.eye(128, dtype=np.float32)
    f32 = np.float32
    LL = cfg.LL
    rows = LL // cfg.GRID_W
    row = np.repeat(np.arange(rows), cfg.GRID_W).astype(f32)
    col = (np.arange(rows * cfg.GRID_W) % cfg.GRID_W).astype(f32)
    inv_freq = (f32(10000.0) ** (-np.arange(16, dtype=f32) / f32(16))).astype(f32)
    ang_r = (row[:, None] * inv_freq).astype(f32)
    ang_c = (col[:, None] * inv_freq).astype(f32)
    cos = np.concatenate([np.cos(ang_r), np.cos(ang_r), np.cos(ang_c), np.cos(ang_c)], axis=1)
    sin = np.concatenate([np.sin(ang_r), np.sin(ang_r), np.sin(ang_c), np.sin(ang_c)], axis=1)
    c["c_cos"] = np.ascontiguousarray(cos.T.astype(f32))
    c["c_sin"] = np.ascontiguousarray(sin.T.astype(f32))
    Rm = np.zeros((64, 64), f32)
    for m in range(64):
        if (m % 32) < 16:
            Rm[m, m + 16] = -1.0
        else:
            Rm[m, m - 16] = 1.0
    c["c_rot"] = np.ascontiguousarray(Rm.T)
    ind2 = np.zeros((2, 128), f32)
    ind2[0, 0:64] = 1.0
    ind2[1, 64:128] = 1.0
    c["c_ind2"] = ind2
    c["c_antiI"] = np.ascontiguousarray(np.eye(128, dtype=f32)[::-1])
    j = np.arange(128)[:, None]
    i = np.arange(128)[None, :]
    c["c_mprev"] = (j >= i).astype(f32)
    c["c_mnext"] = (j <= i).astype(f32)
    return c


def make_in_maps(cfg, inputs):
    NB = cfg.NB
    consts = host_constants(cfg)
    maps = []
    x = np.asarray(inputs["x"], np.float32)
    ctx = np.asarray(inputs["ctx"], np.float32)
    c = np.asarray(inputs["c"], np.float32)
    c_ctx = np.asarray(inputs["c_ctx"], np.float32)
    shared = {}
    for name in ("norm1_g", "norm2_g", "w_mod", "b_mod", "w_in", "at_q_norm", "at_k_norm", "at_sink",
                 "s5_lam_re", "s5_lam_im", "s5_log_step", "s5_b_re", "s5_b_im", "s5_c_re", "s5_c_im", "s5_d",
                 "s5_glu_w", "s5_glu_b", "hg_f_bias", "hg_lb_logits", "hg_norm_g",
                 "w_out", "moe_router_w", "moe_router_b", "moe_w_gu", "moe_b_gu", "moe_w_down", "moe_b_down",
                 "rw_mu", "rw_w0", "rw_w2", "rw_a0", "rw_a2", "rw_g2", "rw_k_k", "rw_k_a", "rw_ln_w", "rw_ln_b"):
        shared[name] = np.ascontiguousarray(np.asarray(inputs[name], np.float32))
    shared["rw_r_k"] = np.ascontiguousarray(np.asarray(inputs["rw_r_k"], np.float32).reshape(cfg.DEPTH, 512))
    shared.update(consts)
    for i in range(cfg.NCORES):
        sl = slice(i * NB, (i + 1) * NB)
        m = dict(shared)
        m["xin"] = np.ascontiguousarray(np.concatenate([ctx[sl], x[sl]], axis=1))
        m["cvec"] = np.ascontiguousarray(np.concatenate([c[sl], c_ctx[None]], axis=0))
        maps.append(m)
    return maps


_CACHE = {}


def kernel(**inputs):
    cfg = Cfg()
    prog = Prog(cfg)
    nc = prog.build()
    maps = make_in_maps(cfg, inputs)
    res = run_bass_kernel_spmd(nc, maps, core_ids=list(range(cfg.NCORES)))
    outs = [r["y"] for r in res.results]
    return np.concatenate(outs, axis=0)
```

```python
import math
from contextlib import ExitStack

import numpy as np
import concourse.bass as bass
import concourse.mybir as mybir
from concourse.bass_utils import run_bass_kernel_spmd

F32 = mybir.dt.float32
BF16 = mybir.dt.bfloat16
AF = mybir.ActivationFunctionType
ALU = mybir.AluOpType
AX = mybir.AxisListType


class Cfg:
    NB = 2
    LC = 256
    LL = 2048
    D = 2048
    DEPTH = 2
    NE = 32
    NCORES = 8
    GRID_W = 64
    debug = ()
    phases = ("attn", "s5", "hg", "rw", "moe")

    @property
    def T(self):
        return self.LC + self.LL


GW = 512
RW_COLS = 1920
S5_COLS = 512
HG_COLS = 2560
AT_COLS = 768
IN_COLS = RW_COLS + S5_COLS + HG_COLS + AT_COLS
C_RW = 0
C_S5 = RW_COLS
C_HG = C_S5 + S5_COLS
C_AT = C_HG + HG_COLS
NORM_EPS = 1e-6


class Dom:
    def __init__(self, name, sem, mult):
        self.name, self.sem, self.mult, self.count = name, sem, mult, 0


class Buf:
    __slots__ = ("w", "r", "name")

    def __init__(self, name=""):
        self.w = None
        self.r = {}
        self.name = name


class K:
    def __init__(self, nc, stack, n_lanes=40):
        self.nc = nc
        self.eng = {"pe": nc.tensor, "act": nc.scalar, "dve": nc.vector,
                    "pool": nc.gpsimd, "sp": nc.sync}
        self.dom = {n: Dom(n, stack.enter_context(nc.semaphore("s_" + n)), 1)
                    for n in self.eng}
        self.lanes = [Dom("l%d" % i, stack.enter_context(nc.semaphore("l%d" % i)), 16)
                      for i in range(n_lanes)]
        self.waited = {n: {} for n in self.eng}
        self.rr = 0
        self.qrr = 0
        self.ninst = 0

    def _wait(self, e, d, c):
        if c <= 0:
            return
        if self.waited[e].get(d.name, 0) >= c:
            return
        self.eng[e].wait_ge(d.sem, c * d.mult)
        self.waited[e][d.name] = c

    def _deps(self, e, r, w, extra=()):
        need = {}

        def add(dc):
            d, c = dc
            if need.get(d, 0) < c:
                need[d] = c
        for b in r:
            if b.w is not None:
                add(b.w)
        for b in w:
            if b.w is not None:
                add(b.w)
            for d, c in b.r.items():
                add((d, c))
        for dc in extra:
            add(dc)
        for d, c in need.items():
            if e == "pe" and d.name == "pe":
                continue
            self._wait(e, d, c)

    def op(self, e, fn, r=(), w=()):
        self._deps(e, r, w)
        ins = fn(self.eng[e])
        d = self.dom[e]
        d.count += 1
        ins.then_inc(d.sem, 1)
        for b in r:
            b.r[d] = d.count
        for b in w:
            b.w = (d, d.count)
            b.r = {}
        self.ninst += 1
        return ins

    def dma(self, q, out, in_, r=(), w=(), **kw):
        if q is None:
            q = ("sp", "act")[self.qrr % 2]
            self.qrr += 1
        lane = self.lanes[self.rr]
        self.rr = (self.rr + 1) % len(self.lanes)
        self._deps(q, r, w, extra=[(lane, lane.count)])
        ins = self.eng[q].dma_start(out=out, in_=in_, **kw)
        lane.count += 1
        ins.then_inc(lane.sem, 16)
        for b in r:
            b.r[lane] = lane.count
        for b in w:
            b.w = (lane, lane.count)
            b.r = {}
        self.ninst += 1
        return ins

    def barrier(self, engines=("pe", "act", "dve", "pool", "sp")):
        doms = list(self.dom.values()) + self.lanes
        for e in engines:
            for d in doms:
                if d.name == e:
                    continue
                self._wait(e, d, d.count)

    def finish(self):
        self.barrier(engines=("sp", "pool", "act", "dve", "pe"))


def _chunks(total, size):
    out = []
    s = 0
    while s < total:
        out.append((s, min(size, total - s)))
        s += size
    return out


class Prog:
    def __init__(self, cfg):
        self.cfg = cfg
        self.nc = bass.Bass("TRN2", target_bir_lowering=False)
        self.stack = ExitStack()
        self.k = None
        self.din = {}
        self.dbg = {}

    def inp(self, name, shape):
        t = self.nc.dram_tensor(name, list(shape), F32, kind="ExternalInput").ap()
        self.din[name] = t
        return t

    def scratch(self, name, shape, dt=F32):
        return self.nc.dram_tensor(name, list(shape), dt).ap()

    def out(self, name, shape, dt=F32):
        return self.nc.dram_tensor(name, list(shape), dt, kind="ExternalOutput").ap()

    def sb(self, st, name, shape, dt=F32):
        self._uid = getattr(self, "_uid", 0) + 1
        return st.enter_context(self.nc.sbuf_tensor("%s_%d" % (name, self._uid), list(shape), dt))

    def build(self):
        cfg, nc = self.cfg, self.nc
        NB, T, D, DEPTH, NE = cfg.NB, cfg.T, cfg.D, cfg.DEPTH, cfg.NE
        R = NB + 1
        st = self.stack
        st.enter_context(nc.allow_non_contiguous_dma(reason="layout"))
        st.enter_context(nc.allow_low_precision(reason="bf16 matmul operands"))
        self.k = k = K(nc, st)
        self.xin = self.inp("xin", [NB, T, D])
        self.cvec = self.inp("cvec", [R, D])
        self.norm1 = self.inp("norm1_g", [DEPTH, D])
        self.norm2 = self.inp("norm2_g", [DEPTH, D])
        self.w_mod = self.inp("w_mod", [DEPTH, D, 6 * D])
        self.b_mod = self.inp("b_mod", [DEPTH, 6 * D])
        self.w_in = self.inp("w_in", [DEPTH, D, IN_COLS])
        self.ident_d = self.inp("ident", [128, 128])
        self.inp_at_q = self.inp("at_q_norm", [DEPTH, 64])
        self.inp_at_k = self.inp("at_k_norm", [DEPTH, 64])
        self.inp_at_sink = self.inp("at_sink", [DEPTH, 8])
        self.inp_s5_lre = self.inp("s5_lam_re", [DEPTH, 2, 32, 64])
        self.inp_s5_lim = self.inp("s5_lam_im", [DEPTH, 2, 32, 64])
        self.inp_s5_ls = self.inp("s5_log_step", [DEPTH, 2, 32])
        self.inp_s5_bre = self.inp("s5_b_re", [DEPTH, 2, 32, 64, 16])
        self.inp_s5_bim = self.inp("s5_b_im", [DEPTH, 2, 32, 64, 16])
        self.inp_s5_cre = self.inp("s5_c_re", [DEPTH, 2, 32, 16, 64])
        self.inp_s5_cim = self.inp("s5_c_im", [DEPTH, 2, 32, 16, 64])
        self.inp_s5_d = self.inp("s5_d", [DEPTH, 512])
        self.inp_s5_glu_w = self.inp("s5_glu_w", [DEPTH, 512, 512])
        self.inp_s5_glu_b = self.inp("s5_glu_b", [DEPTH, 512])
        self.inp_hg_fb = self.inp("hg_f_bias", [DEPTH, 2, 512])
        self.inp_hg_lbl = self.inp("hg_lb_logits", [DEPTH, 512])
        self.inp_hg_ng = self.inp("hg_norm_g", [DEPTH, 128])
        self.inp_rw_mu = self.inp("rw_mu", [DEPTH, 2, 1920])
        self.inp_rw_w0 = self.inp("rw_w0", [DEPTH, 2, 512])
        self.inp_rw_w2 = self.inp("rw_w2", [DEPTH, 2, 64, 512])
        self.inp_rw_a0 = self.inp("rw_a0", [DEPTH, 2, 512])
        self.inp_rw_a2 = self.inp("rw_a2", [DEPTH, 2, 64, 512])
        self.inp_rw_g2 = self.inp("rw_g2", [DEPTH, 128, 512])
        self.inp_rw_kk = self.inp("rw_k_k", [DEPTH, 512])
        self.inp_rw_ka = self.inp("rw_k_a", [DEPTH, 512])
        self.inp_rw_rk = self.inp("rw_r_k", [DEPTH, 512])
        self.inp_rw_lnw = self.inp("rw_ln_w", [DEPTH, 512])
        self.inp_rw_lnb = self.inp("rw_ln_b", [DEPTH, 512])
        self.c_ind2 = self.inp("c_ind2", [2, 128])
        self.c_antiI = self.inp("c_antiI", [128, 128])
        self.RWP = self.scratch("RWP", [5, NB * 8, 128, T])
        self.VTK = self.scratch("VTK", [NB, T, 2, 4, 64], BF16)
        self.YR = self.scratch("YR", [T, 2, NB * 8 * 64])
        self.inp_w_out = self.inp("w_out", [DEPTH, D, D])
        self.inp_rw_ = self.inp("moe_router_w", [DEPTH, D, NE])
        self.inp_rb_ = self.inp("moe_router_b", [DEPTH, NE])
        self.inp_wgu = self.inp("moe_w_gu", [DEPTH, NE, D, 2 * D])
        self.inp_bgu = self.inp("moe_b_gu", [DEPTH, NE, 2 * D])
        self.inp_wd = self.inp("moe_w_down", [DEPTH, NE, D, D])
        self.inp_bd = self.inp("moe_b_down", [DEPTH, NE, D])
        self.MODROW = self.scratch("MODROW", [DEPTH, R, 6 * D])
        self.b_MODROW = Buf("MODROW")
        self.yout = self.out("y", [NB, cfg.LL, D])
        self.b_yout = Buf("yout")
        self.c_cos = self.inp("c_cos", [64, cfg.LL])
        self.c_sin = self.inp("c_sin", [64, cfg.LL])
        self.c_rot = self.inp("c_rot", [64, 64])
        self.c_mprev = self.inp("c_mprev", [128, 128])
        self.c_mnext = self.inp("c_mnext", [128, 128])
        self.ident = self.sb(st, "ident_sb", [128, 128], F32)
        self.b_ident = Buf("ident")
        k.dma("sp", self.ident[:], self.ident_d[:, :], w=[self.b_ident])
        self.ps = [st.enter_context(nc.psum_tensor("psb%d" % i, [128, 512], F32)) for i in range(8)]
        self.b_ps = [Buf("ps%d" % i) for i in range(8)]
        self.XT = self.scratch("XT", [NB, T, D])
        self.ZT = self.scratch("ZT", [NB, IN_COLS, T])
        self.b_XT = [Buf("XT%d" % b) for b in range(NB)]
        self.b_ZT = [Buf("ZT%d" % b) for b in range(NB)]
        if "ot" in cfg.debug:
            self.OT = self.out("dbg_ot", [NB, D, T], BF16)
        else:
            self.OT = self.scratch("OT", [NB, D, T], BF16)
        self.b_OT = [Buf("OT%d" % b) for b in range(NB)]
        if "zt" in cfg.debug:
            self.dbg["zt"] = self.out("dbg_zt", [NB, IN_COLS, T])
        if "mod" in cfg.debug:
            self.dbg["mod"] = self.out("dbg_mod", [128, 96 * R])

        for l in range(DEPTH):
            self.layer(l)
            if cfg.debug and "full" not in cfg.debug:
                break
        k.finish()
        return nc

    def layer(self, l):
        cfg, nc, k = self.cfg, self.nc, self.k
        NB = cfg.NB
        ctx_out = l < cfg.DEPTH - 1 or (bool(cfg.debug) and "full" not in cfg.debug)
        with ExitStack() as lst:
            self.phase_mod(l, lst)
            for b in range(NB):
                with ExitStack() as bst:
                    src = self.xin if l == 0 else self.XT
                    self.phase_norm_T(l, b, bst, src, which=1)
                    self.phase_inproj(l, b, bst)
                    k.barrier()
            if "attn" in cfg.phases:
                for b in range(NB):
                    self.phase_attn(l, b, ctx_out)
            if "s5" in cfg.phases:
                with ExitStack() as st2:
                    self.phase_s5_prep(l, st2)
                    for b in range(NB):
                        self.phase_s5(l, b)
                    k.barrier()
            if "rw" in cfg.phases:
                with ExitStack() as st2:
                    self.phase_rw(l, st2)
                    k.barrier()
            if "hg" in cfg.phases:
                with ExitStack() as st2:
                    self.phase_hg_prep(l, st2)
                    for b in range(NB):
                        self.phase_hg(l, b)
                    k.barrier()
            k.barrier()
            if "moe" in cfg.phases:
                ctx_o = l < cfg.DEPTH - 1
                with ExitStack() as st2:
                    self.phase_outproj(l, st2, ctx_o)
                    k.barrier()
                with ExitStack() as st2:
                    self.phase_moe(l, st2, ctx_o, l == cfg.DEPTH - 1)
                    k.barrier()

    def phase_mod(self, l, lst):
        cfg, nc, k = self.cfg, self.nc, self.k
        NB, D = cfg.NB, cfg.D
        R = NB + 1
        KT = D // 128
        self.modT = self.sb(lst, "modT", [128, 6 * KT, R], F32)
        self.sc1e = self.sb(lst, "sc1e", [128, KT, R], F32)
        self.sc2e = self.sb(lst, "sc2e", [128, KT, R], F32)
        self.b_mod_sb = Buf("modT")
        b_modT = self.b_mod_sb
        with ExitStack() as ph:
            cT = self.sb(ph, "cT", [128, KT, R], F32)
            bm = self.sb(ph, "bm", [128, 6 * KT], F32)
            g1 = self.sb(ph, "g1", [128, KT], F32)
            g2 = self.sb(ph, "g2", [128, KT], F32)
            wm = [self.sb(ph, "wm%d" % i, [128, KT, 512], F32) for i in range(2)]
            b_cT, b_bm, b_g = Buf(), Buf(), Buf()
            b_wm = [Buf(), Buf()]
            for r in range(R):
                k.dma("sp", cT[:, :, r], self.cvec[r].rearrange("(k p) -> p k", p=128), w=[b_cT])
            k.dma("sp", bm[:], self.b_mod[l].rearrange("(j p) -> p j", p=128), w=[b_bm])
            k.dma("sp", g1[:], self.norm1[l].rearrange("(j p) -> p j", p=128), w=[b_g])
            k.dma("sp", g2[:], self.norm2[l].rearrange("(j p) -> p j", p=128), w=[b_g])
            k.op("act", lambda e: e.activation(out=cT[:], in_=cT[:], func=AF.Silu), r=[b_cT], w=[b_cT])
            wsrc = self.w_mod[l].rearrange("(k p) c -> p k c", p=128)
            nch = 6 * D // 512
            for ch in range(nch):
                wb, bwb = wm[ch % 2], b_wm[ch % 2]
                k.dma(None, wb[:], wsrc[:, :, ch * 512:(ch + 1) * 512], w=[bwb])
                pb, bpb = self.ps[ch % 2], self.b_ps[ch % 2]
                for j in range(4):
                    for kt in range(KT):
                        k.op("pe", lambda e, j=j, kt=kt: e.matmul(
                            pb[:, j * R:(j + 1) * R], wb[:, kt, j * 128:(j + 1) * 128], cT[:, kt, :],
                            start=(kt == 0), stop=(kt == KT - 1)), r=[bwb, b_cT], w=[bpb])
                k.op("dve", lambda e, ch=ch: e.tensor_tensor(
                    out=self.modT[:, ch * 4:(ch + 1) * 4, :],
                    in0=pb[:, 0:4 * R].rearrange("p (j r) -> p j r", r=R),
                    in1=bm[:, ch * 4:(ch + 1) * 4].unsqueeze(2).to_broadcast([128, 4, R]),
                    op=ALU.add), r=[bpb, b_bm], w=[b_modT])
            for (dst, g, off) in ((self.sc1e, g1, KT), (self.sc2e, g2, 4 * KT)):
                k.op("dve", lambda e, dst=dst, g=g, off=off: e.scalar_tensor_tensor(
                    out=dst[:], in0=self.modT[:, off:off + KT, :], scalar=1.0,
                    in1=g[:].unsqueeze(2).to_broadcast([128, KT, R]),
                    op0=ALU.add, op1=ALU.mult), r=[b_modT, b_g], w=[b_modT])
            if "mod" in cfg.debug:
                k.dma("sp", self.dbg["mod"][:, :], self.modT[:].rearrange("p j r -> p (j r)"), r=[b_modT])
            k.barrier()

    def phase_norm_T(self, l, b, bst, src, which):
        cfg = self.cfg
        NB, D, T, LC = cfg.NB, cfg.D, cfg.T, cfg.LC
        KT = D // 128
        self.hT = self.sb(bst, "hT", [128, KT, T], BF16)
        self.b_hT = Buf("hT")
        groups = [(src[b, t0:t0 + n, :], n, NB, t0, [self.b_XT[b]]) for (t0, n) in _chunks(LC, 512)] + \
                 [(src[b, LC + t0:LC + t0 + n, :], n, b, LC + t0, [self.b_XT[b]]) for (t0, n) in _chunks(T - LC, 512)]
        self.norm_groups(self.hT, self.b_hT, groups, which)

    def norm_groups(self, dst, b_dst, groups, which, ntmax=4):
        cfg, nc, k = self.cfg, self.nc, self.k
        D = cfg.D
        KT = D // 128
        sce = self.sc1e if which == 1 else self.sc2e
        sh_off = 0 if which == 1 else 3 * KT
        with ExitStack() as ph:
            xt = [self.sb(ph, "xt%d" % i, [128, ntmax, D], F32) for i in range(2)]
            junk = self.sb(ph, "junk", [128, D], BF16)
            ss = self.sb(ph, "ss", [128, 8], F32)
            b_xt = [Buf(), Buf()]
            b_junk, b_ss = Buf(), Buf()
            ev = 0
            for gi, (src_ap, ntok, r, t0, rb) in enumerate(groups):
                nt = ntok // 128
                x, bx = xt[gi % 2], b_xt[gi % 2]
                k.dma(None, x[:, 0:nt, :], src_ap.rearrange("(n p) d -> p n d", p=128), r=rb, w=[bx])
                so = (gi % 2) * 4
                for n in range(nt):
                    k.op("act", lambda e, n=n: e.activation(
                        out=junk[:], in_=x[:, n, :], func=AF.Square, accum_out=ss[:, so + n:so + n + 1]),
                        r=[bx], w=[b_junk, b_ss])
                k.op("dve", lambda e: e.tensor_scalar(
                    out=ss[:, so:so + nt], in0=ss[:, so:so + nt], scalar1=1.0 / D, scalar2=NORM_EPS,
                    op0=ALU.mult, op1=ALU.add), r=[b_ss], w=[b_ss])
                k.op("act", lambda e: e.activation(
                    out=ss[:, so:so + nt], in_=ss[:, so:so + nt], func=AF.Sqrt), r=[b_ss], w=[b_ss])
                k.op("dve", lambda e: e.reciprocal(
                    out=ss[:, so:so + nt], in_=ss[:, so:so + nt]), r=[b_ss], w=[b_ss])
                for n in range(nt):
                    k.op("pool", lambda e, n=n: e.tensor_scalar(
                        out=x[:, n, :], in0=x[:, n, :], scalar1=ss[:, so + n:so + n + 1], scalar2=None,
                        op0=ALU.mult), r=[bx, b_ss], w=[bx])
                for kt in range(KT):
                    pi = kt % 4
                    pb, bpb = self.ps[pi], self.b_ps[pi]
                    for n in range(nt):
                        k.op("pe", lambda e, n=n, kt=kt: e.transpose(
                            out=pb[:, n * 128:(n + 1) * 128], in_=x[:, n, kt * 128:(kt + 1) * 128],
                            identity=self.ident[:]), r=[bx, self.b_ident], w=[bpb])
                    if ev % 2 == 0:
                        k.op("dve", lambda e, kt=kt: e.tensor_scalar(
                            out=dst[:, kt, t0:t0 + ntok], in0=pb[:, 0:ntok],
                            scalar1=sce[:, kt, r:r + 1], scalar2=self.modT[:, sh_off + kt, r:r + 1],
                            op0=ALU.mult, op1=ALU.add), r=[bpb, self.b_mod_sb], w=[b_dst])
                    else:
                        k.op("act", lambda e, kt=kt: e.activation(
                            out=dst[:, kt, t0:t0 + ntok], in_=pb[:, 0:ntok], func=AF.Identity,
                            scale=sce[:, kt, r:r + 1], bias=self.modT[:, sh_off + kt, r:r + 1]),
                            r=[bpb, self.b_mod_sb], w=[b_dst])
                    ev += 1
            k.barrier()

    def phase_inproj(self, l, b, bst):
        cfg, nc, k = self.cfg, self.nc, self.k
        D, T = cfg.D, cfg.T
        KT = D // 128
        with ExitStack() as ph:
            wb = [self.sb(ph, "wi%d" % i, [128, KT, 512], BF16) for i in range(2)]
            zb = [self.sb(ph, "zb%d" % i, [128, T], F32) for i in range(2)]
            b_wb = [Buf(), Buf()]
            b_zb = [Buf(), Buf()]
            wsrc = self.w_in[l].rearrange("(k p) c -> p k c", p=128)
            zi = 0
            ev = 0
            for ci, (c0, cw) in enumerate(_chunks(IN_COLS, 512)):
                w, bw = wb[ci % 2], b_wb[ci % 2]
                k.dma("pool", w[:, :, 0:cw], wsrc[:, :, c0:c0 + cw], w=[bw])
                for ct in range(cw // 128):
                    z, bz = zb[zi % 2], b_zb[zi % 2]
                    zi += 1
                    for (t0, tn) in _chunks(T, 512):
                        pi = 4 + ev % 4
                        pb, bpb = self.ps[pi], self.b_ps[pi]
                        for kt in range(KT):
                            k.op("pe", lambda e, kt=kt: e.matmul(
                                pb[:, 0:tn], w[:, kt, ct * 128:(ct + 1) * 128], self.hT[:, kt, t0:t0 + tn],
                                start=(kt == 0), stop=(kt == KT - 1)), r=[bw, self.b_hT], w=[bpb])
                        if ev % 2 == 0:
                            k.op("dve", lambda e: e.tensor_copy(out=z[:, t0:t0 + tn], in_=pb[:, 0:tn]),
                                 r=[bpb], w=[bz])
                        else:
                            k.op("act", lambda e: e.activation(out=z[:, t0:t0 + tn], in_=pb[:, 0:tn],
                                                               func=AF.Copy), r=[bpb], w=[bz])
                        ev += 1
                    cc = c0 + ct * 128
                    k.dma(None, self.ZT[b, cc:cc + 128, :], z[:, :], r=[bz], w=[self.b_ZT[b]])
                    if "zt" in cfg.debug:
                        k.dma(None, self.dbg["zt"][b, cc:cc + 128, :], z[:, :], r=[bz])
            k.barrier()


    def copy(self, eng, out, in_, r=(), w=()):
        if eng == "act":
            return self.k.op("act", lambda e: e.activation(out=out, in_=in_, func=AF.Copy), r=r, w=w)
        return self.k.op(eng, lambda e: e.tensor_copy(out=out, in_=in_), r=r, w=w)

    def load_col(self, st, name, src_ap, n, bufs):
        t = self.sb(st, name, [n, 1], F32)
        self.k.dma("sp", t[:, :], src_ap.rearrange("(p o) -> p o", o=1), w=bufs)
        return t

    def phase_attn(self, l, b, ctx_out):
        cfg, nc, k = self.cfg, self.nc, self.k
        T, LC, LL = cfg.T, cfg.LC, cfg.LL
        NBLK, NCB = T // 128, LC // 128
        with ExitStack() as ph:
            b_c = Buf("attn_consts")
            qg = self.load_col(ph, "qgain", self.inp_at_q[l], 64, [b_c])
            kg = self.load_col(ph, "kgain", self.inp_at_k[l], 64, [b_c])
            esink = self.sb(ph, "esink", [128, 8], F32)
            k.dma("sp", esink[:, :], self.inp_at_sink[l].partition_broadcast(128), w=[b_c])
            k.op("act", lambda e: e.activation(out=esink[:], in_=esink[:], func=AF.Exp), r=[b_c], w=[b_c])
            cosT = self.sb(ph, "cosT", [64, LL], F32)
            sinT = self.sb(ph, "sinT", [64, LL], F32)
            k.dma("sp", cosT[:, :], self.c_cos[:, :], w=[b_c])
            k.dma("act", sinT[:, :], self.c_sin[:, :], w=[b_c])
            rotT = self.sb(ph, "rotT", [64, 64], F32)
            k.dma("sp", rotT[:, :], self.c_rot[:, :], w=[b_c])
            ones64 = self.sb(ph, "ones64", [64, 64], F32)
            k.op("pool", lambda e: e.memset(ones64[:], 1.0), w=[b_c])
            onesb = self.sb(ph, "onesb", [128, 128], BF16)
            k.op("pool", lambda e: e.memset(onesb[:], 1.0), w=[b_c])
            mprev = self.sb(ph, "mprev", [128, 128], BF16)
            mnext = self.sb(ph, "mnext", [128, 128], BF16)
            k.dma("pool", mprev[:, :], self.c_mprev[:, :], w=[b_c])
            k.dma("pool", mnext[:, :], self.c_mnext[:, :], w=[b_c])
            kf = [self.sb(ph, "kf%d" % j, [64, T], BF16) for j in range(2)]
            Qg = [self.sb(ph, "Qg%d" % j, [64, NBLK, 4, 128], BF16) for j in range(2)]
            Vt = self.sb(ph, "Vt", [128, NBLK, 2, 128], BF16)
            Ob = self.sb(ph, "Ob", [128, 4, T], BF16)
            b_kf, b_Qg, b_Vt, b_Ob = Buf(), Buf(), Buf(), Buf()
            if not ctx_out:
                k.op("pool", lambda e: e.memset(Ob[:, :, 0:LC], 0.0), w=[b_Ob])
            with ExitStack() as p1:
                raw = [self.sb(p1, "raw%d" % i, [64, T], F32) for i in range(2)]
                sq = self.sb(p1, "sq", [64, T], F32)
                rs = self.sb(p1, "rs", [64, T], F32)
                t1 = self.sb(p1, "t1", [64, 512], F32)
                t2 = self.sb(p1, "t2", [64, 512], F32)
                b_raw = [Buf(), Buf()]
                b_sq, b_rs, b_t1, b_t2 = Buf(), Buf(), Buf(), Buf()
                for hi in range(10):
                    isq = hi < 8
                    row0 = C_AT + 64 * hi
                    x, bx = raw[hi % 2], b_raw[hi % 2]
                    k.dma(None, x[:, :], self.ZT[b, row0:row0 + 64, :], r=[self.b_ZT[b]], w=[bx])
                    k.op("act", lambda e: e.activation(out=sq[:], in_=x[:], func=AF.Square), r=[bx], w=[b_sq])
                    for ci, (t0, tn) in enumerate(_chunks(T, 512)):
                        pb, bpb = self.ps[ci % 2], self.b_ps[ci % 2]
                        k.op("pe", lambda e: e.matmul(pb[0:64, 0:tn], ones64[:], sq[:, t0:t0 + tn],
                                                      start=True, stop=True), r=[b_sq, b_c], w=[bpb])
                        k.op("dve", lambda e: e.tensor_scalar(
                            out=rs[:, t0:t0 + tn], in0=pb[0:64, 0:tn], scalar1=1.0 / 64, scalar2=NORM_EPS,
                            op0=ALU.mult, op1=ALU.add), r=[bpb], w=[b_rs])
                    k.op("act", lambda e: e.activation(out=rs[:], in_=rs[:], func=AF.Sqrt), r=[b_rs], w=[b_rs])
                    k.op("dve", lambda e: e.reciprocal(out=rs[:], in_=rs[:]), r=[b_rs], w=[b_rs])
                    gn = qg if isq else kg
                    k.op("dve", lambda e: e.scalar_tensor_tensor(
                        out=x[:], in0=x[:], scalar=gn[:, 0:1], in1=rs[:], op0=ALU.mult, op1=ALU.mult),
                        r=[bx, b_rs, b_c], w=[bx])
                    if isq:
                        kvh, g = hi // 4, hi % 4
                        def dst(t0, tn, kvh=kvh, g=g):
                            return Qg[kvh][:, t0 // 128:(t0 + tn) // 128, g, :]
                        bdst = b_Qg
                    else:
                        j = hi - 8
                        def dst(t0, tn, j=j):
                            return kf[j][:, t0:t0 + tn].rearrange("p (n q) -> p n q", q=128)
                        bdst = b_kf
                    k.op("act", lambda e: e.activation(
                        out=dst(0, LC), in_=x[:, 0:LC].rearrange("p (n q) -> p n q", q=128), func=AF.Copy),
                        r=[bx], w=[bdst])
                    for ci, (t0, tn) in enumerate(_chunks(LL, 512)):
                        pb, bpb = self.ps[2 + ci % 2], self.b_ps[2 + ci % 2]
                        k.op("pe", lambda e: e.matmul(pb[0:64, 0:tn], rotT[:], x[:, LC + t0:LC + t0 + tn],
                                                      start=True, stop=True), r=[bx, b_c], w=[bpb])
                        k.op("dve", lambda e: e.tensor_tensor(out=t1[:, 0:tn], in0=pb[0:64, 0:tn],
                                                              in1=sinT[:, t0:t0 + tn], op=ALU.mult),
                             r=[bpb, b_c], w=[b_t1])
                        k.op("pool", lambda e: e.tensor_tensor(out=t2[:, 0:tn], in0=x[:, LC + t0:LC + t0 + tn],
                                                               in1=cosT[:, t0:t0 + tn], op=ALU.mult),
                             r=[bx, b_c], w=[b_t2])
                        k.op("dve", lambda e: e.tensor_tensor(
                            out=dst(LC + t0, tn), in0=t1[:, 0:tn].rearrange("p (n q) -> p n q", q=128),
                            in1=t2[:, 0:tn].rearrange("p (n q) -> p n q", q=128), op=ALU.add),
                            r=[b_t1, b_t2], w=[bdst])
                vx, bvx = raw[0], b_raw[0]
                vT = self.sb(p1, "vT", [128, T], F32)
                b_vT = Buf()
                k.dma(None, vT[:, :], self.ZT[b, C_AT + 640:C_AT + 768, :], r=[self.b_ZT[b]], w=[b_vT])
                for n in range(NBLK):
                    pb, bpb = self.ps[n % 4], self.b_ps[n % 4]
                    k.op("pe", lambda e: e.transpose(out=pb[:, 0:128], in_=vT[:, n * 128:(n + 1) * 128],
                                                     identity=self.ident[:]), r=[b_vT, self.b_ident], w=[bpb])
                    self.copy("dve" if n % 2 else "act",
                              Vt[:, n, :, :].rearrange("p j (two d) -> p j two d", two=2),
                              pb[:, 0:128].rearrange("p (j d) -> p j d", j=2).unsqueeze(2).to_broadcast([128, 2, 2, 64]),
                              r=[bpb], w=[b_Vt])
                k.barrier()
            with ExitStack() as p2:
                P = [self.sb(p2, "P%d" % i, [128, 5, 512], BF16) for i in range(2)]
                dn = [self.sb(p2, "dn%d" % i, [128, 512], F32) for i in range(2)]
                b_P = [Buf(), Buf()]
                b_dn = [Buf(), Buf()]
                it = 0
                for kvh in range(2):
                    for qb in range(NBLK):
                        if qb < NCB:
                            if not ctx_out:
                                continue
                            kbs = [(i, None) for i in range(NCB)]
                        else:
                            kbs = [(i, None) for i in range(NCB)]
                            if qb - 1 >= NCB:
                                kbs.append((qb - 1, mprev))
                            kbs.append((qb, None))
                            if qb + 1 < NBLK:
                                kbs.append((qb + 1, mnext))
                        Pb, bP = P[it % 2], b_P[it % 2]
                        dnb, bdn = dn[it % 2], b_dn[it % 2]
                        it += 1
                        for i, (kb, msk) in enumerate(kbs):
                            pb, bpb = self.ps[i], self.b_ps[i]
                            k.op("pe", lambda e: e.matmul(
                                pb[:, :], kf[kvh][:, kb * 128:(kb + 1) * 128],
                                Qg[kvh][:, qb, :, :].rearrange("p g q -> p (g q)"),
                                start=True, stop=True), r=[b_kf, b_Qg], w=[bpb])
                            k.op("act", lambda e: e.activation(out=Pb[:, i, :], in_=pb[:, :], func=AF.Exp,
                                                               scale=0.125), r=[bpb], w=[bP])
                            if msk is not None:
                                k.op("dve", lambda e: e.tensor_tensor(
                                    out=Pb[:, i, :].rearrange("p (g q) -> p g q", g=4),
                                    in0=Pb[:, i, :].rearrange("p (g q) -> p g q", g=4),
                                    in1=msk[:].unsqueeze(1).to_broadcast([128, 4, 128]), op=ALU.mult),
                                    r=[bP, b_c], w=[bP])
                        pn, bpn = self.ps[5], self.b_ps[5]
                        pd, bpd = self.ps[6], self.b_ps[6]
                        nk = len(kbs)
                        for i, (kb, msk) in enumerate(kbs):
                            k.op("pe", lambda e: e.matmul(pn[:, :], Vt[:, kb, kvh, :], Pb[:, i, :],
                                                          start=(i == 0), stop=(i == nk - 1)),
                                 r=[b_Vt, bP], w=[bpn])
                        for i, (kb, msk) in enumerate(kbs):
                            k.op("pe", lambda e: e.matmul(pd[:, :], onesb[:], Pb[:, i, :],
                                                          start=(i == 0), stop=(i == nk - 1)),
                                 r=[b_c, bP], w=[bpd])
                        k.op("dve", lambda e: e.tensor_tensor(
                            out=dnb[:].rearrange("p (g q) -> p g q", g=4),
                            in0=pd[:, :].rearrange("p (g q) -> p g q", g=4),
                            in1=esink[:, kvh * 4:(kvh + 1) * 4].unsqueeze(2).to_broadcast([128, 4, 128]),
                            op=ALU.add), r=[bpd, b_c], w=[bdn])
                        k.op("dve", lambda e: e.reciprocal(out=dnb[:], in_=dnb[:]), r=[bdn], w=[bdn])
                        for half in range(2):
                            lo, hi_ = half * 64, half * 64 + 64
                            k.op("dve", lambda e: e.tensor_tensor(
                                out=Ob[lo:hi_, kvh * 2:kvh * 2 + 2, qb * 128:(qb + 1) * 128],
                                in0=pn[lo:hi_, :].rearrange("p (g2 gl q) -> p g2 gl q", g2=2, gl=2)[:, :, half, :],
                                in1=dnb[lo:hi_, :].rearrange("p (g2 gl q) -> p g2 gl q", g2=2, gl=2)[:, :, half, :],
                                op=ALU.mult), r=[bpn, bdn], w=[b_Ob])
                k.dma(None, self.OT[b, 1536:2048, :].rearrange("(j p) t -> p j t", p=128), Ob[:, :, :],
                      r=[b_Ob], w=[self.b_OT[b]])
                k.barrier()


    def segs(self):
        cfg = self.cfg
        return [(t0, n) for (t0, n) in _chunks(cfg.LC, 512)] + \
               [(cfg.LC + t0, n) for (t0, n) in _chunks(cfg.LL, 512)]

    def phase_s5_prep(self, l, lst):
        cfg, nc, k = self.cfg, self.nc, self.k
        P = {}
        self.s5p = P
        bc = Buf("s5consts")
        P["b"] = bc
        NT = 32
        for nm in ("rho", "cs1", "sn1"):
            P[nm] = self.sb(lst, "s5_" + nm, [128, NT], F32)
        P["BTr"] = self.sb(lst, "s5_BTr", [32, NT, 128], BF16)
        P["BTi"] = self.sb(lst, "s5_BTi", [32, NT, 128], BF16)
        P["CWr"] = self.sb(lst, "s5_CWr", [128, NT, 128], BF16)
        P["CWi"] = self.sb(lst, "s5_CWi", [128, NT, 128], BF16)
        P["dcol"] = self.sb(lst, "s5_dcol", [128, 4], F32)
        P["gb"] = self.sb(lst, "s5_gb", [128, 4], F32)
        P["GW"] = self.sb(lst, "s5_GW", [128, 4, 512], BF16)
        k.dma("sp", P["dcol"][:, :], self.inp_s5_d[l].rearrange("(f p) -> p f", p=128), w=[bc])
        k.dma("sp", P["gb"][:, :], self.inp_s5_glu_b[l].rearrange("(f p) -> p f", p=128), w=[bc])
        k.dma("pool", P["GW"][:, :, :], self.inp_s5_glu_w[l].rearrange("(f p) c -> p f c", p=128), w=[bc])
        with ExitStack() as ph:
            def t(name, shape, dt=F32):
                return self.sb(ph, "s5p_" + name, shape, dt)
            lre, lim, stp = t("lre", [128, NT]), t("lim", [128, NT]), t("stp", [128, NT])
            pat = "d (gp g2) p -> (g2 p) (d gp)"
            k.dma("sp", lre[:, :], self.inp_s5_lre[l].rearrange(pat, g2=2), w=[bc])
            k.dma("act", lim[:, :], self.inp_s5_lim[l].rearrange(pat, g2=2), w=[bc])
            ls = self.inp_s5_ls[l].rearrange("d (gp g2) -> g2 (d gp)", g2=2)
            for g2 in range(2):
                k.dma("sp", stp[g2 * 64:(g2 + 1) * 64, :], ls[g2].partition_broadcast(64), w=[bc])
            bre, bim = t("bre", [128, NT, 16]), t("bim", [128, NT, 16])
            patb = "d (gp g2) p c -> (g2 p) (d gp) c"
            k.dma("sp", bre[:, :, :], self.inp_s5_bre[l].rearrange(patb, g2=2), w=[bc])
            k.dma("act", bim[:, :, :], self.inp_s5_bim[l].rearrange(patb, g2=2), w=[bc])
            CNr, CNi = t("CNr", [32, NT, 128]), t("CNi", [32, NT, 128])
            k.op("pool", lambda e: e.memset(CNr[:], 0.0), w=[bc])
            k.op("pool", lambda e: e.memset(CNi[:], 0.0), w=[bc])
            patc = "d (gp g2) c p -> g2 c (d gp) p"
            for g2 in range(2):
                k.dma("sp", CNr[g2 * 16:(g2 + 1) * 16, :, g2 * 64:(g2 + 1) * 64],
                      self.inp_s5_cre[l].rearrange(patc, g2=2)[g2], w=[bc])
                k.dma("act", CNi[g2 * 16:(g2 + 1) * 16, :, g2 * 64:(g2 + 1) * 64],
                      self.inp_s5_cim[l].rearrange(patc, g2=2)[g2], w=[bc])
            dv = lambda fn: k.op("dve", fn, r=[bc], w=[bc])
            ac = lambda fn: k.op("act", fn, r=[bc], w=[bc])
            ac(lambda e: e.activation(out=stp[:], in_=stp[:], func=AF.Exp))
            zr, zi = t("zr", [128, NT]), t("zi", [128, NT])
            dv(lambda e: e.tensor_tensor(out=zr[:], in0=lre[:], in1=stp[:], op=ALU.mult))
            dv(lambda e: e.tensor_tensor(out=zi[:], in0=lim[:], in1=stp[:], op=ALU.mult))
            ac(lambda e: e.activation(out=P["rho"][:], in_=zr[:], func=AF.Exp))
            sa, sh, ca, tmp, tmp2 = t("sa", [128, NT]), t("sh", [128, NT]), t("ca", [128, NT]), t("tmp", [128, NT]), t("tmp2", [128, NT])
            ac(lambda e: e.activation(out=sa[:], in_=zi[:], func=AF.Sin, scale=1.0 / 32))
            ac(lambda e: e.activation(out=sh[:], in_=zi[:], func=AF.Sin, scale=1.0 / 64))
            dv(lambda e: e.tensor_tensor(out=tmp[:], in0=sh[:], in1=sh[:], op=ALU.mult))
            dv(lambda e: e.tensor_scalar(out=ca[:], in0=tmp[:], scalar1=-2.0, scalar2=1.0, op0=ALU.mult, op1=ALU.add))
            for _ in range(5):
                dv(lambda e: e.tensor_tensor(out=tmp[:], in0=ca[:], in1=ca[:], op=ALU.mult))
                dv(lambda e: e.tensor_tensor(out=tmp2[:], in0=sa[:], in1=sa[:], op=ALU.mult))
                dv(lambda e: e.scalar_tensor_tensor(out=sa[:], in0=sa[:], scalar=2.0, in1=ca[:], op0=ALU.mult, op1=ALU.mult))
                dv(lambda e: e.tensor_tensor(out=ca[:], in0=tmp[:], in1=tmp2[:], op=ALU.subtract))
            self.copy("dve", P["cs1"][:], ca[:], r=[bc], w=[bc])
            self.copy("dve", P["sn1"][:], sa[:], r=[bc], w=[bc])
            nr, ni, den, cr, ci = t("nr", [128, NT]), t("ni", [128, NT]), t("den", [128, NT]), t("cr", [128, NT]), t("ci", [128, NT])
            dv(lambda e: e.tensor_tensor(out=nr[:], in0=P["rho"][:], in1=ca[:], op=ALU.mult))
            dv(lambda e: e.tensor_scalar(out=nr[:], in0=nr[:], scalar1=-1.0, scalar2=None, op0=ALU.add))
            dv(lambda e: e.tensor_tensor(out=ni[:], in0=P["rho"][:], in1=sa[:], op=ALU.mult))
            dv(lambda e: e.tensor_tensor(out=den[:], in0=lre[:], in1=lre[:], op=ALU.mult))
            dv(lambda e: e.tensor_tensor(out=tmp[:], in0=lim[:], in1=lim[:], op=ALU.mult))
            dv(lambda e: e.tensor_tensor(out=den[:], in0=den[:], in1=tmp[:], op=ALU.add))
            dv(lambda e: e.reciprocal(out=den[:], in_=den[:]))
            dv(lambda e: e.tensor_tensor(out=cr[:], in0=nr[:], in1=lre[:], op=ALU.mult))
            dv(lambda e: e.tensor_tensor(out=tmp[:], in0=ni[:], in1=lim[:], op=ALU.mult))
            dv(lambda e: e.tensor_tensor(out=cr[:], in0=cr[:], in1=tmp[:], op=ALU.add))
            dv(lambda e: e.tensor_tensor(out=cr[:], in0=cr[:], in1=den[:], op=ALU.mult))
            dv(lambda e: e.tensor_tensor(out=ci[:], in0=ni[:], in1=lre[:], op=ALU.mult))
            dv(lambda e: e.tensor_tensor(out=tmp[:], in0=nr[:], in1=lim[:], op=ALU.mult))
            dv(lambda e: e.tensor_tensor(out=ci[:], in0=ci[:], in1=tmp[:], op=ALU.subtract))
            dv(lambda e: e.tensor_tensor(out=ci[:], in0=ci[:], in1=den[:], op=ALU.mult))
            BBr, BBi = t("BBr", [128, NT, 32]), t("BBi", [128, NT, 32])
            k.op("pool", lambda e: e.memset(BBr[:], 0.0), w=[bc])
            k.op("pool", lambda e: e.memset(BBi[:], 0.0), w=[bc])
            t3, t4 = t("t3", [128, NT, 16]), t("t4", [128, NT, 16])
            crb = cr[:].unsqueeze(2).to_broadcast([128, NT, 16])
            cib = ci[:].unsqueeze(2).to_broadcast([128, NT, 16])
            dv(lambda e: e.tensor_tensor(out=t3[:], in0=bre[:], in1=crb, op=ALU.mult))
            dv(lambda e: e.tensor_tensor(out=t4[:], in0=bim[:], in1=cib, op=ALU.mult))
            for g2 in range(2):
                lo, hi = g2 * 64, g2 * 64 + 64
                dv(lambda e: e.tensor_tensor(out=BBr[lo:hi, :, g2 * 16:(g2 + 1) * 16], in0=t3[lo:hi], in1=t4[lo:hi], op=ALU.subtract))
            dv(lambda e: e.tensor_tensor(out=t3[:], in0=bim[:], in1=crb, op=ALU.mult))
            dv(lambda e: e.tensor_tensor(out=t4[:], in0=bre[:], in1=cib, op=ALU.mult))
            for g2 in range(2):
                lo, hi = g2 * 64, g2 * 64 + 64
                dv(lambda e: e.tensor_tensor(out=BBi[lo:hi, :, g2 * 16:(g2 + 1) * 16], in0=t3[lo:hi], in1=t4[lo:hi], op=ALU.add))
            k.op("pool", lambda e: e.memset(P["CWr"][:], 0.0), w=[bc])
            k.op("pool", lambda e: e.memset(P["CWi"][:], 0.0), w=[bc])
            pb, bpb = self.ps[7], self.b_ps[7]
            for ti in range(NT):
                gq = (ti % 16) % 4
                k.op("pe", lambda e: e.transpose(out=pb[0:32, 0:128], in_=BBr[:, ti, :], identity=self.ident[:]),
                     r=[bc, self.b_ident], w=[bpb])
                k.op("pe", lambda e: e.transpose(out=pb[0:32, 128:256], in_=BBi[:, ti, :], identity=self.ident[:]),
                     r=[bc, self.b_ident], w=[bpb])
                k.op("pe", lambda e: e.transpose(out=pb[:, 256:288], in_=CNr[:, ti, :], identity=self.ident[0:32, 0:32]),
                     r=[bc, self.b_ident], w=[bpb])
                k.op("pe", lambda e: e.transpose(out=pb[:, 288:320], in_=CNi[:, ti, :], identity=self.ident[0:32, 0:32]),
                     r=[bc, self.b_ident], w=[bpb])
                self.copy("act", P["BTr"][:, ti, :], pb[0:32, 0:128], r=[bpb], w=[bc])
                self.copy("dve", P["BTi"][:, ti, :], pb[0:32, 128:256], r=[bpb], w=[bc])
                self.copy("act", P["CWr"][:, ti, gq * 32:(gq + 1) * 32], pb[:, 256:288], r=[bpb], w=[bc])
                k.op("dve", lambda e: e.tensor_scalar(out=P["CWi"][:, ti, gq * 32:(gq + 1) * 32], in0=pb[:, 288:320],
                                                      scalar1=-1.0, scalar2=None, op0=ALU.mult), r=[bpb], w=[bc])
            k.barrier()

    def phase_s5(self, l, b):
        cfg, nc, k = self.cfg, self.nc, self.k
        T, LC, LL = cfg.T, cfg.LC, cfg.LL
        P = self.s5p
        bc = P["b"]
        segs = self.segs()
        assert len(segs) <= 5

        def rsl(n0, ln):
            return slice(n0, (n0 - ln) if n0 - ln >= 0 else None, -1)

        with ExitStack() as ph:
            def t(name, shape, dt=F32):
                return self.sb(ph, "s5_" + name, shape, dt)
            cosN, sinN = t("cosN", [128, T]), t("sinN", [128, T])
            tA, tB = t("tA", [128, T // 2 + 2]), t("tB", [128, T // 2 + 2])
            wre, wim, qre, qim = t("wre", [128, T]), t("wim", [128, T]), t("qre", [128, T]), t("qim", [128, T])
            bur, bui = t("bur", [128, 512]), t("bui", [128, 512])
            ta, tb_, tc, td = t("ta", [128, 512]), t("tb", [128, 512]), t("tc", [128, 512]), t("td", [128, 512])
            hre, him = t("hre", [128, 512], BF16), t("him", [128, 512], BF16)
            uT = [t("uT%d" % i, [32, T], BF16) for i in range(2)]
            uF = t("uF", [128, T])
            Y0 = t("Y0", [128, T])
            YGf = t("YGf", [128, 4, T])
            YGb = t("YGb", [128, 4, T], BF16)
            b_tab, b_w, b_q, b_bu, b_t, b_h, b_uF, b_Y0, b_YG, b_Ob = (Buf() for _ in range(10))
            b_t2 = Buf()
            b_uT = [Buf(), Buf()]
            it = 0
            for ft in range(4):
                nacc = [0] * len(segs)
                for gq in range(4):
                    gp = ft * 4 + gq
                    u, bu_ = uT[it % 2], b_uT[it % 2]
                    it += 1
                    r0 = C_S5 + 32 * gp
                    k.dma("pool", u[:, :], self.ZT[b, r0:r0 + 32, :], r=[self.b_ZT[b]], w=[bu_])
                    for d in range(2):
                        ti = d * 16 + gp
                        k.op("pool", lambda e: e.memset(cosN[:, 0:1], 1.0), w=[b_tab])
                        k.op("pool", lambda e: e.memset(sinN[:, 0:1], 0.0), w=[b_tab])
                        self.copy("dve", cosN[:, 1:2], P["cs1"][:, ti:ti + 1], r=[bc], w=[b_tab])
                        self.copy("dve", sinN[:, 1:2], P["sn1"][:, ti:ti + 1], r=[bc], w=[b_tab])
                        m = 1
                        while m < T - 1:
                            ln = min(m, T - 1 - m)
                            cr_, ci_ = cosN[:, m:m + 1], sinN[:, m:m + 1]
                            src_c, src_s = cosN[:, 1:1 + ln], sinN[:, 1:1 + ln]
                            k.op("pool", lambda e: e.tensor_scalar(out=tA[:, 0:ln], in0=src_s, scalar1=ci_, scalar2=None, op0=ALU.mult),
                                 r=[b_tab], w=[b_t])
                            k.op("dve", lambda e: e.tensor_scalar(out=tB[:, 0:ln], in0=src_s, scalar1=cr_, scalar2=None, op0=ALU.mult),
                                 r=[b_tab], w=[b_t2])
                            k.op("dve", lambda e: e.scalar_tensor_tensor(out=cosN[:, m + 1:m + 1 + ln], in0=src_c, scalar=cr_, in1=tA[:, 0:ln],
                                                                         op0=ALU.mult, op1=ALU.subtract), r=[b_tab, b_t], w=[b_tab])
                            k.op("dve", lambda e: e.scalar_tensor_tensor(out=sinN[:, m + 1:m + 1 + ln], in0=src_c, scalar=ci_, in1=tB[:, 0:ln],
                                                                         op0=ALU.mult, op1=ALU.add), r=[b_tab, b_t2], w=[b_tab])
                            m += ln
                        for (t0, tn) in segs:
                            if d == 0:
                                csl = slice(t0, t0 + tn)
                            elif t0 < LC:
                                csl = rsl(LC - 1 - t0, tn)
                            else:
                                csl = rsl(T - 1 + LC - t0, tn)
                            pr, bpr = self.ps[5], self.b_ps[5]
                            pi_, bpi = self.ps[6], self.b_ps[6]
                            k.op("pe", lambda e: e.matmul(pr[:, 0:tn], P["BTr"][:, ti, :], u[:, t0:t0 + tn], start=True, stop=True),
                                 r=[bc, bu_], w=[bpr])
                            k.op("pe", lambda e: e.matmul(pi_[:, 0:tn], P["BTi"][:, ti, :], u[:, t0:t0 + tn], start=True, stop=True),
                                 r=[bc, bu_], w=[bpi])
                            self.copy("act", bur[:, 0:tn], pr[:, 0:tn], r=[bpr], w=[b_bu])
                            self.copy("act", bui[:, 0:tn], pi_[:, 0:tn], r=[bpi], w=[b_bu])
                            k.op("dve", lambda e: e.tensor_tensor(out=ta[:, 0:tn], in0=bur[:, 0:tn], in1=cosN[:, csl], op=ALU.mult), r=[b_bu, b_tab], w=[b_t])
                            k.op("pool", lambda e: e.tensor_tensor(out=tb_[:, 0:tn], in0=bui[:, 0:tn], in1=sinN[:, csl], op=ALU.mult), r=[b_bu, b_tab], w=[b_t2])
                            k.op("dve", lambda e: e.tensor_tensor(out=wre[:, t0:t0 + tn], in0=ta[:, 0:tn], in1=tb_[:, 0:tn], op=ALU.add), r=[b_t, b_t2], w=[b_w])
                            k.op("pool", lambda e: e.tensor_tensor(out=tc[:, 0:tn], in0=bui[:, 0:tn], in1=cosN[:, csl], op=ALU.mult), r=[b_bu, b_tab], w=[b_t2])
                            k.op("dve", lambda e: e.tensor_tensor(out=td[:, 0:tn], in0=bur[:, 0:tn], in1=sinN[:, csl], op=ALU.mult), r=[b_bu, b_tab], w=[b_t])
                            k.op("pool", lambda e: e.tensor_tensor(out=wim[:, t0:t0 + tn], in0=tc[:, 0:tn], in1=td[:, 0:tn], op=ALU.subtract), r=[b_t, b_t2], w=[b_w])
                        rho_b = P["rho"][:, ti:ti + 1]
                        for (wsrc, qdst) in ((wre, qre), (wim, qim)):
                            if d == 0:
                                k.op("dve", lambda e: e.tensor_tensor_scan(
                                    out=qdst[:, 0:T], data0=rho_b.to_broadcast([128, T]), data1=wsrc[:, 0:T],
                                    initial=0.0, op0=ALU.mult, op1=ALU.add), r=[b_w, bc], w=[b_q])
                            else:
                                k.op("dve", lambda e: e.tensor_tensor_scan(
                                    out=qdst[:, rsl(LC - 1, LC)], data0=rho_b.to_broadcast([128, LC]),
                                    data1=wsrc[:, rsl(LC - 1, LC)], initial=0.0, op0=ALU.mult, op1=ALU.add),
                                    r=[b_w, bc], w=[b_q])
                                k.op("dve", lambda e: e.tensor_tensor_scan(
                                    out=qdst[:, rsl(T - 1, LL)], data0=rho_b.to_broadcast([128, LL]),
                                    data1=wsrc[:, rsl(T - 1, LL)], initial=qdst[:, 0:1], op0=ALU.mult, op1=ALU.add),
                                    r=[b_w, bc, b_q], w=[b_q])
                        for si, (t0, tn) in enumerate(segs):
                            if d == 0:
                                csl = slice(t0, t0 + tn)
                            elif t0 < LC:
                                csl = rsl(LC - 1 - t0, tn)
                            else:
                                csl = rsl(T - 1 + LC - t0, tn)
                            tsl = slice(t0, t0 + tn)
                            k.op("dve", lambda e: e.tensor_tensor(out=ta[:, 0:tn], in0=qre[:, tsl], in1=cosN[:, csl], op=ALU.mult), r=[b_q, b_tab], w=[b_t])
                            k.op("pool", lambda e: e.tensor_tensor(out=tb_[:, 0:tn], in0=qim[:, tsl], in1=sinN[:, csl], op=ALU.mult), r=[b_q, b_tab], w=[b_t2])
                            k.op("dve", lambda e: e.tensor_tensor(out=hre[:, 0:tn], in0=ta[:, 0:tn], in1=tb_[:, 0:tn], op=ALU.subtract), r=[b_t, b_t2], w=[b_h])
                            k.op("pool", lambda e: e.tensor_tensor(out=tc[:, 0:tn], in0=qim[:, tsl], in1=cosN[:, csl], op=ALU.mult), r=[b_q, b_tab], w=[b_t2])
                            k.op("dve", lambda e: e.tensor_tensor(out=td[:, 0:tn], in0=qre[:, tsl], in1=sinN[:, csl], op=ALU.mult), r=[b_q, b_tab], w=[b_t])
                            k.op("pool", lambda e: e.tensor_tensor(out=him[:, 0:tn], in0=tc[:, 0:tn], in1=td[:, 0:tn], op=ALU.add), r=[b_t, b_t2], w=[b_h])
                            py, bpy = self.ps[si], self.b_ps[si]
                            k.op("pe", lambda e: e.matmul(py[:, 0:tn], P["CWr"][:, ti, :], hre[:, 0:tn],
                                                          start=(nacc[si] == 0), stop=False), r=[bc, b_h], w=[bpy])
                            nacc[si] += 1
                            k.op("pe", lambda e: e.matmul(py[:, 0:tn], P["CWi"][:, ti, :], him[:, 0:tn],
                                                          start=False, stop=(nacc[si] == 15)), r=[bc, b_h], w=[bpy])
                            nacc[si] += 1
                r0 = C_S5 + 128 * ft
                k.dma(None, uF[:, :], self.ZT[b, r0:r0 + 128, :], r=[self.b_ZT[b]], w=[b_uF])
                for si, (t0, tn) in enumerate(segs):
                    py, bpy = self.ps[si], self.b_ps[si]
                    tsl = slice(t0, t0 + tn)
                    k.op("dve", lambda e: e.scalar_tensor_tensor(out=Y0[:, tsl], in0=uF[:, tsl], scalar=P["dcol"][:, ft:ft + 1],
                                                                 in1=py[:, 0:tn], op0=ALU.mult, op1=ALU.add),
                         r=[b_uF, bc, bpy], w=[b_Y0])
                k.op("pool", lambda e: e.tensor_tensor(out=wre[:], in0=Y0[:], in1=Y0[:], op=ALU.mult), r=[b_Y0], w=[b_w])
                k.op("pool", lambda e: e.tensor_scalar(out=wre[:], in0=wre[:], scalar1=0.044715, scalar2=1.0, op0=ALU.mult, op1=ALU.add), r=[b_w], w=[b_w])
                k.op("pool", lambda e: e.tensor_tensor(out=wre[:], in0=wre[:], in1=Y0[:], op=ALU.mult), r=[b_w, b_Y0], w=[b_w])
                k.op("act", lambda e: e.activation(out=wre[:], in_=wre[:], func=AF.Sigmoid, scale=1.5957691216057308), r=[b_w], w=[b_w])
                k.op("dve", lambda e: e.tensor_tensor(out=YGf[:, ft, :], in0=Y0[:], in1=wre[:], op=ALU.mult), r=[b_w, b_Y0], w=[b_YG])
                self.copy("act", YGb[:, ft, :], YGf[:, ft, :], r=[b_YG], w=[b_YG])
            ev = 0
            b_ob2 = [Buf(), Buf()]
            for fo in range(4):
                ob, bob = (qre, qim)[fo % 2], b_ob2[fo % 2]
                for si, (t0, tn) in enumerate(segs):
                    pb, bpb = self.ps[ev % 4], self.b_ps[ev % 4]
                    ev += 1
                    for fi in range(4):
                        k.op("pe", lambda e: e.matmul(pb[:, 0:tn], P["GW"][:, fi, fo * 128:(fo + 1) * 128], YGb[:, fi, t0:t0 + tn],
                                                      start=(fi == 0), stop=(fi == 3)), r=[bc, b_YG], w=[bpb])
                    k.op("act", lambda e: e.activation(out=ta[:, 0:tn], in_=pb[:, 0:tn], func=AF.Sigmoid,
                                                       bias=P["gb"][:, fo:fo + 1]), r=[bpb, bc], w=[b_t])
                    k.op("dve", lambda e: e.tensor_tensor(out=ob[:, t0:t0 + tn], in0=YGf[:, fo, t0:t0 + tn], in1=ta[:, 0:tn], op=ALU.mult),
                         r=[b_t, b_YG], w=[bob])
                k.dma("pool", self.OT[b, 512 + fo * 128:512 + (fo + 1) * 128, :], ob[:, :], r=[bob], w=[self.b_OT[b]])
            k.barrier()


    def phase_hg_prep(self, l, lst):
        cfg, nc, k = self.cfg, self.nc, self.k
        DEPTH = cfg.DEPTH
        P = {}
        self.hgp = P
        bc = Buf("hgconsts")
        P["b"] = bc
        P["lb"] = self.sb(lst, "hg_lb", [128, 4], F32)
        P["oml"] = self.sb(lst, "hg_oml", [128, 4], F32)
        P["fb"] = self.sb(lst, "hg_fb", [128, 2, 4], F32)
        P["ng"] = self.sb(lst, "hg_ng", [128, 1], F32)
        P["Z"] = self.sb(lst, "hg_Z", [128, 255], BF16)
        P["identb"] = self.sb(lst, "hg_identb", [128, 128], BF16)
        P["ones"] = self.sb(lst, "hg_ones", [128, 128], F32)
        k.op("pool", lambda e: e.memset(P["Z"][:], 0.0), w=[bc])
        k.op("pool", lambda e: e.memset(P["Z"][:, 127:128], 1.0), r=[bc], w=[bc])
        k.op("pool", lambda e: e.memset(P["ones"][:], 1.0), w=[bc])
        self.copy("dve", P["identb"][:], self.ident[:], r=[self.b_ident], w=[bc])
        k.dma("sp", P["fb"][:, :, :], self.inp_hg_fb[l].rearrange("d (f p) -> p d f", p=128), w=[bc])
        k.dma("sp", P["ng"][:, :], self.inp_hg_ng[l].rearrange("(p o) -> p o", o=1), w=[bc])
        with ExitStack() as ph:
            lg = self.sb(ph, "hg_lg", [128, DEPTH, 4], F32)
            ssum = self.sb(ph, "hg_ssum", [128, 4], F32)
            k.dma("sp", lg[:, :, :], self.inp_hg_lbl.rearrange("l (f p) -> p l f", p=128), w=[bc])
            k.op("act", lambda e: e.activation(out=lg[:], in_=lg[:], func=AF.Exp), r=[bc], w=[bc])
            self.copy("dve", ssum[:], lg[:, 0, :], r=[bc], w=[bc])
            for j in range(1, DEPTH):
                k.op("dve", lambda e: e.tensor_tensor(out=ssum[:], in0=ssum[:], in1=lg[:, j, :], op=ALU.add), r=[bc], w=[bc])
            k.op("dve", lambda e: e.reciprocal(out=ssum[:], in_=ssum[:]), r=[bc], w=[bc])
            k.op("pool", lambda e: e.memset(P["lb"][:], 0.0), r=[bc], w=[bc])
            for j in range(1, l + 1):
                k.op("dve", lambda e: e.tensor_tensor(out=P["lb"][:], in0=P["lb"][:], in1=lg[:, j, :], op=ALU.add), r=[bc], w=[bc])
            k.op("dve", lambda e: e.tensor_tensor(out=P["lb"][:], in0=P["lb"][:], in1=ssum[:], op=ALU.mult), r=[bc], w=[bc])
            k.op("dve", lambda e: e.tensor_scalar(out=P["oml"][:], in0=P["lb"][:], scalar1=-1.0, scalar2=1.0,
                                                  op0=ALU.mult, op1=ALU.add), r=[bc], w=[bc])
            k.barrier()

    def phase_hg(self, l, b):
        cfg, nc, k = self.cfg, self.nc, self.k
        T, LC, LL = cfg.T, cfg.LC, cfg.LL
        P = self.hgp
        bc = P["b"]
        segs = self.segs()

        def rsl(n0, ln):
            return slice(n0, (n0 - ln) if n0 - ln >= 0 else None, -1)

        with ExitStack() as ph:
            def t(name, shape, dt=F32):
                return self.sb(ph, "hg_" + name, shape, dt)
            fT, kT, qT, gT, oS = t("fT", [128, T]), t("kT", [128, T]), t("qT", [128, T]), t("gT", [128, T]), t("oS", [128, T])
            vT = t("vT", [128, T], BF16)
            kv = [t("kv%d" % i, [128, T]) for i in range(2)]
            S = [t("S%d" % i, [128, T]) for i in range(2)]
            qs = [t("qs%d" % i, [128, T], BF16) for i in range(2)]
            b_f, b_q, b_v, b_g, b_o = Buf(), Buf(), Buf(), Buf(), Buf()
            b_kv, b_S, b_qs = [Buf(), Buf()], [Buf(), Buf()], [Buf(), Buf()]
            it = 0
            for h in range(4):
                r_q = C_HG + 128 * h
                k.dma(None, qT[:, :], self.ZT[b, r_q:r_q + 128, :], r=[self.b_ZT[b]], w=[b_q])
                r_v = C_HG + 1536 + 128 * h
                k.dma("pool", vT[:, :], self.ZT[b, r_v:r_v + 128, :], r=[self.b_ZT[b]], w=[b_v])
                r_g = C_HG + 2048 + 128 * h
                k.dma(None, gT[:, :], self.ZT[b, r_g:r_g + 128, :], r=[self.b_ZT[b]], w=[b_g])
                nacc = 0
                for d in range(2):
                    r_f = C_HG + 512 * (1 + d) + 128 * h
                    k.dma(None, fT[:, :], self.ZT[b, r_f:r_f + 128, :], r=[self.b_ZT[b]], w=[b_f])
                    k.op("act", lambda e: e.activation(out=fT[:], in_=fT[:], func=AF.Sigmoid, bias=P["fb"][:, d, h:h + 1]),
                         r=[b_f, bc], w=[b_f])
                    k.op("dve", lambda e: e.tensor_scalar(out=fT[:], in0=fT[:], scalar1=P["oml"][:, h:h + 1], scalar2=P["lb"][:, h:h + 1],
                                                          op0=ALU.mult, op1=ALU.add), r=[b_f, bc], w=[b_f])
                    k.op("pool", lambda e: e.tensor_scalar(out=kT[:], in0=fT[:], scalar1=-1.0, scalar2=1.0,
                                                           op0=ALU.mult, op1=ALU.add), r=[b_f], w=[b_f])
                    for j in range(128):
                        kvb, bkv = kv[it % 2], b_kv[it % 2]
                        Sb, bS = S[it % 2], b_S[it % 2]
                        qsb, bqs = qs[it % 2], b_qs[it % 2]
                        it += 1
                        for ci, (t0, tn) in enumerate(segs):
                            pb, bpb = self.ps[5 + ci % 2], self.b_ps[5 + ci % 2]
                            k.op("pe", lambda e: e.matmul(pb[:, 0:tn], P["identb"][:, j:j + 1].to_broadcast([128, 128]),
                                                          vT[:, t0:t0 + tn], start=True, stop=True), r=[bc, b_v], w=[bpb])
                            k.op("dve", lambda e: e.tensor_tensor(out=kvb[:, t0:t0 + tn], in0=pb[:, 0:tn], in1=kT[:, t0:t0 + tn], op=ALU.mult),
                                 r=[bpb, b_f], w=[bkv])
                        if d == 0:
                            k.op("dve", lambda e: e.tensor_tensor_scan(out=Sb[:, 0:T], data0=fT[:, 0:T], data1=kvb[:, 0:T], initial=0.0,
                                                                       op0=ALU.mult, op1=ALU.add), r=[b_f, bkv], w=[bS])
                        else:
                            k.op("dve", lambda e: e.tensor_tensor_scan(out=Sb[:, rsl(LC - 1, LC)], data0=fT[:, rsl(LC - 1, LC)],
                                                                       data1=kvb[:, rsl(LC - 1, LC)], initial=0.0,
                                                                       op0=ALU.mult, op1=ALU.add), r=[b_f, bkv], w=[bS])
                            k.op("dve", lambda e: e.tensor_tensor_scan(out=Sb[:, rsl(T - 1, LL)], data0=fT[:, rsl(T - 1, LL)],
                                                                       data1=kvb[:, rsl(T - 1, LL)], initial=Sb[:, 0:1],
                                                                       op0=ALU.mult, op1=ALU.add), r=[b_f, bkv, bS], w=[bS])
                        k.op("pool", lambda e: e.tensor_tensor(out=qsb[:], in0=Sb[:], in1=qT[:], op=ALU.mult), r=[bS, b_q], w=[bqs])
                        for ci, (t0, tn) in enumerate(segs):
                            py, bpy = self.ps[ci], self.b_ps[ci]
                            k.op("pe", lambda e: e.matmul(py[:, 0:tn], P["Z"][:, 127 - j:255 - j], qsb[:, t0:t0 + tn],
                                                          start=(nacc == 0), stop=(nacc == 255)), r=[bc, bqs], w=[bpy])
                        nacc += 1
                for ci, (t0, tn) in enumerate(segs):
                    py, bpy = self.ps[ci], self.b_ps[ci]
                    self.copy("act", oS[:, t0:t0 + tn], py[:, 0:tn], r=[bpy], w=[b_o])
                sq, bsq = kv[0], b_kv[0]
                rs, brs = kv[1], b_kv[1]
                k.op("act", lambda e: e.activation(out=sq[:], in_=oS[:], func=AF.Square), r=[b_o], w=[bsq])
                for ci, (t0, tn) in enumerate(segs):
                    pb, bpb = self.ps[5 + ci % 2], self.b_ps[5 + ci % 2]
                    k.op("pe", lambda e: e.matmul(pb[:, 0:tn], P["ones"][:], sq[:, t0:t0 + tn], start=True, stop=True),
                         r=[bc, bsq], w=[bpb])
                    k.op("dve", lambda e: e.tensor_scalar(out=rs[:, t0:t0 + tn], in0=pb[:, 0:tn], scalar1=1.0 / 128, scalar2=NORM_EPS,
                                                          op0=ALU.mult, op1=ALU.add), r=[bpb], w=[brs])
                k.op("act", lambda e: e.activation(out=rs[:], in_=rs[:], func=AF.Sqrt), r=[brs], w=[brs])
                k.op("dve", lambda e: e.reciprocal(out=rs[:], in_=rs[:]), r=[brs], w=[brs])
                k.op("dve", lambda e: e.scalar_tensor_tensor(out=oS[:], in0=oS[:], scalar=P["ng"][:, 0:1], in1=rs[:],
                                                             op0=ALU.mult, op1=ALU.mult), r=[b_o, brs, bc], w=[b_o])
                k.op("act", lambda e: e.activation(out=gT[:], in_=gT[:], func=AF.Sigmoid), r=[b_g], w=[b_g])
                k.op("dve", lambda e: e.tensor_tensor(out=oS[:], in0=oS[:], in1=gT[:], op=ALU.mult), r=[b_o, b_g], w=[b_o])
                k.dma("pool", self.OT[b, 1024 + 128 * h:1024 + 128 * (h + 1), :], oS[:, :], r=[b_o], w=[self.b_OT[b]])
            k.barrier()


    def phase_rw(self, l, lst):
        cfg, nc, k = self.cfg, self.nc, self.k
        NB, T, LC, LL = cfg.NB, cfg.T, cfg.LC, cfg.LL
        NBLK = T // 128
        NCOL = NB * 8
        TB, TV = 64, 8
        segs = self.segs()
        bc = Buf("rwconsts")

        def rsl(n0, ln):
            return slice(n0, (n0 - ln) if n0 - ln >= 0 else None, -1)

        def col(b, d, hp):
            return (b * 2 + d) * 4 + hp

        bones = self.sb(lst, "rw_bones", [128, 128], F32)
        bonesb = self.sb(lst, "rw_bonesb", [128, 128], BF16)
        ind2 = self.sb(lst, "rw_ind2", [2, 128], BF16)
        ZZ = self.sb(lst, "rw_ZZ", [128, 254], BF16)
        k.op("pool", lambda e: e.memset(bones[:], 0.0), w=[bc])
        k.op("pool", lambda e: e.memset(bones[0:64, 0:64], 1.0), r=[bc], w=[bc])
        k.op("pool", lambda e: e.memset(bones[64:128, 64:128], 1.0), r=[bc], w=[bc])
        self.copy("dve", bonesb[:], bones[:], r=[bc], w=[bc])
        k.dma("pool", ind2[:, :], self.c_ind2[:, :], w=[bc])
        antiI = self.sb(lst, "rw_antiI", [128, 128], F32)
        k.dma("sp", antiI[:, :], self.c_antiI[:, :], w=[bc])
        k.op("pool", lambda e: e.memset(ZZ[:], 0.0), r=[bc], w=[bc])
        k.op("pool", lambda e: e.memset(ZZ[0:64, 126:127], 1.0), r=[bc], w=[bc])
        k.op("pool", lambda e: e.memset(ZZ[64:128, 127:128], 1.0), r=[bc], w=[bc])
        pc = {}
        for nm, src in (("kk", self.inp_rw_kk[l]), ("ka", self.inp_rw_ka[l]), ("rk", self.inp_rw_rk[l]),
                        ("lnw", self.inp_rw_lnw[l]), ("lnb", self.inp_rw_lnb[l])):
            pc[nm] = self.sb(lst, "rw_" + nm, [128, 4], F32)
            k.dma("sp", pc[nm][:, :], src.rearrange("(f p) -> p f", p=128), w=[bc])
        pc["omka"] = self.sb(lst, "rw_omka", [128, 4], F32)
        k.op("dve", lambda e: e.tensor_scalar(out=pc["omka"][:], in0=pc["ka"][:], scalar1=-1.0, scalar2=1.0, op0=ALU.mult, op1=ALU.add),
             r=[bc], w=[bc])
        for nm, src in (("w0", self.inp_rw_w0[l]), ("a0", self.inp_rw_a0[l])):
            pc[nm] = self.sb(lst, "rw_" + nm, [128, 2, 4], F32)
            k.dma("sp", pc[nm][:, :, :], src.rearrange("d (f p) -> p d f", p=128), w=[bc])
        W2 = self.sb(lst, "rw_W2", [64, 2, 512], BF16)
        A2 = self.sb(lst, "rw_A2", [64, 2, 512], BF16)
        G2 = self.sb(lst, "rw_G2", [128, 512], BF16)
        k.dma("pool", W2[:, :, :], self.inp_rw_w2[l].rearrange("d r c -> r d c"), w=[bc])
        k.dma("pool", A2[:, :, :], self.inp_rw_a2[l].rearrange("d r c -> r d c"), w=[bc])
        k.dma("pool", G2[:, :], self.inp_rw_g2[l], w=[bc])
        mu = self.sb(lst, "rw_mu", [128, 15, 2], F32)
        c0 = self.sb(lst, "rw_c0", [128, 15], F32)
        for m_ in range(2):
            k.dma("sp", mu[:, :, m_], self.inp_rw_mu[l][m_].rearrange("(f p) -> p f", p=128), w=[bc])
        k.op("dve", lambda e: e.tensor_tensor(out=c0[:], in0=mu[:, :, 0], in1=mu[:, :, 1], op=ALU.add), r=[bc], w=[bc])
        k.op("dve", lambda e: e.tensor_scalar(out=c0[:], in0=c0[:], scalar1=-1.0, scalar2=1.0, op0=ALU.mult, op1=ALU.add), r=[bc], w=[bc])

        with ExitStack() as ph:
            zb = [self.sb(ph, "rw_zb%d" % i, [128, T], F32) for i in range(2)]
            zs = [self.sb(ph, "rw_zs%d" % i, [128, T], F32) for i in range(2)]
            b_zb, b_zs = [Buf(), Buf()], [Buf(), Buf()]
            it = 0
            for b in range(NB):
                for f in range(15):
                    z, bz, o, bo = zb[it % 2], b_zb[it % 2], zs[it % 2], b_zs[it % 2]
                    it += 1
                    k.dma(None, z[:, :], self.ZT[b, f * 128:(f + 1) * 128, :], r=[self.b_ZT[b]], w=[bz])
                    k.op("act", lambda e: e.activation(out=o[:], in_=z[:], func=AF.Copy, scale=c0[:, f:f + 1]), r=[bz, bc], w=[bo])
                    for (lo, hi) in ((0, LC), (LC, T)):
                        k.op("dve", lambda e: e.scalar_tensor_tensor(out=o[:, lo + 1:hi], in0=z[:, lo:hi - 1], scalar=mu[:, f, 0:1],
                                                                     in1=o[:, lo + 1:hi], op0=ALU.mult, op1=ALU.add), r=[bz, bc, bo], w=[bo])
                        k.op("dve", lambda e: e.scalar_tensor_tensor(out=o[:, lo:hi - 1], in0=z[:, lo + 1:hi], scalar=mu[:, f, 1:2],
                                                                     in1=o[:, lo:hi - 1], op0=ALU.mult, op1=ALU.add), r=[bz, bc, bo], w=[bo])
                    k.dma(None, self.ZT[b, f * 128:(f + 1) * 128, :], o[:, :], r=[bo], w=[self.b_ZT[b]])
            k.barrier()

        RWP, VTK, YR = self.RWP, self.VTK, self.YR
        b_RWP, b_VTK, b_YR = Buf(), Buf(), Buf()
        with ExitStack() as ph:
            def t(name, shape, dt=F32):
                return self.sb(ph, "rwB_" + name, shape, dt)
            rT, kT, kk, aT, tmp, bv, ke, dec = (t(n, [128, T]) for n in ("rT", "kT", "kk", "aT", "tmp", "bv", "ke", "dec"))
            vT = t("vT", [128, T])
            vtk = t("vtk", [128, NBLK, 128], BF16)
            dwT = t("dwT", [64, 2, T], BF16)
            daT = t("daT", [64, 2, T], BF16)
            b_r, b_k, b_kk, b_a, b_tmp, b_bv, b_ke, b_dec, b_v, b_vtk, b_dw = (Buf() for _ in range(11))

            rev = [t("rev%d" % i, [128, T]) for i in range(2)]
            b_rev = [Buf(), Buf()]
            rcnt = [0]

            def store(arr, cc, src, bsrc, d):
                dst = RWP[arr, cc]
                if d == 0:
                    k.dma(None, dst[:, :], src[:, :], r=[bsrc], w=[b_RWP])
                else:
                    rv, brv = rev[rcnt[0] % 2], b_rev[rcnt[0] % 2]
                    rcnt[0] += 1
                    k.op("pool", lambda e: e.tensor_copy(out=rv[:, 0:LC], in_=src[:, rsl(LC - 1, LC)]), r=[bsrc], w=[brv])
                    k.op("pool", lambda e: e.tensor_copy(out=rv[:, LC:T], in_=src[:, rsl(T - 1, LL)]), r=[bsrc, brv], w=[brv])
                    k.dma(None, dst[:, :], rv[:, :], r=[brv], w=[b_RWP])

            for b in range(NB):
                for d in range(2):
                    k.dma("pool", dwT[:, d, :], self.ZT[b, 1536 + 64 * d:1600 + 64 * d, :], r=[self.b_ZT[b]], w=[b_dw])
                    k.dma("pool", daT[:, d, :], self.ZT[b, 1664 + 64 * d:1728 + 64 * d, :], r=[self.b_ZT[b]], w=[b_dw])
                k.op("act", lambda e: e.activation(out=dwT[:], in_=dwT[:], func=AF.Tanh), r=[b_dw], w=[b_dw])
                for hp in range(4):
                    k.dma(None, rT[:, :], self.ZT[b, 128 * hp:128 * (hp + 1), :], r=[self.b_ZT[b]], w=[b_r])
                    k.dma(None, kT[:, :], self.ZT[b, 512 + 128 * hp:512 + 128 * (hp + 1), :], r=[self.b_ZT[b]], w=[b_k])
                    k.dma(None, vT[:, :], self.ZT[b, 1024 + 128 * hp:1024 + 128 * (hp + 1), :], r=[self.b_ZT[b]], w=[b_v])
                    for n in range(NBLK):
                        pb, bpb = self.ps[n % 2], self.b_ps[n % 2]
                        k.op("pe", lambda e: e.transpose(out=pb[:, 0:128], in_=vT[:, n * 128:(n + 1) * 128], identity=self.ident[:]),
                             r=[b_v, self.b_ident], w=[bpb])
                        self.copy("act", vtk[:, n, :], pb[:, 0:128], r=[bpb], w=[b_vtk])
                    for hl in range(2):
                        k.dma(None, VTK[b, :, hl, hp, :].rearrange("(n p) v -> p n v", p=128), vtk[:, :, hl * 64:(hl + 1) * 64],
                              r=[b_vtk], w=[b_VTK])
                    k.op("dve", lambda e: e.tensor_scalar(out=kk[:], in0=kT[:], scalar1=pc["kk"][:, hp:hp + 1], scalar2=None, op0=ALU.mult),
                         r=[b_k, bc], w=[b_kk])
                    k.op("act", lambda e: e.activation(out=tmp[:], in_=kk[:], func=AF.Square), r=[b_kk], w=[b_tmp])
                    for ci, (t0, tn) in enumerate(segs):
                        pb, bpb = self.ps[2 + ci % 2], self.b_ps[2 + ci % 2]
                        k.op("pe", lambda e: e.matmul(pb[:, 0:tn], bones[:], tmp[:, t0:t0 + tn], start=True, stop=True), r=[bc, b_tmp], w=[bpb])
                        k.op("act", lambda e: e.activation(out=aT[:, t0:t0 + tn], in_=pb[:, 0:tn], func=AF.Sqrt), r=[bpb], w=[b_a])
                    k.op("dve", lambda e: e.tensor_scalar(out=aT[:], in0=aT[:], scalar1=1e-12, scalar2=None, op0=ALU.max), r=[b_a], w=[b_a])
                    k.op("dve", lambda e: e.reciprocal(out=aT[:], in_=aT[:]), r=[b_a], w=[b_a])
                    k.op("dve", lambda e: e.tensor_tensor(out=kk[:], in0=kk[:], in1=aT[:], op=ALU.mult), r=[b_kk, b_a], w=[b_kk])
                    k.op("pool", lambda e: e.tensor_scalar(out=tmp[:], in0=kk[:], scalar1=-1.0, scalar2=None, op0=ALU.mult), r=[b_kk], w=[b_tmp])
                    for d in range(2):
                        cc = col(b, d, hp)
                        store(0, cc, tmp, b_tmp, d)
                        store(4, cc, rT, b_r, d)
                        for ci, (t0, tn) in enumerate(segs):
                            pb, bpb = self.ps[4 + ci % 2], self.b_ps[4 + ci % 2]
                            k.op("pe", lambda e: e.matmul(pb[:, 0:tn], W2[:, d, hp * 128:(hp + 1) * 128], dwT[:, d, t0:t0 + tn],
                                                          start=True, stop=True), r=[bc, b_dw], w=[bpb])
                            k.op("act", lambda e: e.activation(out=dec[:, t0:t0 + tn], in_=pb[:, 0:tn], func=AF.Sigmoid,
                                                               bias=pc["w0"][:, d, hp:hp + 1]), r=[bpb, bc], w=[b_dec])
                            pb2, bpb2 = self.ps[6 + ci % 2], self.b_ps[6 + ci % 2]
                            k.op("pe", lambda e: e.matmul(pb2[:, 0:tn], A2[:, d, hp * 128:(hp + 1) * 128], daT[:, d, t0:t0 + tn],
                                                          start=True, stop=True), r=[bc, b_dw], w=[bpb2])
                            k.op("act", lambda e: e.activation(out=aT[:, t0:t0 + tn], in_=pb2[:, 0:tn], func=AF.Sigmoid,
                                                               bias=pc["a0"][:, d, hp:hp + 1]), r=[bpb2, bc], w=[b_a])
                        k.op("act", lambda e: e.activation(out=dec[:], in_=dec[:], func=AF.Exp, scale=-0.6065306597126334), r=[b_dec], w=[b_dec])
                        store(2, cc, dec, b_dec, d)
                        k.op("dve", lambda e: e.tensor_tensor(out=bv[:], in0=kk[:], in1=aT[:], op=ALU.mult), r=[b_kk, b_a], w=[b_bv])
                        store(1, cc, bv, b_bv, d)
                        k.op("dve", lambda e: e.tensor_scalar(out=ke[:], in0=aT[:], scalar1=pc["ka"][:, hp:hp + 1], scalar2=pc["omka"][:, hp:hp + 1],
                                                              op0=ALU.mult, op1=ALU.add), r=[b_a, bc], w=[b_ke])
                        k.op("pool", lambda e: e.tensor_tensor(out=ke[:], in0=ke[:], in1=kT[:], op=ALU.mult), r=[b_ke, b_k], w=[b_ke])
                        store(3, cc, ke, b_ke, d)
            k.barrier()

        with ExitStack() as ph:
            def t(name, shape, dt=F32):
                return self.sb(ph, "rwC_" + name, shape, dt)
            ST = t("ST", [128, NCOL, 64])
            X = t("X", [128, NCOL, 64])
            tmpb = [t("tmpb%d" % i, [128, NCOL, 64], BF16) for i in range(2)]
            t2 = [t("t2%d" % i, [128, NCOL, 64]) for i in range(2)]
            t3 = [t("t3%d" % i, [128, NCOL, 64]) for i in range(2)]
            vb = [t("vb%d" % i, [128, NCOL, 64]) for i in range(2)]
            tq = [t("tq%d" % i, [128, NCOL, 64], BF16) for i in range(2)]
            arr = [t("arr%d" % i, [128, 5, NCOL, TB]) for i in range(2)]
            vrow = [t("vrow%d" % i, [2, TV, NCOL, 64], BF16) for i in range(2)]
            yev = [t("yev%d" % i, [128, NCOL * 64]) for i in range(2)]
            b_ST, b_X = Buf(), Buf()
            b_tmpb, b_t2, b_t3, b_vb, b_tq, b_arr, b_vrow, b_yev = ([Buf(), Buf()] for _ in range(8))
            k.op("pool", lambda e: e.memset(ST[:], 0.0), w=[b_ST])
            NH = NCOL * 64 // 512
            assert NH <= 2
            for blk in range(T // TB):
                n0 = blk * TB
                A_, bA = arr[blk % 2], b_arr[blk % 2]
                for ai in range(5):
                    k.dma(None, A_[:, ai, :, :], RWP[ai, :, :, n0:n0 + TB].rearrange("c p n -> p c n"), r=[b_RWP], w=[bA])
                for nl in range(TB):
                    n = n0 + nl
                    i2 = n % 2
                    if nl % TV == 0:
                        vi_ = (n // TV) % 2
                        vr, bvr = vrow[vi_], b_vrow[vi_]
                        for b in range(NB):
                            for d in range(2):
                                if d == 0:
                                    src = VTK[b, n:n + TV]
                                elif n < LC:
                                    src = VTK[b, rsl(LC - 1 - n, TV)]
                                else:
                                    src = VTK[b, rsl(T - 1 + LC - n, TV)]
                                c0_ = (b * 2 + d) * 4
                                k.dma(None, vr[:, :, c0_:c0_ + 4, :].rearrange("hl t c v -> hl t (c v)"),
                                      src.rearrange("t hl hp v -> hl t (hp v)"), r=[b_VTK], w=[bvr])
                    vr, bvr = vrow[(n // TV) % 2], b_vrow[(n // TV) % 2]
                    nv = nl % TV

                    def bcs(ai):
                        return A_[:, ai, :, nl:nl + 1].to_broadcast([128, NCOL, 64])
                    for hh in range(NH):
                        pv, bpv = self.ps[2 + hh], self.b_ps[2 + hh]
                        k.op("pe", lambda e: e.matmul(pv[:, :], ind2[:, :], vr[:, nv, hh * 8:(hh + 1) * 8, :],
                                                      start=True, stop=True), r=[bc, bvr], w=[bpv])
                        k.op("dve", lambda e: e.tensor_tensor(out=t3[i2][:, hh * 8:(hh + 1) * 8, :],
                                                              in0=pv[:, :].rearrange("p (c v) -> p c v", v=64),
                                                              in1=A_[:, 3, hh * 8:(hh + 1) * 8, nl:nl + 1].to_broadcast([128, 8, 64]),
                                                              op=ALU.mult), r=[bpv, bA], w=[b_t3[i2]])
                    k.op("dve", lambda e: e.tensor_tensor(out=tmpb[i2][:], in0=ST[:], in1=bcs(0), op=ALU.mult),
                         r=[b_ST, bA], w=[b_tmpb[i2]])
                    for hh in range(NH):
                        psa, bpsa = self.ps[hh], self.b_ps[hh]
                        k.op("pe", lambda e: e.matmul(psa[:, :], bonesb[:, :], tmpb[i2][:, hh * 8:(hh + 1) * 8, :],
                                                      start=True, stop=True), r=[bc, b_tmpb[i2]], w=[bpsa])
                    k.op("pool", lambda e: e.tensor_tensor(out=X[:], in0=ST[:], in1=bcs(2), op=ALU.mult), r=[b_ST, bA], w=[b_X])
                    k.op("pool", lambda e: e.tensor_tensor(out=X[:], in0=X[:], in1=t3[i2][:], op=ALU.add), r=[b_X, b_t3[i2]], w=[b_X])
                    for hh in range(NH):
                        psa, bpsa = self.ps[hh], self.b_ps[hh]
                        k.op("dve", lambda e: e.tensor_tensor(out=t2[i2][:, hh * 8:(hh + 1) * 8, :],
                                                              in0=psa[:, :].rearrange("p (c v) -> p c v", v=64),
                                                              in1=A_[:, 1, hh * 8:(hh + 1) * 8, nl:nl + 1].to_broadcast([128, 8, 64]),
                                                              op=ALU.mult), r=[bpsa, bA], w=[b_t2[i2]])
                    k.op("dve", lambda e: e.tensor_tensor(out=ST[:], in0=X[:], in1=t2[i2][:], op=ALU.add), r=[b_X, b_t2[i2]], w=[b_ST])
                    k.op("pool", lambda e: e.tensor_tensor(out=tq[i2][:], in0=ST[:], in1=bcs(4), op=ALU.mult), r=[b_ST, bA], w=[b_tq[i2]])
                    for hh in range(NH):
                        py, bpy = self.ps[4 + hh], self.b_ps[4 + hh]
                        k.op("pe", lambda e: e.matmul(py[:, :], ZZ[:, 126 - 2 * nl:254 - 2 * nl], tq[i2][:, hh * 8:(hh + 1) * 8, :],
                                                      start=(nl == 0), stop=(nl == TB - 1)), r=[bc, b_tq[i2]], w=[bpy])
                ye, bye = yev[blk % 2], b_yev[blk % 2]
                for hh in range(NH):
                    py, bpy = self.ps[4 + hh], self.b_ps[4 + hh]
                    self.copy("act", ye[:, hh * 512:(hh + 1) * 512], py[:, :], r=[bpy], w=[bye])
                k.dma(None, YR[n0:n0 + TB, :, :].rearrange("n hl c -> (n hl) c"), ye[:, :], r=[bye], w=[b_YR])
            k.barrier()

        with ExitStack() as ph:
            def t(name, shape, dt=F32):
                return self.sb(ph, "rwD_" + name, shape, dt)
            yt = [t("yt%d" % i, [128, 2, 128]) for i in range(2)]
            yT, rT, kT, vT, m1, m2 = (t(n, [128, T]) for n in ("yT", "rT", "kT", "vT", "m1", "m2"))
            gl = t("gl", [128, T], BF16)
            b_yt = [Buf(), Buf()]
            b_y, b_r, b_k, b_v, b_m1, b_m2, b_gl = (Buf() for _ in range(7))
            it = 0
            for b in range(NB):
                k.dma("pool", gl[:, :], self.ZT[b, 1792:1920, :], r=[self.b_ZT[b]], w=[b_gl])
                k.op("act", lambda e: e.activation(out=gl[:], in_=gl[:], func=AF.Sigmoid), r=[b_gl], w=[b_gl])
                for hp in range(4):
                    k.dma(None, rT[:, :], self.ZT[b, 128 * hp:128 * (hp + 1), :], r=[self.b_ZT[b]], w=[b_r])
                    k.dma(None, kT[:, :], self.ZT[b, 512 + 128 * hp:512 + 128 * (hp + 1), :], r=[self.b_ZT[b]], w=[b_k])
                    k.dma(None, vT[:, :], self.ZT[b, 1024 + 128 * hp:1024 + 128 * (hp + 1), :], r=[self.b_ZT[b]], w=[b_v])
                    for n in range(NBLK):
                        y2, by2 = yt[it % 2], b_yt[it % 2]
                        it += 1
                        p0 = n * 128
                        c_f, c_b = col(b, 0, hp), col(b, 1, hp)
                        k.dma(None, y2[:, 0, :].rearrange("p (hl v) -> p hl v", hl=2),
                              YR[p0:p0 + 128, :, c_f * 64:(c_f + 1) * 64], r=[b_YR], w=[by2])
                        nb0 = (LC - 1 - p0) if p0 < LC else (T - 1 + LC - p0)
                        k.dma(None, y2[:, 1, :].rearrange("p (hl v) -> p hl v", hl=2),
                              YR[nb0 - 127:nb0 + 1, :, c_b * 64:(c_b + 1) * 64], r=[b_YR], w=[by2])
                        pb, bpb = self.ps[n % 4], self.b_ps[n % 4]
                        k.op("pe", lambda e: e.matmul(pb[:, 0:128], y2[:, 0, :], self.ident[:], start=True, stop=False),
                             r=[by2, self.b_ident], w=[bpb])
                        k.op("pe", lambda e: e.matmul(pb[:, 0:128], y2[:, 1, :], antiI[:], start=False, stop=True),
                             r=[by2, bc], w=[bpb])
                        self.copy("act" if n % 2 else "dve", yT[:, p0:p0 + 128], pb[:, 0:128], r=[bpb], w=[b_y])
                    for ci, (t0, tn) in enumerate(segs):
                        pb, bpb = self.ps[4 + ci % 2], self.b_ps[4 + ci % 2]
                        k.op("pe", lambda e: e.matmul(pb[:, 0:tn], bones[:], yT[:, t0:t0 + tn], start=True, stop=True), r=[bc, b_y], w=[bpb])
                        k.op("dve", lambda e: e.scalar_tensor_tensor(out=m1[:, t0:t0 + tn], in0=pb[:, 0:tn], scalar=-1.0 / 64,
                                                                     in1=yT[:, t0:t0 + tn], op0=ALU.mult, op1=ALU.add),
                             r=[bpb, b_y], w=[b_m1])
                    k.op("act", lambda e: e.activation(out=m2[:], in_=m1[:], func=AF.Square), r=[b_m1], w=[b_m2])
                    for ci, (t0, tn) in enumerate(segs):
                        pb, bpb = self.ps[6 + ci % 2], self.b_ps[6 + ci % 2]
                        k.op("pe", lambda e: e.matmul(pb[:, 0:tn], bones[:], m2[:, t0:t0 + tn], start=True, stop=True), r=[bc, b_m2], w=[bpb])
                        k.op("dve", lambda e: e.tensor_scalar(out=yT[:, t0:t0 + tn], in0=pb[:, 0:tn], scalar1=1.0 / 64, scalar2=64e-5,
                                                              op0=ALU.mult, op1=ALU.add), r=[bpb, b_y], w=[b_y])
                    k.op("act", lambda e: e.activation(out=yT[:], in_=yT[:], func=AF.Sqrt), r=[b_y], w=[b_y])
                    k.op("dve", lambda e: e.reciprocal(out=yT[:], in_=yT[:]), r=[b_y], w=[b_y])
                    k.op("dve", lambda e: e.tensor_tensor(out=m1[:], in0=m1[:], in1=yT[:], op=ALU.mult), r=[b_m1, b_y], w=[b_m1])
                    k.op("dve", lambda e: e.tensor_scalar(out=m1[:], in0=m1[:], scalar1=pc["lnw"][:, hp:hp + 1], scalar2=pc["lnb"][:, hp:hp + 1],
                                                          op0=ALU.mult, op1=ALU.add), r=[b_m1, bc], w=[b_m1])
                    k.op("dve", lambda e: e.scalar_tensor_tensor(out=m2[:], in0=rT[:], scalar=pc["rk"][:, hp:hp + 1], in1=kT[:],
                                                                  op0=ALU.mult, op1=ALU.mult), r=[b_r, b_k, bc, b_m2], w=[b_m2])
                    for ci, (t0, tn) in enumerate(segs):
                        pb, bpb = self.ps[4 + ci % 2], self.b_ps[4 + ci % 2]
                        k.op("pe", lambda e: e.matmul(pb[:, 0:tn], bones[:], m2[:, t0:t0 + tn], start=True, stop=True), r=[bc, b_m2], w=[bpb])
                        k.op("dve", lambda e: e.tensor_tensor(out=yT[:, t0:t0 + tn], in0=pb[:, 0:tn], in1=vT[:, t0:t0 + tn], op=ALU.mult),
                             r=[bpb, b_v, b_y], w=[b_y])
                    k.op("pool", lambda e: e.tensor_tensor(out=m1[:], in0=m1[:], in1=yT[:], op=ALU.add), r=[b_m1, b_y], w=[b_m1])
                    for ci, (t0, tn) in enumerate(segs):
                        pb, bpb = self.ps[6 + ci % 2], self.b_ps[6 + ci % 2]
                        k.op("pe", lambda e: e.matmul(pb[:, 0:tn], G2[:, hp * 128:(hp + 1) * 128], gl[:, t0:t0 + tn], start=True, stop=True),
                             r=[bc, b_gl], w=[bpb])
                        k.op("dve", lambda e: e.tensor_tensor(out=m2[:, t0:t0 + tn], in0=pb[:, 0:tn], in1=m1[:, t0:t0 + tn], op=ALU.mult),
                             r=[bpb, b_m1, b_m2], w=[b_m2])
                    k.dma("pool", self.OT[b, 128 * hp:128 * (hp + 1), :], m2[:, :], r=[b_m2], w=[self.b_OT[b]])
            k.barrier()


    def phase_outproj(self, l, lst, ctx_out):
        cfg, nc, k = self.cfg, self.nc, self.k
        NB, T, LC, D = cfg.NB, cfg.T, cfg.LC, cfg.D
        KT = D // 128
        R = NB + 1
        bc = Buf("opconsts")
        for r in range(R):
            k.dma(None, self.MODROW[l, r].rearrange("(j p) -> p j", p=128), self.modT[:, :, r], r=[self.b_mod_sb], w=[self.b_MODROW])
        WO = self.sb(lst, "op_WO", [128, KT, D], BF16)
        k.dma("pool", WO[:, :, :], self.inp_w_out[l].rearrange("(k p) c -> p k c", p=128), w=[bc])
        with ExitStack() as ph:
            OTs = self.sb(ph, "op_OT", [128, KT, T], BF16)
            G1 = self.sb(ph, "op_G1", [128, D], F32)
            xt = [self.sb(ph, "op_x%d" % i, [128, D], F32) for i in range(2)]
            tm = [self.sb(ph, "op_t%d" % i, [128, 512], F32) for i in range(2)]
            b_OTs, b_G1 = Buf(), Buf()
            b_x, b_tm = [Buf(), Buf()], [Buf(), Buf()]
            it = 0
            ev = 0
            for b in range(NB):
                k.dma(None, OTs[:, :, :], self.OT[b].rearrange("(j p) t -> p j t", p=128), r=[self.b_OT[b]], w=[b_OTs])
                src = self.xin if l == 0 else self.XT
                cur_r = None
                for tt in range(T // 128):
                    t0 = tt * 128
                    if t0 < LC and not ctx_out:
                        continue
                    r = NB if t0 < LC else b
                    if r != cur_r:
                        k.dma("sp", G1[:, :], self.MODROW[l, r, 2 * D:3 * D].partition_broadcast(128), r=[self.b_MODROW], w=[b_G1])
                        cur_r = r
                    x, bx = xt[it % 2], b_x[it % 2]
                    it += 1
                    k.dma(None, x[:, :], src[b, t0:t0 + 128, :], r=[self.b_XT[b]], w=[bx])
                    for c in range(D // 512):
                        pb, bpb = self.ps[ev % 4], self.b_ps[ev % 4]
                        tb, btb = tm[ev % 2], b_tm[ev % 2]
                        ev += 1
                        for kt in range(KT):
                            k.op("pe", lambda e: e.matmul(pb[:, :], OTs[:, kt, t0:t0 + 128], WO[:, kt, c * 512:(c + 1) * 512],
                                                          start=(kt == 0), stop=(kt == KT - 1)), r=[b_OTs, bc], w=[bpb])
                        k.op("dve", lambda e: e.tensor_tensor(out=tb[:], in0=pb[:, :], in1=G1[:, c * 512:(c + 1) * 512], op=ALU.mult),
                             r=[bpb, b_G1], w=[btb])
                        k.op("pool", lambda e: e.tensor_tensor(out=x[:, c * 512:(c + 1) * 512], in0=x[:, c * 512:(c + 1) * 512], in1=tb[:], op=ALU.add),
                             r=[btb, bx], w=[bx])
                    k.dma(None, self.XT[b, t0:t0 + 128, :], x[:, :], r=[bx], w=[self.b_XT[b]])
            k.barrier()

    def phase_moe(self, l, lst, ctx_out, last):
        cfg, nc, k = self.cfg, self.nc, self.k
        NB, T, LC, LL, D, NE = cfg.NB, cfg.T, cfg.LC, cfg.LL, cfg.D, cfg.NE
        KT = D // 128
        HT = D // 128
        bc = Buf("moeconsts")
        lo = 0 if ctx_out else LC
        ntk = T - lo
        nt_all = ntk // 128
        n_pass = -(-nt_all // 6)
        per = -(-nt_all // n_pass)
        passes = []
        tcur = 0
        while tcur < nt_all:
            n_ = min(per, nt_all - tcur)
            passes.append((lo + tcur * 128, n_ * 128))
            tcur += n_
        TGM = per * 128
        RW = self.sb(lst, "moe_RW", [128, KT, NE], BF16)
        k.dma("pool", RW[:, :, :], self.inp_rw_[l].rearrange("(k p) e -> p k e", p=128), w=[bc])
        RB = self.sb(lst, "moe_RB", [128, NE], F32)
        k.dma("sp", RB[:, :], self.inp_rb_[l].partition_broadcast(128), w=[bc])
        BGU = self.sb(lst, "moe_BGU", [128, NE, HT, 2], F32)
        for e_ in range(NE):
            k.dma(None, BGU[:, e_, :, :], self.inp_bgu[l, e_].rearrange("(h p two) -> p h two", p=128, two=2), w=[bc])
        BD = self.sb(lst, "moe_BD", [NE, D], F32)
        k.dma("sp", BD[:, :], self.inp_bd[l], w=[bc])
        identb = self.sb(lst, "moe_identb", [128, 128], BF16)
        self.copy("dve", identb[:], self.ident[:], r=[self.b_ident], w=[bc])
        with ExitStack() as ph:
            def t(name, shape, dt=F32):
                return self.sb(ph, "moe_" + name, shape, dt)
            X = t("X", [128, KT, TGM], BF16)
            Yacc = t("Yacc", [128, KT, TGM])
            act = t("act", [128, HT, TGM], BF16)
            GT = t("GT", [NE, TGM])
            GTb = t("GTb", [NE, TGM], BF16)
            L = t("L", [128, NE])
            E = t("E", [128, NE])
            m8 = t("m8", [128, 8])
            sm = t("sm", [128, 4])
            b_X, b_Y, b_act, b_GT, b_L = Buf(), Buf(), Buf(), Buf(), Buf()
            b_wgu, b_wdn, b_g1, b_u1, b_sg = ([Buf(), Buf()] for _ in range(5))
            for b in range(NB):
                for (p0, TG) in passes:
                    chunks = _chunks(TG, 512)
                    groups = []
                    for (c0, cn) in _chunks(TG, 256):
                        a0 = p0 + c0
                        a1 = a0 + cn
                        cuts = [a0] + ([LC] if a0 < LC < a1 else []) + [a1]
                        for ci in range(len(cuts) - 1):
                            s0, s1 = cuts[ci], cuts[ci + 1]
                            groups.append((self.XT[b, s0:s1, :], s1 - s0, NB if s0 < LC else b, s0 - p0, [self.b_XT[b]]))
                    self.norm_groups(X, b_X, groups, which=2, ntmax=2)
                    for tt in range(TG // 128):
                        pb, bpb = self.ps[tt % 2], self.b_ps[tt % 2]
                        for kt in range(KT):
                            k.op("pe", lambda e: e.matmul(pb[:, 0:NE], X[:, kt, tt * 128:(tt + 1) * 128], RW[:, kt, :],
                                                          start=(kt == 0), stop=(kt == KT - 1)), r=[b_X, bc], w=[bpb])
                        k.op("dve", lambda e: e.tensor_tensor(out=L[:], in0=pb[:, 0:NE], in1=RB[:], op=ALU.add), r=[bpb, bc], w=[b_L])
                        k.op("dve", lambda e: e.max(out=m8[:], in_=L[:]), r=[b_L], w=[b_L])
                        k.op("dve", lambda e: e.tensor_scalar(out=E[:], in0=L[:], scalar1=m8[:, 3:4], scalar2=None, op0=ALU.is_ge), r=[b_L], w=[b_L])
                        k.op("dve", lambda e: e.tensor_scalar(out=sm[:, 0:1], in0=m8[:, 0:1], scalar1=-1.0, scalar2=None, op0=ALU.mult), r=[b_L], w=[b_L])
                        k.op("act", lambda e: e.activation(out=L[:], in_=L[:], func=AF.Exp, bias=sm[:, 0:1]), r=[b_L], w=[b_L])
                        k.op("dve", lambda e: e.tensor_tensor(out=E[:], in0=E[:], in1=L[:], op=ALU.mult), r=[b_L], w=[b_L])
                        k.op("dve", lambda e: e.tensor_reduce(out=sm[:, 1:2], in_=E[:], axis=AX.X, op=ALU.add), r=[b_L], w=[b_L])
                        k.op("dve", lambda e: e.reciprocal(out=sm[:, 1:2], in_=sm[:, 1:2]), r=[b_L], w=[b_L])
                        k.op("dve", lambda e: e.tensor_scalar(out=E[:], in0=E[:], scalar1=sm[:, 1:2], scalar2=None, op0=ALU.mult), r=[b_L], w=[b_L])
                        pt, bpt = self.ps[2 + tt % 2], self.b_ps[2 + tt % 2]
                        k.op("pe", lambda e: e.transpose(out=pt[0:NE, 0:128], in_=E[:, :], identity=self.ident[:]), r=[b_L, self.b_ident], w=[bpt])
                        self.copy("act", GT[:, tt * 128:(tt + 1) * 128], pt[0:NE, 0:128], r=[bpt], w=[b_GT])
                        self.copy("dve", GTb[:, tt * 128:(tt + 1) * 128], pt[0:NE, 0:128], r=[bpt], w=[b_GT])
                    pa = ExitStack()
                    wgu = [self.sb(pa, "moe_wgu%d" % i, [128, KT, 512], BF16) for i in range(2)]
                    wdn = [self.sb(pa, "moe_wdn%d" % i, [128, HT, 256], BF16) for i in range(2)]
                    g1 = [self.sb(pa, "moe_g1%d" % i, [128, 512], F32) for i in range(2)]
                    u1 = [self.sb(pa, "moe_u1%d" % i, [128, 512], F32) for i in range(2)]
                    sg = [self.sb(pa, "moe_sg%d" % i, [128, 512], F32) for i in range(2)]
                    ev = 0
                    for dt in range(KT):
                        for (c0, cn) in chunks:
                            pb, bpb = self.ps[6 + ev % 2], self.b_ps[6 + ev % 2]
                            ev += 1
                            k.op("pe", lambda e: e.matmul(pb[:, 0:cn], BD[:, dt * 128:(dt + 1) * 128], GT[:, c0:c0 + cn], start=True, stop=True),
                                 r=[bc, b_GT], w=[bpb])
                            self.copy("act", Yacc[:, dt, c0:c0 + cn], pb[:, 0:cn], r=[bpb], w=[b_Y])
                    igu = 0
                    idn = 0
                    ie = 0
                    gu_list = [(e2, hq2) for e2 in range(NE) for hq2 in range(HT // 2)]
                    dn_list = [(e2, dq2) for e2 in range(NE) for dq2 in range(D // 256)]

                    def issue_gu(i):
                        e2, hq2 = gu_list[i]
                        src2 = self.inp_wgu[l, e2].rearrange("(k p) c -> p k c", p=128)
                        k.dma("pool", wgu[i % 2][:, :, :], src2[:, :, hq2 * 512:(hq2 + 1) * 512], w=[b_wgu[i % 2]])

                    def issue_dn(i):
                        e2, dq2 = dn_list[i]
                        src2 = self.inp_wd[l, e2].rearrange("(h p) c -> p h c", p=128)
                        k.dma("pool", wdn[i % 2][:, :, :], src2[:, :, dq2 * 256:(dq2 + 1) * 256], w=[b_wdn[i % 2]])

                    issue_gu(0)
                    issue_dn(0)
                    for e_ in range(NE):
                        for hq in range(HT // 2):
                            if igu + 1 < len(gu_list):
                                issue_gu(igu + 1)
                            w, bw = wgu[igu % 2], b_wgu[igu % 2]
                            igu += 1
                            for h2 in range(2):
                                ht = hq * 2 + h2
                                for (c0, cn) in chunks:
                                    i2 = ie % 2
                                    ie += 1
                                    pg, bpg = self.ps[0 + i2], self.b_ps[0 + i2]
                                    pu, bpu = self.ps[2 + i2], self.b_ps[2 + i2]
                                    pgb, bpgb = self.ps[4 + i2], self.b_ps[4 + i2]
                                    for kt in range(KT):
                                        k.op("pe", lambda e: e.matmul(pg[:, 0:cn], w[:, kt, h2 * 256:(h2 + 1) * 256:2], X[:, kt, c0:c0 + cn],
                                                                      start=(kt == 0), stop=(kt == KT - 1)), r=[bw, b_X], w=[bpg])
                                    for kt in range(KT):
                                        k.op("pe", lambda e: e.matmul(pu[:, 0:cn], w[:, kt, h2 * 256 + 1:(h2 + 1) * 256:2], X[:, kt, c0:c0 + cn],
                                                                      start=(kt == 0), stop=(kt == KT - 1)), r=[bw, b_X], w=[bpu])
                                    k.op("pe", lambda e: e.matmul(pgb[:, 0:cn], identb[0:NE, e_:e_ + 1].to_broadcast([NE, 128]), GTb[:, c0:c0 + cn],
                                                                  start=True, stop=True), r=[bc, b_GT], w=[bpgb])
                                    k.op("dve", lambda e: e.tensor_scalar(out=g1[i2][:, 0:cn], in0=pg[:, 0:cn], scalar1=BGU[:, e_, ht, 0:1], scalar2=7.0,
                                                                          op0=ALU.add, op1=ALU.min), r=[bpg, bc], w=[b_g1[i2]])
                                    k.op("act", lambda e: e.activation(out=sg[i2][:, 0:cn], in_=g1[i2][:, 0:cn], func=AF.Sigmoid, scale=1.702),
                                         r=[b_g1[i2]], w=[b_sg[i2]])
                                    k.op("dve", lambda e: e.tensor_scalar(out=u1[i2][:, 0:cn], in0=pu[:, 0:cn], scalar1=BGU[:, e_, ht, 1:2], scalar2=7.0,
                                                                          op0=ALU.add, op1=ALU.min), r=[bpu, bc], w=[b_u1[i2]])
                                    k.op("pool", lambda e: e.tensor_scalar(out=u1[i2][:, 0:cn], in0=u1[i2][:, 0:cn], scalar1=-7.0, scalar2=1.0,
                                                                           op0=ALU.max, op1=ALU.add), r=[b_u1[i2]], w=[b_u1[i2]])
                                    k.op("pool", lambda e: e.tensor_tensor(out=g1[i2][:, 0:cn], in0=g1[i2][:, 0:cn], in1=sg[i2][:, 0:cn], op=ALU.mult),
                                         r=[b_g1[i2], b_sg[i2]], w=[b_g1[i2]])
                                    k.op("pool", lambda e: e.tensor_tensor(out=g1[i2][:, 0:cn], in0=g1[i2][:, 0:cn], in1=u1[i2][:, 0:cn], op=ALU.mult),
                                         r=[b_g1[i2], b_u1[i2]], w=[b_g1[i2]])
                                    k.op("dve", lambda e: e.tensor_tensor(out=act[:, ht, c0:c0 + cn], in0=pgb[:, 0:cn], in1=g1[i2][:, 0:cn], op=ALU.mult),
                                         r=[bpgb, b_g1[i2]], w=[b_act])
                        for dq in range(D // 256):
                            if idn + 1 < len(dn_list):
                                issue_dn(idn + 1)
                            w, bw = wdn[idn % 2], b_wdn[idn % 2]
                            idn += 1
                            for d2 in range(2):
                                dt = dq * 2 + d2
                                for (c0, cn) in chunks:
                                    pb, bpb = self.ps[6 + ev % 2], self.b_ps[6 + ev % 2]
                                    ev += 1
                                    for ht in range(HT):
                                        k.op("pe", lambda e: e.matmul(pb[:, 0:cn], w[:, ht, d2 * 128:(d2 + 1) * 128], act[:, ht, c0:c0 + cn],
                                                                      start=(ht == 0), stop=(ht == HT - 1)), r=[bw, b_act], w=[bpb])
                                    k.op("dve", lambda e: e.tensor_tensor(out=Yacc[:, dt, c0:c0 + cn], in0=pb[:, 0:cn], in1=Yacc[:, dt, c0:c0 + cn], op=ALU.add),
                                         r=[bpb, b_Y], w=[b_Y])
                    k.barrier()
                    pa.close()
                    pa = ExitStack()
                    self.moe_x = [self.sb(pa, "moe_x%d" % i, [128, D], F32) for i in range(2)]
                    self.b_moe_x = [Buf(), Buf()]
                    for tt in range(TG // 128):
                        s0 = p0 + tt * 128
                        r = NB if s0 < LC else b
                        xt_, bxt = self.moe_x[tt % 2], self.b_moe_x[tt % 2]
                        k.dma(None, xt_[:, :], self.XT[b, s0:s0 + 128, :], r=[self.b_XT[b]], w=[bxt])
                        for dt in range(KT):
                            k.op("pool", lambda e: e.tensor_scalar(out=Yacc[:, dt, tt * 128:(tt + 1) * 128], in0=Yacc[:, dt, tt * 128:(tt + 1) * 128],
                                                                   scalar1=self.modT[:, 5 * KT + dt, r:r + 1], scalar2=None, op0=ALU.mult),
                                 r=[b_Y, self.b_mod_sb], w=[b_Y])
                        for c in range(KT // 4):
                            pb, bpb = self.ps[c % 4], self.b_ps[c % 4]
                            for j in range(4):
                                dt = c * 4 + j
                                k.op("pe", lambda e: e.transpose(out=pb[:, j * 128:(j + 1) * 128], in_=Yacc[:, dt, tt * 128:(tt + 1) * 128],
                                                                 identity=self.ident[:]), r=[b_Y, self.b_ident], w=[bpb])
                            k.op("dve", lambda e: e.tensor_tensor(out=xt_[:, c * 512:(c + 1) * 512], in0=pb[:, :], in1=xt_[:, c * 512:(c + 1) * 512], op=ALU.add),
                                 r=[bpb, bxt], w=[bxt])
                        if last:
                            k.dma(None, self.yout[b, s0 - LC:s0 - LC + 128, :], xt_[:, :], r=[bxt], w=[self.b_yout])
                        else:
                            k.dma(None, self.XT[b, s0:s0 + 128, :], xt_[:, :], r=[bxt], w=[self.b_XT[b]])
                    k.barrier()
                    pa.close()

def host_constants(cfg):
    c = {}
    c["ident"] = np.eye(128, dtype=np.float32)
    f32 = np.float32
    LL = cfg.LL
    rows = LL // cfg.GRID_W
    row = np.repeat(np.arange(rows), cfg.GRID_W).astype(f32)
    col = (np.arange(rows * cfg.GRID_W) % cfg.GRID_W).astype(f32)
    inv_freq = (f32(10000.0) ** (-np.arange(16, dtype=f32) / f32(16))).astype(f32)
    ang_r = (row[:, None] * inv_freq).astype(f32)
    ang_c = (col[:, None] * inv_freq).astype(f32)
    cos = np.concatenate([np.cos(ang_r), np.cos(ang_r), np.cos(ang_c), np.cos(ang_c)], axis=1)
    sin = np.concatenate([np.sin(ang_r), np.sin(ang_r), np.sin(ang_c), np.sin(ang_c)], axis=1)
    c["c_cos"] = np.ascontiguousarray(cos.T.astype(f32))
    c["c_sin"] = np.ascontiguousarray(sin.T.astype(f32))
    Rm = np.zeros((64, 64), f32)
    for m in range(64):
        if (m % 32) < 16:
            Rm[m, m + 16] = -1.0
        else:
            Rm[m, m - 16] = 1.0
    c["c_rot"] = np.ascontiguousarray(Rm.T)
    ind2 = np.zeros((2, 128), f32)
    ind2[0, 0:64] = 1.0
    ind2[1, 64:128] = 1.0
    c["c_ind2"] = ind2
    c["c_antiI"] = np.ascontiguousarray(np.eye(128, dtype=f32)[::-1])
    j = np.arange(128)[:, None]
    i = np.arange(128)[None, :]
    c["c_mprev"] = (j >= i).astype(f32)
    c["c_mnext"] = (j <= i).astype(f32)
    return c


def make_in_maps(cfg, inputs):
    NB = cfg.NB
    consts = host_constants(cfg)
    maps = []
    x = np.asarray(inputs["x"], np.float32)
    ctx = np.asarray(inputs["ctx"], np.float32)
    c = np.asarray(inputs["c"], np.float32)
    c_ctx = np.asarray(inputs["c_ctx"], np.float32)
    shared = {}
    for name in ("norm1_g", "norm2_g", "w_mod", "b_mod", "w_in", "at_q_norm", "at_k_norm", "at_sink",
                 "s5_lam_re", "s5_lam_im", "s5_log_step", "s5_b_re", "s5_b_im", "s5_c_re", "s5_c_im", "s5_d",
                 "s5_glu_w", "s5_glu_b", "hg_f_bias", "hg_lb_logits", "hg_norm_g",
                 "w_out", "moe_router_w", "moe_router_b", "moe_w_gu", "moe_b_gu", "moe_w_down", "moe_b_down",
                 "rw_mu", "rw_w0", "rw_w2", "rw_a0", "rw_a2", "rw_g2", "rw_k_k", "rw_k_a", "rw_ln_w", "rw_ln_b"):
        shared[name] = np.ascontiguousarray(np.asarray(inputs[name], np.float32))
    shared["rw_r_k"] = np.ascontiguousarray(np.asarray(inputs["rw_r_k"], np.float32).reshape(cfg.DEPTH, 512))
    shared.update(consts)
    for i in range(cfg.NCORES):
        sl = slice(i * NB, (i + 1) * NB)
        m = dict(shared)
        m["xin"] = np.ascontiguousarray(np.concatenate([ctx[sl], x[sl]], axis=1))
        m["cvec"] = np.ascontiguousarray(np.concatenate([c[sl], c_ctx[None]], axis=0))
        maps.append(m)
    return maps


_CACHE = {}


def kernel(**inputs):
    cfg = Cfg()
    prog = Prog(cfg)
    nc = prog.build()
    maps = make_in_maps(cfg, inputs)
    res = run_bass_kernel_spmd(nc, maps, core_ids=list(range(cfg.NCORES)))
    outs = [r["y"] for r in res.results]
    return np.concatenate(outs, axis=0)
```
